# Optimizing a Trainium2 kernel written in Bass

```python
import jax, jax.numpy as jnp
from jax import lax
import numpy as np

D_MODEL = 1024
BATCH = 8
SEQ = 4096
DEPTH = 4

D_MIX = D_MODEL
D_LRU = D_MIX // 2
D_ATTN = D_MIX - D_LRU
LRU_BLOCKS = 8
LRU_BLOCK_DIM = D_LRU // LRU_BLOCKS
CONV_WIDTH = 4
LRU_C = 8.0
HEAD_DIM = 64
N_HEADS = D_ATTN // HEAD_DIM
DILATED_CONFIGS = ((128, 1), (512, 4), (2048, 16))
ATTN_BLOCK = 128
D_IN = 2 * D_LRU + 3 * D_ATTN
N_GROUPS = 4
EXPERTS_PER_GROUP = 8
N_EXPERTS = N_GROUPS * EXPERTS_PER_GROUP
TOP_K = 2
D_EXPERT = D_MODEL // 2
MOE_BLOCK = 128
EPS = 1e-6

kernel_name = "hymba_style_rglru_dilated_attn_hmoe"


def rms_norm(x, g):
    xf = x.astype(jnp.float32)
    y = xf * lax.rsqrt(jnp.mean(xf * xf, axis=-1, keepdims=True) + EPS)
    return (y * g.astype(jnp.float32)).astype(x.dtype)


def causal_depthwise_conv(x, w, b):
    k_width = w.shape[0]
    s_len = x.shape[1]
    xp = jnp.pad(x, ((0, 0), (k_width - 1, 0), (0, 0)))
    out = b
    for j in range(k_width):
        out = out + w[j] * xp[:, j:j + s_len]
    return out


def _lin_rec_combine(c1, c2):
    a1, b1 = c1
    a2, b2 = c2
    return a1 * a2, a2 * b1 + b2


def rg_lru(x, w_a, b_a, w_x, b_x, lam):
    xh = x.reshape(x.shape[0], x.shape[1], LRU_BLOCKS, LRU_BLOCK_DIM)
    r = jax.nn.sigmoid(jnp.einsum('bshi,hij->bshj', xh, w_a).reshape(x.shape) + b_a)
    i = jax.nn.sigmoid(jnp.einsum('bshi,hij->bshj', xh, w_x).reshape(x.shape) + b_x)
    log_a = -LRU_C * r * jax.nn.softplus(-lam)
    a = jnp.exp(log_a)
    u = jnp.sqrt(-jnp.expm1(2.0 * log_a)) * (i * x)
    _, h = lax.associative_scan(_lin_rec_combine, (a, u), axis=1)
    return h


def _dilated_branch(q, k, v, window, dilation):
    b, h, sp, dh = q.shape
    steps = window // dilation
    n_sub = sp // dilation
    nb = n_sub // ATTN_BLOCK

    def to_blocks(t):
        t = t.reshape(b, h, n_sub, dilation, dh).transpose(0, 1, 3, 2, 4)
        return t.reshape(b, h, dilation, nb, ATTN_BLOCK, dh)

    def with_prev(t):
        prev = jnp.pad(t, ((0, 0), (0, 0), (0, 0), (1, 0), (0, 0), (0, 0)))[:, :, :, :-1]
        return jnp.concatenate([prev, t], axis=4)

    qb = to_blocks(q)
    kc = with_prev(to_blocks(k))
    vc = with_prev(to_blocks(v))
    s = jnp.einsum('bhrnqd,bhrnkd->bhrnqk', qb, kc) * (HEAD_DIM ** -0.5)
    qi = jnp.arange(ATTN_BLOCK)[:, None]
    kj = jnp.arange(2 * ATTN_BLOCK)[None, :]
    dist = ATTN_BLOCK + qi - kj
    band = (dist >= 0) & (dist <= steps)
    first = (jnp.arange(nb) == 0)[:, None, None]
    valid = band[None] & ~(first & (kj < ATTN_BLOCK)[None])
    s = jnp.where(valid, s, -jnp.inf)
    m = jnp.max(s, axis=-1, keepdims=True)
    p = jnp.exp(s - m)
    denom = jnp.sum(p, axis=-1, keepdims=True)
    o = jnp.einsum('bhrnqk,bhrnkd->bhrnqd', p, vc) / denom
    lse = (m + jnp.log(denom))[..., 0]
    o = o.reshape(b, h, dilation, n_sub, dh).transpose(0, 1, 3, 2, 4).reshape(b, h, sp, dh)
    lse = lse.reshape(b, h, dilation, n_sub).transpose(0, 1, 3, 2).reshape(b, h, sp)
    return o, lse


def dilated_attention(q, k, v):
    s_len = q.shape[1]
    seg = ATTN_BLOCK * max(d for _, d in DILATED_CONFIGS)
    sp = -(-s_len // seg) * seg

    def prep(t):
        t = t.astype(jnp.float32).transpose(0, 2, 1, 3)
        return jnp.pad(t, ((0, 0), (0, 0), (0, sp - s_len), (0, 0)))

    qp, kp, vp = prep(q), prep(k), prep(v)
    outs = []
    lses = []
    for window, dilation in DILATED_CONFIGS:
        o, lse = _dilated_branch(qp, kp, vp, window, dilation)
        outs.append(o)
        lses.append(lse)
    wts = jax.nn.softmax(jnp.stack(lses, axis=0), axis=0)
    o = jnp.sum(jnp.stack(outs, axis=0) * wts[..., None], axis=0)
    return o[:, :, :s_len].transpose(0, 2, 1, 3)


def hierarchical_moe(h2, wg, bg, we, be, w_gate, w_up, w_down):
    n_tok, d = h2.shape
    hf = h2.astype(jnp.float32)
    group_prob = jax.nn.softmax(hf @ wg.astype(jnp.float32) + bg.astype(jnp.float32), axis=-1)
    p_top, g_idx = lax.top_k(group_prob, 1)
    expert_logits = (hf @ we.astype(jnp.float32) + be.astype(jnp.float32)).reshape(
        n_tok, N_GROUPS, EXPERTS_PER_GROUP)
    gi = jnp.broadcast_to(g_idx[:, :, None], (n_tok, 1, EXPERTS_PER_GROUP))
    local_logits = jnp.take_along_axis(expert_logits, gi, axis=1)[:, 0]
    top_vals, top_local = lax.top_k(local_logits, TOP_K)
    gate_w = jax.nn.softmax(top_vals, axis=-1) * p_top
    expert_idx = g_idx * EXPERTS_PER_GROUP + top_local

    n_asg = n_tok * TOP_K
    e_flat = expert_idx.reshape(n_asg)
    tok_flat = jnp.repeat(jnp.arange(n_tok, dtype=jnp.int32), TOP_K)
    g_flat = gate_w.reshape(n_asg)
    order = jnp.argsort(e_flat)
    e_s, tok_s, g_s = e_flat[order], tok_flat[order], g_flat[order]
    counts = jnp.bincount(e_flat, length=N_EXPERTS)
    padded = ((counts + MOE_BLOCK - 1) // MOE_BLOCK) * MOE_BLOCK
    start = jnp.cumsum(counts) - counts
    pend = jnp.cumsum(padded)
    pstart = pend - padded
    dest = pstart[e_s] + (jnp.arange(n_asg, dtype=jnp.int32) - start[e_s])
    n_rows = n_asg + N_EXPERTS * MOE_BLOCK
    n_blk = n_rows // MOE_BLOCK
    x_buf = jnp.zeros((n_rows, d), h2.dtype).at[dest].set(h2[tok_s])
    blk_expert = jnp.minimum(
        jnp.searchsorted(pend, jnp.arange(n_blk, dtype=jnp.int32) * MOE_BLOCK, side='right'),
        N_EXPERTS - 1)

    def expert_block(args):
        xb, e = args
        hid = jax.nn.silu(xb @ w_gate[e]) * (xb @ w_up[e])
        return hid @ w_down[e]

    y_buf = lax.map(expert_block, (x_buf.reshape(n_blk, MOE_BLOCK, d), blk_expert))
    y_buf = y_buf.reshape(n_rows, d)
    contrib = g_s[:, None].astype(y_buf.dtype) * y_buf[dest]
    return jnp.zeros((n_tok, d), h2.dtype).at[tok_s].add(contrib.astype(h2.dtype))


def setup_inputs(seed: int = 0) -> dict:
    key = jax.random.key(seed)
    ks = jax.random.split(key, 23)
    f32 = jnp.float32
    L = DEPTH

    def nrm(k, shape, scale):
        return jax.random.normal(k, shape, f32) * scale

    def gain(k, shape):
        return 1.0 + 0.05 * jax.random.normal(k, shape, f32)

    res_scale = (2.0 * DEPTH) ** -0.5
    u = jax.random.uniform(ks[9], (L, D_LRU), f32, 0.9, 0.999)
    a0 = u ** (1.0 / LRU_C)
    lam = jnp.log(a0) - jnp.log1p(-a0)
    return {
        "x": jax.random.normal(ks[0], (BATCH, SEQ, D_MODEL), f32),
        "norm_mix": gain(ks[1], (L, D_MODEL)),
        "w_in": nrm(ks[2], (L, D_MODEL, D_IN), D_MODEL ** -0.5),
        "conv_w": nrm(ks[3], (L, CONV_WIDTH, D_LRU), CONV_WIDTH ** -0.5),
        "conv_b": nrm(ks[4], (L, D_LRU), 0.02),
        "lru_w_a": nrm(ks[5], (L, LRU_BLOCKS, LRU_BLOCK_DIM, LRU_BLOCK_DIM), LRU_BLOCK_DIM ** -0.5),
        "lru_b_a": nrm(ks[6], (L, D_LRU), 0.02),
        "lru_w_x": nrm(ks[7], (L, LRU_BLOCKS, LRU_BLOCK_DIM, LRU_BLOCK_DIM), LRU_BLOCK_DIM ** -0.5),
        "lru_b_x": nrm(ks[8], (L, D_LRU), 0.02),
        "lru_lambda": lam,
        "q_norm": gain(ks[10], (L, HEAD_DIM)),
        "k_norm": gain(ks[11], (L, HEAD_DIM)),
        "norm_out_lru": gain(ks[12], (L, D_LRU)),
        "norm_out_attn": gain(ks[13], (L, D_ATTN)),
        "w_out": nrm(ks[14], (L, D_MIX, D_MODEL), D_MIX ** -0.5 * res_scale),
        "norm_ffn": gain(ks[15], (L, D_MODEL)),
        "router_group_w": nrm(ks[16], (L, D_MODEL, N_GROUPS), D_MODEL ** -0.5),
        "router_group_b": nrm(ks[17], (L, N_GROUPS), 0.01),
        "router_expert_w": nrm(ks[18], (L, D_MODEL, N_EXPERTS), D_MODEL ** -0.5),
        "router_expert_b": nrm(ks[19], (L, N_EXPERTS), 0.01),
        "w_gate": nrm(ks[20], (L, N_EXPERTS, D_MODEL, D_EXPERT), D_MODEL ** -0.5),
        "w_up": nrm(ks[21], (L, N_EXPERTS, D_MODEL, D_EXPERT), D_MODEL ** -0.5),
        "w_down": nrm(ks[22], (L, N_EXPERTS, D_EXPERT, D_MODEL), D_EXPERT ** -0.5 * res_scale),
    }


def reference(x, norm_mix, w_in, conv_w, conv_b, lru_w_a, lru_b_a, lru_w_x, lru_b_x,
              lru_lambda, q_norm, k_norm, norm_out_lru, norm_out_attn, w_out, norm_ffn,
              router_group_w, router_group_b, router_expert_w, router_expert_b,
              w_gate, w_up, w_down):
    b, s_len, d = x.shape
    o1 = D_LRU
    o2 = 2 * D_LRU
    o3 = o2 + D_ATTN
    o4 = o3 + D_ATTN
    for l in range(DEPTH):
        h = rms_norm(x, norm_mix[l])
        proj = jnp.einsum('bsd,de->bse', h, w_in[l])
        x_lru = proj[..., :o1]
        gate_lru = proj[..., o1:o2]
        q = proj[..., o2:o3].reshape(b, s_len, N_HEADS, HEAD_DIM)
        k = proj[..., o3:o4].reshape(b, s_len, N_HEADS, HEAD_DIM)
        v = proj[..., o4:].reshape(b, s_len, N_HEADS, HEAD_DIM)

        xc = causal_depthwise_conv(x_lru, conv_w[l], conv_b[l]).astype(jnp.float32)
        hl = rg_lru(xc, lru_w_a[l], lru_b_a[l], lru_w_x[l], lru_b_x[l], lru_lambda[l])
        y_lru = (hl * jax.nn.gelu(gate_lru.astype(jnp.float32))).astype(x.dtype)

        q = rms_norm(q, q_norm[l])
        k = rms_norm(k, k_norm[l])
        y_attn = dilated_attention(q, k, v).reshape(b, s_len, D_ATTN).astype(x.dtype)

        merged = jnp.concatenate(
            [rms_norm(y_lru, norm_out_lru[l]), rms_norm(y_attn, norm_out_attn[l])], axis=-1)
        x = x + jnp.einsum('bse,ed->bsd', merged, w_out[l])

        h2 = rms_norm(x, norm_ffn[l]).reshape(b * s_len, d)
        y = hierarchical_moe(h2, router_group_w[l], router_group_b[l], router_expert_w[l],
                             router_expert_b[l], w_gate[l], w_up[l], w_down[l])
        x = x + y.reshape(b, s_len, d)
    return x
```

```python
from contextlib import ExitStack

import numpy as np
import concourse.bass as bass
import concourse.mybir as mybir
from concourse.bass_utils import run_bass_kernel_spmd

F32 = mybir.dt.float32
F32R = mybir.dt.float32r
BF16 = mybir.dt.bfloat16
AF = mybir.ActivationFunctionType
ALU = mybir.AluOpType

S = 4096
D = 1024
DEPTH = 4
DIN = 2560
NE = 32
EPS = 1e-6
SAME_ENGINE_SYNC = True
MOE_SPARSE = True
_DBG_SKIP_W = False
NSB = 47
I32 = mybir.dt.int32


class Buf:
    __slots__ = ("ap", "w", "r", "name")

    def __init__(self, ap, name=""):
        self.ap = ap
        self.w = {}
        self.r = {}
        self.name = name

    def __getitem__(self, k):
        return self.ap[k]


def _merge(d, ev):
    for k, v in ev.items():
        if d.get(k, 0) < v:
            d[k] = v


class Sched:
    def __init__(self, nc, n_dma_sems=24):
        self.nc = nc
        self.E = {"pe": nc.tensor, "act": nc.scalar, "dve": nc.vector, "pool": nc.gpsimd, "sp": nc.sync}
        self.csem = {}
        self.ccnt = {}
        for e in ("pe", "act", "dve", "pool"):
            self.csem[e] = nc.alloc_semaphore("c_" + e)
            self.ccnt[e] = 0
        self.nring = n_dma_sems
        self.dsem = [nc.alloc_semaphore("d_%d" % i) for i in range(2 * n_dma_sems)]
        self.dcnt = [0] * (2 * n_dma_sems)
        self.dnext = {False: 0, True: 0}
        self.seen = {e: {} for e in self.E}
        self.n_inst = 0
        self.n_wait = 0
        self.bregs = {}

    def _sem(self, key):
        return self.csem[key[1]] if key[0] == "c" else self.dsem[key[1]]

    def _wait(self, eng, ev):
        seen = self.seen[eng]
        for key, val in ev.items():
            if key[0] == "c" and key[1] == eng:
                if eng == "pe" or not SAME_ENGINE_SYNC:
                    continue
            if seen.get(key, 0) >= val:
                continue
            self.E[eng].wait_ge(self._sem(key), val)
            seen[key] = val
            self.n_wait += 1

    def _deps(self, eng, reads, writes):
        for b in reads:
            self._wait(eng, b.w)
        for b in writes:
            self._wait(eng, b.w)
            self._wait(eng, b.r)

    def _post(self, ev, reads, writes):
        for b in reads:
            _merge(b.r, ev)
        for b in writes:
            _merge(b.w, ev)

    def op(self, eng, fn, reads=(), writes=()):
        self._deps(eng, reads, writes)
        inst = fn(self.E[eng])
        self.ccnt[eng] += 1
        inst.then_inc(self.csem[eng], 1)
        ev = {("c", eng): self.ccnt[eng]}
        self._post(ev, reads, writes)
        self.n_inst += 1
        return ev

    def _ring(self, sw):
        i = self.dnext[sw]
        self.dnext[sw] = (i + 1) % self.nring
        return i + (self.nring if sw else 0)

    def dma(self, eng, out, in_, reads=(), writes=(), **kw):
        i = self._ring(eng == "pool")
        if self.dcnt[i] > 0:
            self._wait(eng, {("d", i): self.dcnt[i]})
        self._deps(eng, reads, writes)
        self.dcnt[i] += 16
        self.E[eng].dma_start(out=out, in_=in_, **kw).then_inc(self.dsem[i], 16)
        ev = {("d", i): self.dcnt[i]}
        self._post(ev, reads, writes)
        self.n_inst += 1
        return ev

    def indirect(self, out, out_off, in_, in_off, reads=(), writes=(), bounds=None):
        i = self._ring(True)
        if self.dcnt[i] > 0:
            self._wait("pool", {("d", i): self.dcnt[i]})
        self._deps("pool", reads, writes)
        self.dcnt[i] += 16
        kw = {}
        if bounds is not None:
            if bounds not in self.bregs:
                self.bregs[bounds] = self.nc.gpsimd.to_reg(bounds)
            kw = dict(bounds_check=self.bregs[bounds], oob_is_err=False)
        self.nc.gpsimd.indirect_dma_start(out=out, out_offset=out_off, in_=in_, in_offset=in_off, **kw).then_inc(
            self.dsem[i], 16)
        ev = {("d", i): self.dcnt[i]}
        self._post(ev, reads, writes)
        self.n_inst += 1
        return ev

    def barrier(self):
        allev = {}
        for e, c in self.ccnt.items():
            if c:
                allev[("c", e)] = c
        for i, c in enumerate(self.dcnt):
            if c:
                allev[("d", i)] = c
        for eng in self.E:
            self._wait(eng, allev)


def build(n_layers=DEPTH, debug=False, stop_after=None):
    nc = bass.Bass("TRN2", target_bir_lowering=False)
    s = Sched(nc)

    def din(name, shape):
        return nc.dram_tensor(name, shape, F32, kind="ExternalInput").ap()

    def dtmp(name, shape, dt=F32):
        return nc.dram_tensor(name, shape, dt, kind=("ExternalOutput" if debug else "Internal")).ap()

    x_in = din("x", [S, D])
    norm_mix = din("norm_mix", [DEPTH, D])
    w_in = din("w_in", [DEPTH, D, DIN])
    conv_w = din("conv_w", [DEPTH, 4, 512])
    conv_b = din("conv_b", [DEPTH, 512])
    lru_w_a = din("lru_w_a", [DEPTH, 8, 64, 64])
    lru_b_a = din("lru_b_a", [DEPTH, 512])
    lru_w_x = din("lru_w_x", [DEPTH, 8, 64, 64])
    lru_b_x = din("lru_b_x", [DEPTH, 512])
    lru_lambda = din("lru_lambda", [DEPTH, 512])
    q_norm = din("q_norm", [DEPTH, 64])
    k_norm = din("k_norm", [DEPTH, 64])
    norm_out_lru = din("norm_out_lru", [DEPTH, 512])
    norm_out_attn = din("norm_out_attn", [DEPTH, 512])
    w_out = din("w_out", [DEPTH, D, D])
    norm_ffn = din("norm_ffn", [DEPTH, D])
    router_group_w = din("router_group_w", [DEPTH, D, 4])
    router_group_b = din("router_group_b", [DEPTH, 4])
    router_expert_w = din("router_expert_w", [DEPTH, D, NE])
    router_expert_b = din("router_expert_b", [DEPTH, NE])
    w_gate = din("w_gate", [DEPTH, NE, D, 512])
    w_up = din("w_up", [DEPTH, NE, D, 512])
    w_down = din("w_down", [DEPTH, NE, 512, D])
    c_ident = din("c_ident", [128, 128])
    c_mask = din("c_mask", [128, 256])
    c_lt = din("c_lt", [128, 128])
    c_mbias = din("c_mbias", [128, 256])
    c_pio2 = din("c_pio2", [128, 2])
    out = nc.dram_tensor("out", [S, D], F32, kind="ExternalOutput").ap()

    projT = dtmp("projT", [2048, S])
    vtok = dtmp("vtok", [S, 512], BF16)
    ymixT = dtmp("ymixT", [1024, S])
    mergedT = dtmp("mergedT", [1024, S])
    xmid = dtmp("xmid", [S, D])
    h2T = dtmp("h2T", [1024, S])
    xres = [dtmp("xres%d" % i, [S, D]) for i in range(2)]
    h2tok = dtmp("h2tok", [S, D])
    xbuf = dtmp("xbuf", [NSB * 512, D])
    ybuf = dtmp("ybuf", [NSB * 512, D])

    tcount = [0]

    def tile(es, name, shape, dt=F32):
        tcount[0] += 1
        name = "%s_%d" % (name, tcount[0])
        return Buf(es.enter_context(nc.sbuf_tensor(name, shape, dt)), name)

    gstack = ExitStack()
    ident = tile(gstack, "ident", [128, 128])
    ones_r = tile(gstack, "ones_r", [128, 128], F32R)
    ones_b = tile(gstack, "ones_b", [128, 128], BF16)
    OH1 = tile(gstack, "OH1", [128, 32, NE])
    OH2 = tile(gstack, "OH2", [128, 32, NE])
    W12 = tile(gstack, "W12", [128, 32, 2])
    S1i = tile(gstack, "S1i", [128, 32], I32)
    S2i = tile(gstack, "S2i", [128, 32], I32)
    IDXWi = tile(gstack, "IDXWi", [128, NSB, 2], I32)
    pio2 = tile(gstack, "pio2", [128, 2])
    Ltb = tile(gstack, "Ltb", [128, 128], BF16)
    identb = tile(gstack, "identb", [128, 128], BF16)
    mbias = tile(gstack, "mbias", [128, 256], BF16)
    Gall = None if MOE_SPARSE else tile(gstack, "Gall", [128, 32, NE])
    banks = [Buf(nc.alloc_psum_tensor("bank%d" % i, [128, 512], F32), "bank%d" % i) for i in range(8)]

    s.dma("sp", ident[:], c_ident[:, :], writes=[ident])
    s.dma("pool", Ltb[:], c_lt[:, :], writes=[Ltb])
    s.dma("pool", identb[:], c_ident[:, :], writes=[identb])
    s.dma("pool", mbias[:], c_mbias[:, :], writes=[mbias])
    s.dma("sp", pio2[:], c_pio2[:, :], writes=[pio2])
    ones_f = tile(gstack, "ones_f", [128, 128])
    eps_t = tile(gstack, "eps_t", [128, 1])
    s.op("dve", lambda e: e.memset(eps_t[:], EPS), writes=[eps_t])
    zeros_f = tile(gstack, "zeros_f", [128, 512])
    s.op("dve", lambda e: e.memset(ones_f[:], 1.0), writes=[ones_f])
    s.op("dve", lambda e: e.memset(zeros_f[:], 0.0), writes=[zeros_f])
    s.op("dve", lambda e: e.tensor_copy(out=ones_r[:], in_=ones_f[:]), reads=[ones_f], writes=[ones_r])
    s.op("dve", lambda e: e.memset(ones_b[:], 1.0), writes=[ones_b])

    def evac(i, dst_ap, src_ap, reads, writes):
        if i % 2 == 0:
            return s.op("act", lambda e: e.copy(out=dst_ap, in_=src_ap), reads=reads, writes=writes)
        return s.op("dve", lambda e: e.tensor_copy(out=dst_ap, in_=src_ap), reads=reads, writes=writes)

    def rstd_inplace(T, ap, scale):
        np_ = ap.partition_size()
        s.op("act", lambda e: e.activation(out=ap, in_=ap, func=AF.Ln, scale=scale, bias=eps_t[0:np_, 0:1]),
             reads=[T, eps_t], writes=[T])
        s.op("act", lambda e: e.activation(out=ap, in_=ap, func=AF.Exp, scale=-0.5), reads=[T], writes=[T])

    def rms_token_major(X, Y, SS, junk, gbc):
        for j in range(4):
            jt, jap = (junk, junk[:]) if junk is not None else (Y, Y[:, j, :])
            s.op("act", lambda e, j=j, jap=jap: e.activation(out=jap, in_=X[:, j, :], func=AF.Square,
                                                             accum_out=SS[:, j:j + 1]),
                 reads=[X], writes=[jt, SS])
        rstd_inplace(SS, SS[:], 1.0 / D)
        for j in range(4):
            s.op("dve", lambda e, j=j: e.scalar_tensor_tensor(out=Y[:, j, :], in0=X[:, j, :], scalar=SS[:, j:j + 1],
                                                              in1=gbc[:], op0=ALU.mult, op1=ALU.mult),
                 reads=[X, SS, gbc], writes=[Y])

    def transpose_chunk(X, HT, pbanks):
        for k in range(8):
            pT = pbanks[k % len(pbanks)]
            for j in range(4):
                s.op("pe", lambda e, j=j, k=k, pT=pT: e.transpose(pT[:, j * 128:(j + 1) * 128],
                                                                  X[:, j, k * 128:(k + 1) * 128], ident[:]),
                     reads=[X, ident], writes=[pT])
            evac(k, HT[:, k, :], pT[:, :], [pT], [HT])

    def phase_A(l, xsrc, fuse_g=False, xstore=None):
        with ExitStack() as es:
            win = tile(es, "win", [128, 8, DIN], F32R)
            gbc = tile(es, "gbc", [128, D])
            xin = [tile(es, "xin%d" % i, [128, 4, D]) for i in range(3)]
            junk = tile(es, "junk", [128, D])
            ss = [tile(es, "ss%d" % i, [128, 4]) for i in range(3)]
            hT = [tile(es, "hT%d" % i, [128, 8, 512], F32R) for i in range(2)]
            ost = [tile(es, "ost%d" % i, [128, 512]) for i in range(4)]
            vst = [tile(es, "vst%d" % i, [128, 512], BF16) for i in range(2)]
            if fuse_g:
                y1t = [tile(es, "y1t%d" % i, [128, D]) for i in range(2)]
                y2t = [tile(es, "y2t%d" % i, [128, D]) for i in range(2)]
            wsrc = w_in[l].rearrange("(k p) n -> p k n", p=128)
            for c5 in range(5):
                s.dma("pool", win[:, :, c5 * 512:(c5 + 1) * 512], wsrc[:, :, c5 * 512:(c5 + 1) * 512], writes=[win])
            s.dma("pool", gbc[:], norm_mix[l].partition_broadcast(128), writes=[gbc])

            def load(c):
                X = xin[c % 3]
                rows = slice(c * 512, (c + 1) * 512)
                s.dma("sp", X[:], xsrc[rows, :].rearrange("(j p) d -> p j d", p=128), writes=[X])
                if fuse_g:
                    for j in range(4):
                        t = c * 4 + j
                        Y1, Y2 = y1t[t % 2], y2t[t % 2]
                        s.indirect(Y1[:], None, ybuf[:, :], IOA(ap=S1i[:, t:t + 1], axis=0), reads=[S1i], writes=[Y1])
                        s.indirect(Y2[:], None, ybuf[:, :], IOA(ap=S2i[:, t:t + 1], axis=0), reads=[S2i], writes=[Y2])
                        s.op("dve", lambda e, Y1=Y1, t=t, j=j: e.scalar_tensor_tensor(
                            out=X[:, j, :], in0=Y1[:], scalar=W12[:, t, 0:1], in1=X[:, j, :], op0=ALU.mult, op1=ALU.add),
                            reads=[Y1, W12, X], writes=[X])
                        s.op("dve", lambda e, Y2=Y2, t=t, j=j: e.scalar_tensor_tensor(
                            out=X[:, j, :], in0=Y2[:], scalar=W12[:, t, 1:2], in1=X[:, j, :], op0=ALU.mult, op1=ALU.add),
                            reads=[Y2, W12, X], writes=[X])
                    s.dma("sp", xstore[rows, :].rearrange("(j p) d -> p j d", p=128), X[:], reads=[X])

            def prep_rms(c):
                rms_token_major(xin[c % 3], xin[c % 3], ss[c % 3], junk, gbc)

            def prep_T(c):
                transpose_chunk(xin[c % 3], hT[c % 2], banks[0:2])

            evc = [0]

            def mm(c):
                HT = hT[c % 2]
                for f in range(16):
                    pO = banks[2 + f % 4]
                    for k in range(8):
                        s.op("pe", lambda e, f=f, k=k, pO=pO: e.matmul(pO[:, :], lhsT=win[:, k, f * 128:(f + 1) * 128],
                                                                       rhs=HT[:, k, :], start=(k == 0), stop=(k == 7)),
                             reads=[win, HT], writes=[pO])
                    O = ost[f % 4]
                    evac(evc[0], O[:], pO[:, :], [pO], [O])
                    evc[0] += 1
                    s.dma("sp", projT[f * 128:(f + 1) * 128, c * 512:(c + 1) * 512], O[:], reads=[O])
                for j in range(4):
                    pV = banks[6 + j % 2]
                    for k in range(8):
                        s.op("pe", lambda e, j=j, k=k, pV=pV: e.matmul(pV[:, :], lhsT=HT[:, k, j * 128:(j + 1) * 128],
                                                                       rhs=win[:, k, 2048:2560], start=(k == 0),
                                                                       stop=(k == 7)),
                             reads=[win, HT], writes=[pV])
                    V = vst[j % 2]
                    evac(evc[0], V[:], pV[:, :], [pV], [V])
                    evc[0] += 1
                    t0 = c * 512 + j * 128
                    s.dma("sp", vtok[t0:t0 + 128, :], V[:], reads=[V])

            load(0)
            load(1)
            if MOE_SPARSE and l == 0:
                zt = tile(es, "zt", [128, 2, D])
                s.op("pool", lambda e: e.memset(zt[:], 0.0), writes=[zt])
                for i in range(NSB * 2):
                    s.dma("pool", xbuf[i * 256:(i + 1) * 256, :].rearrange("(j p) d -> p j d", p=128), zt[:], reads=[zt])
            prep_rms(0)
            prep_T(0)
            prep_rms(1)
            for c in range(8):
                if c + 2 < 8:
                    load(c + 2)
                    prep_rms(c + 2)
                if c + 1 < 8:
                    prep_T(c + 1)
                mm(c)
        s.barrier()

    def phase_B(l):
        HT_ = 1024
        NQ = S // HT_
        with ExitStack() as es:
            lp = tile(es, "lp", [128, 4, 16])
            tmpp = tile(es, "tmpp", [128, 4, 4])
            wab = tile(es, "wab", [128, 4, 128], F32R)
            wxb = tile(es, "wxb", [128, 4, 128], F32R)
            xl_2 = [tile(es, "xl%d" % i_, [128, 3 + HT_], F32R) for i_ in range(2)]
            xc_2 = [tile(es, "xc%d" % i_, [128, HT_], F32R) for i_ in range(2)]
            dg = tile(es, "dg", [128, 4, 128], F32R)
            rt_2 = [tile(es, "rt%d" % i_, [128, HT_]) for i_ in range(2)]
            it_2 = [tile(es, "it%d" % i_, [128, HT_]) for i_ in range(2)]
            wt_2 = [tile(es, "wt%d" % i_, [128, HT_]) for i_ in range(2)]
            em_2 = [tile(es, "em%d" % i_, [128, HT_]) for i_ in range(2)]
            t1_2 = [tile(es, "t1%d" % i_, [128, HT_]) for i_ in range(2)]
            at_2 = [tile(es, "at%d" % i_, [128, HT_]) for i_ in range(2)]
            hh_2 = [tile(es, "hh%d" % i_, [128, HT_]) for i_ in range(2)]
            gt_2 = [tile(es, "gt%d" % i_, [128, HT_]) for i_ in range(2)]
            t2_2 = [tile(es, "t2%d" % i_, [128, HT_]) for i_ in range(2)]
            hlast = tile(es, "hlast", [128, 1])
            def pload(col, src):
                s.dma("sp", lp[:, :, col], src.rearrange("(j p) -> p j", p=128), writes=[lp],
                      allow_slow_non_contiguous=True)
            for t in range(4):
                pload(t, conv_w[l, t])
            pload(4, conv_b[l])
            pload(5, lru_b_a[l])
            pload(6, lru_b_x[l])
            pload(7, lru_lambda[l])
            z = tmpp[:, :, 0]
            q = tmpp[:, :, 1]
            lnz = tmpp[:, :, 2]
            msk = tmpp[:, :, 3]
            s.op("act", lambda e: e.activation(out=z, in_=lp[:, :, 7], func=AF.Exp, scale=-1.0), reads=[lp], writes=[tmpp])
            s.op("act", lambda e: e.activation(out=lnz, in_=z, func=AF.Ln, bias=1.0), reads=[tmpp], writes=[tmpp])
            coef = [1.0, -1.0 / 2, 1.0 / 3, -1.0 / 4, 1.0 / 5, -1.0 / 6, 1.0 / 7]
            s.op("dve", lambda e: e.tensor_scalar(out=q, in0=z, scalar1=coef[6], scalar2=None, op0=ALU.mult),
                 reads=[tmpp], writes=[tmpp])
            for ci in (5, 4, 3, 2, 1, 0):
                s.op("dve", lambda e, ci=ci: e.scalar_tensor_tensor(out=q, in0=q, scalar=coef[ci], in1=z, op0=ALU.add,
                                                                    op1=ALU.mult), reads=[tmpp], writes=[tmpp])
            s.op("dve", lambda e: e.tensor_scalar(out=msk, in0=z, scalar1=0.1, scalar2=None, op0=ALU.is_lt),
                 reads=[tmpp], writes=[tmpp])
            s.op("dve", lambda e: e.tensor_tensor(out=q, in0=q, in1=lnz, op=ALU.subtract), reads=[tmpp], writes=[tmpp])
            s.op("dve", lambda e: e.tensor_tensor(out=q, in0=q, in1=msk, op=ALU.mult), reads=[tmpp], writes=[tmpp])
            s.op("dve", lambda e: e.tensor_tensor(out=q, in0=q, in1=lnz, op=ALU.add), reads=[tmpp], writes=[tmpp])
            s.op("dve", lambda e: e.tensor_scalar(out=lp[:, :, 8], in0=q, scalar1=-8.0, scalar2=None, op0=ALU.mult),
                 reads=[tmpp], writes=[lp])
            s.op("dve", lambda e: e.tensor_scalar(out=lp[:, :, 9], in0=q, scalar1=-4.0, scalar2=None, op0=ALU.mult),
                 reads=[tmpp], writes=[lp])
            s.op("dve", lambda e: e.tensor_copy(out=wab[:].rearrange("p a b -> p (a b)"), in_=zeros_f[:]),
                 reads=[zeros_f], writes=[wab])
            s.op("dve", lambda e: e.tensor_copy(out=wxb[:].rearrange("p a b -> p (a b)"), in_=zeros_f[:]),
                 reads=[zeros_f], writes=[wxb])
            for jj in range(4):
                for hb in range(2):
                    p0 = hb * 64
                    s.dma("pool", wab[p0:p0 + 64, jj, p0:p0 + 64], lru_w_a[l, 2 * jj + hb], writes=[wab])
                    s.dma("pool", wxb[p0:p0 + 64, jj, p0:p0 + 64], lru_w_x[l, 2 * jj + hb], writes=[wxb])
            bk = [0]
            def run_pass(jj, hf, pi_):
                t0 = hf * HT_
                xl, xc, rt, it, wt, em = xl_2[pi_ % 2], xc_2[pi_ % 2], rt_2[pi_ % 2], it_2[pi_ % 2], wt_2[pi_ % 2], em_2[pi_ % 2]
                t1, at, hh, gt, t2 = t1_2[pi_ % 2], at_2[pi_ % 2], hh_2[pi_ % 2], gt_2[pi_ % 2], t2_2[pi_ % 2]
                if hf == 0:
                    s.op("dve", lambda e: e.tensor_copy(out=xl[:, 0:3], in_=zeros_f[:, 0:3]), reads=[zeros_f],
                         writes=[xl])
                    yield
                    s.dma("pool", xl[:, 3:3 + HT_], projT[jj * 128:(jj + 1) * 128, 0:HT_], writes=[xl])
                    yield
                    for k in range(4):
                        s.op("dve", lambda e, jj=jj, k=k: e.tensor_scalar(out=dg[:, k, :], in0=ident[:],
                                                                          scalar1=lp[:, jj, k:k + 1], scalar2=None,
                                                                          op0=ALU.mult), reads=[ident, lp], writes=[dg])
                        yield
                else:
                    s.dma("pool", xl[:, :], projT[jj * 128:(jj + 1) * 128, t0 - 3:t0 + HT_], writes=[xl])
                    yield
                s.dma("sp", gt[:], projT[512 + jj * 128:512 + (jj + 1) * 128, t0:t0 + HT_], writes=[gt])
                yield
                for cb in range(HT_ // 512):
                    pC = banks[bk[0] % 8]
                    bk[0] += 1
                    for k in range(4):
                        s.op("pe", lambda e, pC=pC, cb=cb, k=k: e.matmul(
                            pC[:, :], lhsT=dg[:, k, :], rhs=xl[:, cb * 512 + k:cb * 512 + k + 512], start=(k == 0),
                            stop=(k == 3)), reads=[dg, xl], writes=[pC])
                        yield
                    s.op("act", lambda e, pC=pC, cb=cb, jj=jj: e.add(out=xc[:, cb * 512:(cb + 1) * 512], in_=pC[:, :],
                                                                     add=lp[:, jj, 4:5]), reads=[pC, lp], writes=[xc])
                    yield
                for cb in range(HT_ // 512):
                    cols = slice(cb * 512, (cb + 1) * 512)
                    for (wb, dst, bcol) in ((wab, rt, 5), (wxb, it, 6)):
                        pR = banks[bk[0] % 8]
                        bk[0] += 1
                        s.op("pe", lambda e, wb=wb, pR=pR, cols=cols, jj=jj: e.matmul(
                            pR[:, :], lhsT=wb[:, jj, :], rhs=xc[:, cols], start=True, stop=True),
                            reads=[wb, xc], writes=[pR])
                        yield
                        s.op("act", lambda e, dst=dst, pR=pR, cols=cols, jj=jj, bcol=bcol: e.activation(
                            out=dst[:, cols], in_=pR[:, :], func=AF.Sigmoid, bias=lp[:, jj, bcol:bcol + 1]),
                            reads=[pR, lp], writes=[dst])
                        yield
                s.op("act", lambda e, jj=jj: e.activation(out=at[:], in_=rt[:], func=AF.Exp, scale=lp[:, jj, 8:9]),
                     reads=[rt, lp], writes=[at])
                yield
                s.op("act", lambda e, jj=jj: e.activation(out=t1[:], in_=rt[:], func=AF.Tanh, scale=lp[:, jj, 9:10]),
                     reads=[rt, lp], writes=[t1])
                yield
                s.op("dve", lambda e: e.scalar_tensor_tensor(out=em[:], in0=at[:], scalar=1.0, in1=t1[:], op0=ALU.add,
                                                             op1=ALU.mult), reads=[at, t1], writes=[em])
                yield
                s.op("dve", lambda e: e.scalar_tensor_tensor(out=t1[:], in0=em[:], scalar=2.0, in1=em[:], op0=ALU.add,
                                                             op1=ALU.mult), reads=[em], writes=[t1])
                yield
                s.op("act", lambda e: e.activation(out=t1[:], in_=t1[:], func=AF.Sqrt, scale=-1.0),
                     reads=[t1], writes=[t1])
                yield
                s.op("pool", lambda e: e.tensor_tensor(out=it[:], in0=it[:], in1=xc[:].bitcast(F32), op=ALU.mult),
                     reads=[it, xc], writes=[it])
                yield
                s.op("dve", lambda e: e.tensor_tensor(out=it[:], in0=it[:], in1=t1[:], op=ALU.mult),
                     reads=[it, t1], writes=[it])
                yield
                if hf == 0:
                    s.op("dve", lambda e: e.tensor_tensor_scan(out=hh[:], data0=at[:], data1=it[:], initial=0.0,
                                                               op0=ALU.mult, op1=ALU.add),
                         reads=[at, it], writes=[hh])
                    yield
                else:
                    s.op("dve", lambda e: e.tensor_tensor_scan(out=hh[:], data0=at[:], data1=it[:],
                                                               initial=hlast[:, 0:1], op0=ALU.mult, op1=ALU.add),
                         reads=[at, it, hlast], writes=[hh])
                    yield
                if hf + 1 < NQ:
                    s.op("act", lambda e: e.copy(out=hlast[:, 0:1], in_=hh[:, HT_ - 1:HT_]), reads=[hh], writes=[hlast])
                    yield
                s.op("pool", lambda e: e.tensor_tensor(out=t2[:], in0=gt[:], in1=gt[:], op=ALU.mult),
                     reads=[gt], writes=[t2])
                yield
                s.op("pool", lambda e: e.tensor_scalar(out=t2[:], in0=t2[:], scalar1=0.044715, scalar2=1.0, op0=ALU.mult,
                                                       op1=ALU.add), reads=[t2], writes=[t2])
                yield
                s.op("pool", lambda e: e.tensor_tensor(out=t2[:], in0=t2[:], in1=gt[:], op=ALU.mult),
                     reads=[t2, gt], writes=[t2])
                yield
                s.op("act", lambda e: e.activation(out=t2[:], in_=t2[:], func=AF.Sigmoid, scale=1.5957691216057308),
                     reads=[t2], writes=[t2])
                yield
                s.op("pool", lambda e: e.tensor_tensor(out=t2[:], in0=t2[:], in1=gt[:], op=ALU.mult),
                     reads=[t2, gt], writes=[t2])
                yield
                s.op("dve", lambda e: e.tensor_tensor(out=t2[:], in0=t2[:], in1=hh[:], op=ALU.mult),
                     reads=[t2, hh], writes=[t2])
                yield
                s.dma("sp", ymixT[jj * 128:(jj + 1) * 128, t0:t0 + HT_], t2[:], reads=[t2])
                yield

            passes = [(jj, hf) for jj in range(4) for hf in range(NQ)]
            gens = [run_pass(jj, hf, i_) for i_, (jj, hf) in enumerate(passes)]
            SKEW = 18
            active = []
            nxt = 0
            while active or nxt < len(gens):
                if nxt < len(gens) and len(active) < 2 and (not active or active[0][1] >= SKEW):
                    active.append([gens[nxt], 0])
                    nxt += 1
                for ent in list(active):
                    try:
                        next(ent[0])
                        ent[1] += 1
                    except StopIteration:
                        active.remove(ent)
        s.barrier()

    def phase_C(l):
        NBUF, LA = 6, 3
        with ExitStack() as es:
            gqk = tile(es, "gqk", [64, 2])
            qraw = [tile(es, "qraw%d" % i, [64, S]) for i in range(2)]
            kraw = [tile(es, "kraw%d" % i, [64, S]) for i in range(2)]
            qn = [tile(es, "qn%d" % i, [64, S], BF16) for i in range(2)]
            kn = [tile(es, "kn%d" % i, [64, S], BF16) for i in range(2)]
            Vd = [[tile(es, "Vd%d_%d" % (i, j), [128, 32, 128], BF16) for j in range(3)] for i in range(2)]
            for i in range(2):
                for j in range(3):
                    s.op("dve", lambda e, i=i, j=j: e.memset(Vd[i][j][:], 1.0), writes=[Vd[i][j]])
            acc = tile(es, "acc", [128, S])
            dsh = tile(es, "dsh", [64, S])
            sq = [tile(es, "sq%d" % i, [64, 512], F32R) for i in range(2)]
            rs = [tile(es, "rs%d" % i, [64, 512]) for i in range(2)]
            pm_ = [tile(es, "pm%d" % i, [128, 256], BF16) for i in range(NBUF)]
            NPS = 3
            pS_ = [banks[i] for i in range(NPS)]
            pO_ = [banks[3 + i] for i in range(NPS)]
            pN_ = [banks[6], banks[7]]
            s.dma("sp", gqk[:, 0:1], q_norm[l].rearrange("(p o) -> p o", o=1), writes=[gqk])
            s.dma("sp", gqk[:, 1:2], k_norm[l].rearrange("(p o) -> p o", o=1), writes=[gqk])

            def load(h):
                s.dma("sp", qraw[h % 2][:], projT[1024 + h * 64:1024 + (h + 1) * 64, :], writes=[qraw[h % 2]])
                s.dma("sp", kraw[h % 2][:], projT[1536 + h * 64:1536 + (h + 1) * 64, :], writes=[kraw[h % 2]])
                for bi, d in enumerate((1, 4, 16)):
                    nb = 32 // d
                    vv = vtok.rearrange("(n p r) c -> r p n c", p=128, r=d)
                    V = Vd[h % 2][bi]
                    for r in range(d):
                        s.dma("sp", V[:, r * nb:(r + 1) * nb, 0:64], vv[r][:, :, h * 64:(h + 1) * 64], writes=[V])

            def qknorm_steps(h):
                steps = []
                for (raw, nrm, gc) in ((qraw[h % 2], qn[h % 2], 0), (kraw[h % 2], kn[h % 2], 1)):
                    for cb in range(8):
                        def step(raw=raw, nrm=nrm, gc=gc, cb=cb):
                            cols = slice(cb * 512, (cb + 1) * 512)
                            SQ, RS, pN = sq[cb % 2], rs[cb % 2], pN_[cb % 2]
                            s.op("act", lambda e: e.activation(out=SQ[:], in_=raw[:, cols], func=AF.Square),
                                 reads=[raw], writes=[SQ])
                            s.op("pe", lambda e: e.matmul(pN[0:64, :], lhsT=ones_r[0:64, 0:64], rhs=SQ[:], start=True,
                                                          stop=True), reads=[ones_r, SQ], writes=[pN])
                            s.op("act", lambda e: e.activation(out=RS[:], in_=pN[0:64, :], func=AF.Ln, scale=1.0 / 64,
                                                               bias=eps_t[0:64, 0:1]), reads=[pN, eps_t], writes=[RS])
                            s.op("act", lambda e: e.activation(out=RS[:], in_=RS[:], func=AF.Exp, scale=-0.5),
                                 reads=[RS], writes=[RS])
                            s.op("dve", lambda e: e.scalar_tensor_tensor(out=nrm[:, cols], in0=raw[:, cols],
                                                                         scalar=gqk[:, gc:gc + 1], in1=RS[:],
                                                                         op0=ALU.mult, op1=ALU.mult),
                                 reads=[raw, gqk, RS], writes=[nrm])
                        steps.append(step)
                return steps

            load(0)
            for st in qknorm_steps(0):
                st()
            for h in range(8):
                if h + 1 < 8:
                    load(h + 1)
                    pending = qknorm_steps(h + 1)
                else:
                    pending = []
                QN, KN, VD = qn[h % 2], kn[h % 2], Vd[h % 2]
                blocks = []
                for bi, d in enumerate((1, 4, 16)):
                    nb = 32 // d
                    for r in range(d):
                        for n in range(nb):
                            blocks.append((bi, d, nb, r, n))

                def stage1(i):
                    bi, d, nb, r, n = blocks[i]
                    qv = QN[:].rearrange("p (n i r) -> p r n i", i=128, r=d)
                    kv = KN[:].rearrange("p (n i r) -> p r n i", i=128, r=d)
                    pS, PM_ = pS_[i % NPS], pm_[i % NBUF]
                    w = 256 if n + 1 < nb else 128
                    nq = w // 128
                    s.op("pe", lambda e: e.matmul(pS[:, 0:w], lhsT=kv[:, r, n, :], rhs=qv[:, r, n:n + nq, :], start=True,
                                                  stop=False), reads=[KN, QN], writes=[pS])
                    s.op("pe", lambda e: e.matmul(pS[:, 0:w], lhsT=identb[:], rhs=mbias[:, 0:w], start=False, stop=True),
                         reads=[identb, mbias], writes=[pS])
                    s.op("act", lambda e: e.activation(out=PM_[:, 0:w], in_=pS[:, 0:w], func=AF.Exp, scale=0.125),
                         reads=[pS], writes=[PM_])

                def stage2(i):
                    bi, d, nb, r, n = blocks[i]
                    av = acc[:].rearrange("p (n i r) -> p r n i", i=128, r=d)
                    pO, PMc, PMp = pO_[i % NPS], pm_[i % NBUF], pm_[(i - 1) % NBUF]
                    V = VD[bi]
                    blk = r * nb + n
                    if n > 0:
                        s.op("pe", lambda e: e.matmul(pO[:, 0:128], lhsT=V[:, blk - 1, :], rhs=PMp[:, 128:256], start=True,
                                                      stop=False), reads=[V, PMp], writes=[pO])
                        s.op("pe", lambda e: e.matmul(pO[:, 0:128], lhsT=V[:, blk, :], rhs=PMc[:, 0:128], start=False,
                                                      stop=True), reads=[V, PMc], writes=[pO])
                    else:
                        s.op("pe", lambda e: e.matmul(pO[:, 0:128], lhsT=V[:, blk, :], rhs=PMc[:, 0:128], start=True,
                                                      stop=True), reads=[V, PMc], writes=[pO])
                    dst = av[:, r, n, :]
                    if bi == 0:
                        s.op("dve", lambda e: e.tensor_copy(out=dst, in_=pO[:, 0:128]), reads=[pO], writes=[acc])
                    else:
                        s.op("dve", lambda e: e.tensor_tensor(out=dst, in0=dst, in1=pO[:, 0:128], op=ALU.add),
                             reads=[pO, acc], writes=[acc])

                nblk = len(blocks)
                for i in range(nblk + LA):
                    if i < nblk:
                        stage1(i)
                    if i - LA >= 0:
                        stage2(i - LA)
                    if pending and i % 6 == 5:
                        pending.pop(0)()
                while pending:
                    pending.pop(0)()
                s.op("act", lambda e: e.activation(out=acc[64:128, :], in_=acc[64:128, :], func=AF.Ln), reads=[acc],
                     writes=[acc])
                s.op("act", lambda e: e.activation(out=acc[64:128, :], in_=acc[64:128, :], func=AF.Exp, scale=-1.0),
                     reads=[acc], writes=[acc])
                s.op("dve", lambda e: e.tensor_copy(out=dsh[:], in_=acc[64:128, :]), reads=[acc], writes=[dsh])
                s.op("dve", lambda e: e.tensor_tensor(out=dsh[:], in0=acc[0:64, :], in1=dsh[:], op=ALU.mult),
                     reads=[acc, dsh], writes=[dsh])
                s.dma("sp", ymixT[512 + h * 64:512 + (h + 1) * 64, :], dsh[:], reads=[dsh])
        s.barrier()

    def phase_N(l):
        with ExitStack() as es:
            gg = tile(es, "gg", [128, 2, 4])
            ya = [tile(es, "ya%d" % i, [128, 4, 512]) for i in range(2)]
            sq4 = [tile(es, "sq4%d" % i, [128, 4, 512], F32R) for i in range(2)]
            rsn = [tile(es, "rsn%d" % i, [128, 512]) for i in range(2)]
            mo = [tile(es, "mo%d" % i, [128, 4, 512]) for i in range(2)]
            s.dma("sp", gg[:, 0, :], norm_out_lru[l].rearrange("(j p) -> p j", p=128), writes=[gg],
                  allow_slow_non_contiguous=True)
            s.dma("sp", gg[:, 1, :], norm_out_attn[l].rearrange("(j p) -> p j", p=128), writes=[gg],
                  allow_slow_non_contiguous=True)
            it_ = 0
            for half in range(2):
                for cb in range(8):
                    cols = slice(cb * 512, (cb + 1) * 512)
                    YA, SQ, RS, MO, pS = ya[it_ % 2], sq4[it_ % 2], rsn[it_ % 2], mo[it_ % 2], banks[it_ % 2]
                    it_ += 1
                    s.dma("sp", YA[:], ymixT[half * 512:(half + 1) * 512, cols].rearrange("(j p) c -> p j c", p=128),
                          writes=[YA])
                    for jj in range(4):
                        s.op("act", lambda e, YA=YA, SQ=SQ, jj=jj: e.activation(out=SQ[:, jj, :], in_=YA[:, jj, :],
                                                                                func=AF.Square),
                             reads=[YA], writes=[SQ])
                    for jj in range(4):
                        s.op("pe", lambda e, SQ=SQ, pS=pS, jj=jj: e.matmul(pS[:, :], lhsT=ones_r[:, :], rhs=SQ[:, jj, :],
                                                                           start=(jj == 0), stop=(jj == 3)),
                             reads=[ones_r, SQ], writes=[pS])
                    s.op("dve", lambda e, RS=RS, pS=pS: e.tensor_scalar(out=RS[:], in0=pS[:, :], scalar1=1.0 / 512,
                                                                        scalar2=EPS, op0=ALU.mult, op1=ALU.add),
                         reads=[pS], writes=[RS])
                    s.op("act", lambda e, RS=RS: e.activation(out=RS[:], in_=RS[:], func=AF.Sqrt), reads=[RS], writes=[RS])
                    s.op("dve", lambda e, RS=RS: e.reciprocal(out=RS[:], in_=RS[:]), reads=[RS], writes=[RS])
                    for jj in range(4):
                        s.op("dve", lambda e, YA=YA, MO=MO, RS=RS, jj=jj, half=half: e.scalar_tensor_tensor(
                            out=MO[:, jj, :], in0=YA[:, jj, :], scalar=gg[:, half, jj:jj + 1], in1=RS[:], op0=ALU.mult,
                            op1=ALU.mult), reads=[YA, gg, RS], writes=[MO])
                    s.dma("sp", mergedT[half * 512:(half + 1) * 512, cols].rearrange("(j p) c -> p j c", p=128), MO[:],
                          reads=[MO])
        s.barrier()

    def phase_D(l, xsrc):
        with ExitStack() as es:
            wout = tile(es, "wout", [128, 8, D], F32R)
            gbc = tile(es, "gbc2", [128, D])
            wr = tile(es, "wr", [128, 8, 36])
            bbc = tile(es, "bbc", [128, 36])
            xin = [tile(es, "xd%d" % i, [128, 4, D]) for i in range(2)]
            mT = [tile(es, "mT%d" % i, [128, 8, 512], F32R) for i in range(2)]
            ym = [tile(es, "ym%d" % i, [128, 8, 512]) for i in range(2)]
            sq4 = [tile(es, "sq4%d" % i, [128, 4, 512], F32R) for i in range(1)] * 2
            rsn = [tile(es, "rsn%d" % i, [128, 512]) for i in range(1)] * 2
            gg = tile(es, "gg", [128, 8])
            h2b = [tile(es, "h2_%d" % i, [128, 4, D]) for i in range(2)]
            h2t = [tile(es, "h2t%d" % i, [128, 8, 512]) for i in range(1)] * 2
            s.dma("sp", gg[:, 0:4], norm_out_lru[l].rearrange("(j p) -> p j", p=128), writes=[gg],
                  allow_slow_non_contiguous=True)
            s.dma("sp", gg[:, 4:8], norm_out_attn[l].rearrange("(j p) -> p j", p=128), writes=[gg],
                  allow_slow_non_contiguous=True)
            junk = None
            ss = [tile(es, "ssd%d" % i, [128, 4]) for i in range(2)]
            rt_ = [tile(es, "rtd%d" % i, [128, 96]) for i in range(2)]
            LGS = tile(es, "LGS", [128, 32, 4])
            DD = tile(es, "DD", [128, 32])
            PT = tile(es, "PT", [128, 32])
            wsrc = w_out[l].rearrange("(k p) n -> p k n", p=128)
            for c2 in range(2):
                s.dma("pool", wout[:, :, c2 * 512:(c2 + 1) * 512], wsrc[:, :, c2 * 512:(c2 + 1) * 512], writes=[wout])
            s.dma("pool", gbc[:], norm_ffn[l].partition_broadcast(128), writes=[gbc])
            s.dma("sp", wr[:, :, 0:4], router_group_w[l].rearrange("(k p) n -> p k n", p=128), writes=[wr])
            s.dma("sp", wr[:, :, 4:36], router_expert_w[l].rearrange("(k p) n -> p k n", p=128), writes=[wr])
            s.dma("pool", bbc[:, 0:4], router_group_b[l].partition_broadcast(128), writes=[bbc])
            s.dma("pool", bbc[:, 4:36], router_expert_b[l].partition_broadcast(128), writes=[bbc])

            def load(c):
                cols = slice(c * 512, (c + 1) * 512)
                s.dma("sp", ym[c % 2][:], ymixT[:, cols].rearrange("(k p) c -> p k c", p=128), writes=[ym[c % 2]])
                s.dma("sp", xin[c % 2][:], xsrc[c * 512:(c + 1) * 512, :].rearrange("(j p) d -> p j d", p=128),
                      writes=[xin[c % 2]])

            nrm_i = [0]

            def mixnorm(c):
                YM, MT = ym[c % 2], mT[c % 2]
                for half in range(2):
                    i_ = nrm_i[0]
                    nrm_i[0] += 1
                    SQ, RS, pS = sq4[i_ % 2], rsn[i_ % 2], banks[i_ % 2]
                    for jj in range(4):
                        s.op("act", lambda e, jj=jj: e.activation(out=SQ[:, jj, :], in_=YM[:, half * 4 + jj, :],
                                                                  func=AF.Square), reads=[YM], writes=[SQ])
                    for jj in range(4):
                        s.op("pe", lambda e, jj=jj: e.matmul(pS[:, :], lhsT=ones_r[:, :], rhs=SQ[:, jj, :], start=(jj == 0),
                                                             stop=(jj == 3)), reads=[ones_r, SQ], writes=[pS])
                    s.op("act", lambda e: e.activation(out=RS[:], in_=pS[:, :], func=AF.Ln, scale=1.0 / 512,
                                                       bias=eps_t[:, 0:1]), reads=[pS, eps_t], writes=[RS])
                    s.op("act", lambda e: e.activation(out=RS[:], in_=RS[:], func=AF.Exp, scale=-0.5), reads=[RS],
                         writes=[RS])
                    for jj in range(4):
                        k_ = half * 4 + jj
                        s.op("dve", lambda e, k_=k_: e.scalar_tensor_tensor(out=MT[:, k_, :], in0=YM[:, k_, :],
                                                                            scalar=gg[:, k_:k_ + 1], in1=RS[:],
                                                                            op0=ALU.mult, op1=ALU.mult),
                             reads=[YM, gg, RS], writes=[MT])

            pi = [0]

            def MM(c):
                X, MT = xin[c % 2], mT[c % 2]
                for j in range(4):
                    for hf in range(2):
                        pY = banks[2 + pi[0] % 4]
                        pi[0] += 1
                        hc = slice(hf * 512, (hf + 1) * 512)
                        for k in range(8):
                            s.op("pe", lambda e, pY=pY, k=k, j=j, hc=hc: e.matmul(
                                pY[:, :], lhsT=MT[:, k, j * 128:(j + 1) * 128], rhs=wout[:, k, hc], start=(k == 0),
                                stop=(k == 7)), reads=[MT, wout], writes=[pY])
                        s.op("dve", lambda e, pY=pY, j=j, hc=hc: e.tensor_tensor(out=X[:, j, hc], in0=pY[:, :],
                                                                                in1=X[:, j, hc], op=ALU.add),
                             reads=[pY, X], writes=[X])
                s.dma("pool", xmid[c * 512:(c + 1) * 512, :].rearrange("(j p) d -> p j d", p=128), X[:], reads=[X])

            def RMS(c):
                rms_token_major(xin[c % 2], h2b[c % 2], ss[c % 2], junk, gbc)

            def TR(c):
                H2T = h2t[c % 2]
                h2 = h2b[c % 2]
                transpose_chunk(h2, H2T, banks[0:2])
                if MOE_SPARSE:
                    s.dma("pool", h2tok[c * 512:(c + 1) * 512, :].rearrange("(j p) d -> p j d", p=128), h2[:], reads=[h2])
                else:
                    s.dma("pool", h2T[:, c * 512:(c + 1) * 512].rearrange("(k p) c -> p k c", p=128), H2T[:], reads=[H2T])
                router(c, H2T)

            def router(c, H2T):
                for j in range(4):
                    R, pR = rt_[j % 2], banks[6 + j % 2]
                    ti = c * 4 + j
                    for k in range(8):
                        s.op("pe", lambda e, pR=pR, k=k, j=j: e.matmul(
                            pR[:, 0:36], lhsT=H2T[:, k, j * 128:(j + 1) * 128], rhs=wr[:, k, :], start=(k == 0),
                            stop=(k == 7)), reads=[H2T, wr], writes=[pR])
                    lg, gmax, oh, pen, m8 = R[:, 0:36], R[:, 36:37], R[:, 40:44], R[:, 44:48], R[:, 52:60]

                    def dv(fn, R=R, er=(), ew=()):
                        s.op("dve", fn, reads=[R] + list(er), writes=[R] + list(ew))

                    s.op("dve", lambda e, lg=lg, pR=pR: e.tensor_tensor(out=lg, in0=pR[:, 0:36], in1=bbc[:], op=ALU.add),
                         reads=[pR, bbc], writes=[R])
                    dv(lambda e, lg=lg, gmax=gmax: e.tensor_reduce(out=gmax, in_=lg[:, 0:4], axis=mybir.AxisListType.X,
                                                                   op=ALU.max))
                    dv(lambda e, lg=lg, gmax=gmax, ti=ti: e.tensor_scalar(out=LGS[:, ti, :], in0=lg[:, 0:4], scalar1=gmax,
                                                                          scalar2=None, op0=ALU.subtract), ew=[LGS])
                    dv(lambda e, lg=lg, gmax=gmax, oh=oh: e.tensor_scalar(out=oh, in0=lg[:, 0:4], scalar1=gmax, scalar2=None,
                                                                          op0=ALU.is_equal))
                    dv(lambda e, oh=oh, pen=pen: e.tensor_scalar(out=pen, in0=oh, scalar1=-1.0, scalar2=1.0e30,
                                                                 op0=ALU.add, op1=ALU.mult))
                    for g in range(4):
                        dv(lambda e, lg=lg, pen=pen, g=g: e.tensor_scalar(
                            out=lg[:, 4 + g * 8:12 + g * 8], in0=lg[:, 4 + g * 8:12 + g * 8], scalar1=pen[:, g:g + 1],
                            scalar2=None, op0=ALU.add))
                    dv(lambda e, m8=m8, lg=lg: e.max(out=m8, in_=lg[:, 4:36]))
                    dv(lambda e, m8=m8, ti=ti: e.tensor_tensor(out=DD[:, ti:ti + 1], in0=m8[:, 1:2], in1=m8[:, 0:1],
                                                               op=ALU.subtract), ew=[DD])
                    dv(lambda e, lg=lg, m8=m8, ti=ti: e.tensor_scalar(out=OH1[:, ti, :], in0=lg[:, 4:36], scalar1=m8[:, 0:1],
                                                                      scalar2=None, op0=ALU.is_equal), ew=[OH1])
                    dv(lambda e, lg=lg, m8=m8, ti=ti: e.tensor_scalar(out=OH2[:, ti, :], in0=lg[:, 4:36], scalar1=m8[:, 1:2],
                                                                      scalar2=None, op0=ALU.is_equal), ew=[OH2])

            def gate_weights():
                fl = "p a b -> p (a b)"
                s.op("act", lambda e: e.activation(out=LGS[:].rearrange(fl), in_=LGS[:].rearrange(fl), func=AF.Exp),
                     reads=[LGS], writes=[LGS])
                s.op("dve", lambda e: e.tensor_reduce(out=PT[:], in_=LGS[:], axis=mybir.AxisListType.X, op=ALU.add),
                     reads=[LGS], writes=[PT])
                s.op("dve", lambda e: e.reciprocal(out=PT[:], in_=PT[:]), reads=[PT], writes=[PT])
                s.op("act", lambda e: e.activation(out=DD[:], in_=DD[:], func=AF.Exp), reads=[DD], writes=[DD])
                s.op("dve", lambda e: e.tensor_scalar(out=DD[:], in0=DD[:], scalar1=1.0, scalar2=None, op0=ALU.add),
                     reads=[DD], writes=[DD])
                s.op("dve", lambda e: e.reciprocal(out=DD[:], in_=DD[:]), reads=[DD], writes=[DD])
                s.op("dve", lambda e: e.tensor_tensor(out=W12[:, :, 0], in0=DD[:], in1=PT[:], op=ALU.mult),
                     reads=[DD, PT], writes=[W12])
                s.op("dve", lambda e: e.tensor_tensor(out=W12[:, :, 1], in0=PT[:], in1=W12[:, :, 0], op=ALU.subtract),
                     reads=[PT, W12], writes=[W12])

            load(0)
            mixnorm(0)
            if 1 < 8:
                load(1)
                mixnorm(1)
            MM(0)
            RMS(0)
            for c in range(8):
                if c + 2 < 8:
                    load(c + 2)
                    mixnorm(c + 2)
                if c + 1 < 8:
                    MM(c + 1)
                    RMS(c + 1)
                TR(c)
            gate_weights()
        s.barrier()

    def phase_E(l, xdst):
        T = 1024
        with ExitStack() as es:
            hsc = tile(es, "hsc", [128, 8, T], F32R)
            yacc = tile(es, "yacc", [128, T // 128, D])
            wg = [tile(es, "wg%d" % i, [128, 8, 512], F32R) for i in range(2)]
            wu = [tile(es, "wu%d" % i, [128, 8, 512], F32R) for i in range(2)]
            wd = [tile(es, "wd%d" % i, [128, 4, D], F32R) for i in range(2)]
            hid = [tile(es, "hid%d" % i, [128, 4, 512], F32R) for i in range(2)]
            sg = [tile(es, "sg%d" % i, [128, 512]) for i in range(2)]
            wi = 0
            gi = 0
            hi = 0
            for sc in range(S // T):
                t0 = sc * T
                s.dma("pool", hsc[:], h2T[:, t0:t0 + T].rearrange("(k p) c -> p k c", p=128), writes=[hsc])
                s.dma("sp", yacc[:], xmid[t0:t0 + T, :].rearrange("(j p) d -> p j d", p=128), writes=[yacc])
                for e_ in range(NE):
                    WG, WU, WD = wg[wi % 2], wu[wi % 2], wd[wi % 2]
                    wi += 1
                    s.dma("pool", WG[:], w_gate[l, e_].rearrange("(k p) n -> p k n", p=128), writes=[WG])
                    s.dma("pool", WU[:], w_up[l, e_].rearrange("(k p) n -> p k n", p=128), writes=[WU])
                    s.dma("pool", WD[:], w_down[l, e_].rearrange("(m p) n -> p m n", p=128), writes=[WD])
                    for sb in range(T // 512):
                        cols = slice(sb * 512, (sb + 1) * 512)
                        HID = hid[hi % 2]
                        hi += 1
                        for m in range(4):
                            pG, pU, SG = banks[gi % 2], banks[2 + gi % 2], sg[gi % 2]
                            gi += 1
                            for k in range(8):
                                s.op("pe", lambda e, pG=pG, WG=WG, k=k, m=m, cols=cols: e.matmul(
                                    pG[:, :], lhsT=WG[:, k, m * 128:(m + 1) * 128], rhs=hsc[:, k, cols], start=(k == 0),
                                    stop=(k == 7)), reads=[WG, hsc], writes=[pG])
                            for k in range(8):
                                s.op("pe", lambda e, pU=pU, WU=WU, k=k, m=m, cols=cols: e.matmul(
                                    pU[:, :], lhsT=WU[:, k, m * 128:(m + 1) * 128], rhs=hsc[:, k, cols], start=(k == 0),
                                    stop=(k == 7)), reads=[WU, hsc], writes=[pU])
                            s.op("act", lambda e, SG=SG, pG=pG: e.activation(out=SG[:], in_=pG[:, :], func=AF.Silu),
                                 reads=[pG], writes=[SG])
                            s.op("dve", lambda e, HID=HID, SG=SG, pU=pU, m=m: e.tensor_tensor(
                                out=HID[:, m, :], in0=SG[:], in1=pU[:, :], op=ALU.mult), reads=[SG, pU], writes=[HID])
                        for j in range(4):
                            tl = sb * 4 + j
                            tg = sc * (T // 128) + tl
                            for hf in range(2):
                                pY = banks[4 + (j * 2 + hf) % 4]
                                hc = slice(hf * 512, (hf + 1) * 512)
                                for m in range(4):
                                    s.op("pe", lambda e, pY=pY, HID=HID, WD=WD, m=m, j=j, hc=hc: e.matmul(
                                        pY[:, :], lhsT=HID[:, m, j * 128:(j + 1) * 128], rhs=WD[:, m, hc], start=(m == 0),
                                        stop=(m == 3)), reads=[HID, WD], writes=[pY])
                                s.op("dve", lambda e, pY=pY, tl=tl, tg=tg, hc=hc, e_=e_: e.scalar_tensor_tensor(
                                    out=yacc[:, tl, hc], in0=pY[:, :], scalar=Gall[:, tg, e_:e_ + 1], in1=yacc[:, tl, hc],
                                    op0=ALU.mult, op1=ALU.add), reads=[pY, Gall, yacc], writes=[yacc])
                s.dma("sp", xdst[t0:t0 + T, :].rearrange("(j p) d -> p j d", p=128), yacc[:], reads=[yacc])
        s.barrier()

    IOA = bass.IndirectOffsetOnAxis

    def phase_R(l):
        with ExitStack() as es:
            Ab = tile(es, "Ab", [128, 1024], BF16)
            RK = tile(es, "RK", [128, 32, NE])
            CNT = tile(es, "CNT", [128, 32, NE])
            INC = tile(es, "INC", [128, 32, NE])
            TMP = tile(es, "TMP", [128, 32, NE])
            ones32 = tile(es, "ones32", [128, 32])
            sm = tile(es, "sm", [128, 8, 32])
            smi = tile(es, "smi", [128, 32], I32)
            SF = tile(es, "SF", [128, 2, 32])
            EB = tile(es, "EB", [128, NSB])
            EBX = tile(es, "EBX", [128, NSB])
            IDXf = tile(es, "IDXf", [128, NSB, 2])
            TOT, PC, PEND, BASE, YY, NBf, junk = (sm[:, i, :] for i in range(7))
            fl = "p a b -> p (a b)"
            s.op("dve", lambda e: e.memset(ones32[:], 1.0), writes=[ones32])
            s.op("dve", lambda e: e.tensor_tensor(out=Ab[:], in0=OH1[:].rearrange(fl), in1=OH2[:].rearrange(fl), op=ALU.add),
                 reads=[OH1, OH2], writes=[Ab])
            for c in range(2):
                cs = slice(c * 512, (c + 1) * 512)
                s.op("pe", lambda e, c=c, cs=cs: e.matmul(banks[c][:, :], lhsT=Ltb[:], rhs=Ab[:, cs], start=True, stop=True),
                     reads=[Ltb, Ab], writes=[banks[c]])
                evac(c, RK[:].rearrange(fl)[:, cs], banks[c][:, :], [banks[c]], [RK])
                s.op("pe", lambda e, c=c, cs=cs: e.matmul(banks[2 + c][:, :], lhsT=ones_b[:], rhs=Ab[:, cs], start=True,
                                                          stop=True), reads=[ones_b, Ab], writes=[banks[2 + c]])
                evac(c + 1, CNT[:].rearrange(fl)[:, cs], banks[2 + c][:, :], [banks[2 + c]], [CNT])
            for e_ in range(NE):
                s.op("dve", lambda e, e_=e_: e.tensor_tensor_scan(out=INC[:, :, e_], data0=ones32[:], data1=CNT[:, :, e_],
                                                                  initial=0.0, op0=ALU.mult, op1=ALU.add),
                     reads=[ones32, CNT], writes=[INC])
            s.op("dve", lambda e: e.tensor_copy(out=TOT, in_=INC[:, 31, :]), reads=[INC], writes=[sm])
            s.op("dve", lambda e: e.tensor_scalar(out=YY, in0=TOT, scalar1=1.0 / 512, scalar2=511.0 / 512 - 0.4990234375,
                                                  op0=ALU.mult, op1=ALU.add), reads=[sm], writes=[sm])
            s.op("dve", lambda e: e.tensor_copy(out=smi[:], in_=YY), reads=[sm], writes=[smi])
            s.op("dve", lambda e: e.tensor_copy(out=NBf, in_=smi[:]), reads=[smi], writes=[sm])
            s.op("dve", lambda e: e.tensor_scalar(out=PC, in0=NBf, scalar1=512.0, scalar2=None, op0=ALU.mult),
                 reads=[sm], writes=[sm])
            s.op("dve", lambda e: e.tensor_tensor_scan(out=PEND, data0=ones32[:], data1=PC, initial=0.0, op0=ALU.mult,
                                                       op1=ALU.add), reads=[sm, ones32], writes=[sm])
            s.op("dve", lambda e: e.tensor_tensor(out=BASE, in0=PEND, in1=PC, op=ALU.subtract), reads=[sm], writes=[sm])
            s.op("dve", lambda e: e.tensor_tensor(out=RK[:], in0=RK[:], in1=INC[:], op=ALU.add), reads=[RK, INC], writes=[RK])
            s.op("dve", lambda e: e.tensor_tensor(out=RK[:], in0=RK[:], in1=CNT[:], op=ALU.subtract), reads=[RK, CNT],
                 writes=[RK])
            for t in range(32):
                s.op("dve", lambda e, t=t: e.tensor_tensor(out=RK[:, t, :], in0=RK[:, t, :], in1=BASE, op=ALU.add),
                     reads=[RK, sm], writes=[RK])
            for (OH, k, Si) in ((OH1, 0, S1i), (OH2, 1, S2i)):
                s.op("dve", lambda e, OH=OH: e.tensor_tensor(out=TMP[:], in0=OH[:], in1=RK[:], op=ALU.mult),
                     reads=[OH, RK], writes=[TMP])
                s.op("dve", lambda e, k=k: e.tensor_reduce(out=SF[:, k, :], in_=TMP[:], axis=mybir.AxisListType.X,
                                                           op=ALU.add), reads=[TMP], writes=[SF])
                s.op("dve", lambda e, k=k, Si=Si: e.tensor_copy(out=Si[:], in_=SF[:, k, :]), reads=[SF], writes=[Si])
            for sb in range(NSB):
                s.op("dve", lambda e, sb=sb: e.tensor_scalar(out=junk, in0=PEND, scalar1=512.0 * sb, scalar2=None,
                                                             op0=ALU.is_le, op1=ALU.add, accum_out=EB[:, sb:sb + 1]),
                     reads=[sm], writes=[sm, EB])
            s.op("dve", lambda e: e.tensor_scalar(out=EBX[:], in0=EB[:], scalar1=float(NE) - 0.5, scalar2=1.0e4,
                                                  op0=ALU.is_gt, op1=ALU.mult), reads=[EB], writes=[EBX])
            s.op("dve", lambda e: e.scalar_tensor_tensor(out=EB[:], in0=EB[:], scalar=float(NE * l), in1=EBX[:],
                                                         op0=ALU.add, op1=ALU.add), reads=[EB, EBX], writes=[EB])
            for k2 in range(2):
                s.op("dve", lambda e, k2=k2: e.tensor_scalar(out=IDXf[:, :, k2], in0=EB[:], scalar1=256.0,
                                                             scalar2=pio2[:, k2:k2 + 1], op0=ALU.mult, op1=ALU.add),
                     reads=[EB, pio2], writes=[IDXf])
            s.op("dve", lambda e: e.tensor_copy(out=IDXWi[:], in_=IDXf[:]), reads=[IDXf], writes=[IDXWi])
        s.barrier()

    def phase_S():
        with ExitStack() as es:
            ht = [tile(es, "ht%d" % i, [128, D]) for i in range(2)]
            for t in range(32):
                H = ht[t % 2]
                s.dma("sp", H[:], h2tok[t * 128:(t + 1) * 128, :], writes=[H])
                s.indirect(xbuf[:, :], IOA(ap=S1i[:, t:t + 1], axis=0), H[:], None, reads=[H, S1i])
                s.indirect(xbuf[:, :], IOA(ap=S2i[:, t:t + 1], axis=0), H[:], None, reads=[H, S2i])
        s.barrier()

    def phase_E2(l):
        wgt = w_gate.rearrange("l e (p k2 k4) n -> (l e p k2) (k4 n)", p=128, k2=2)
        wut = w_up.rearrange("l e (p k2 k4) n -> (l e p k2) (k4 n)", p=128, k2=2)
        wdt = w_down.rearrange("l e (p m2 m4) n -> (l e p m2) (m4 n)", p=128, m2=2)
        with ExitStack() as es:
            wg = [tile(es, "wg%d" % i, [128, 8, 512], F32R) for i in range(2)]
            wu = [tile(es, "wu%d" % i, [128, 8, 512], F32R) for i in range(2)]
            wd = [tile(es, "wd%d" % i, [128, 4, D], F32R) for i in range(2)]
            xb = [tile(es, "xb%d" % i, [128, 4, D]) for i in range(2)]
            xbT = [tile(es, "xbT%d" % i, [128, 8, 512], F32R) for i in range(2)]
            hid = [tile(es, "hid%d" % i, [128, 4, 512], F32R) for i in range(1)] * 2
            sg = [tile(es, "sg%d" % i, [128, 512]) for i in range(2)]
            yb = [tile(es, "yb%d" % i, [128, 4, D]) for i in range(1)] * 2
            fl = "p a b -> p (a b)"
            gi = 0

            def loadw(sb):
                if _DBG_SKIP_W and sb >= 2:
                    return
                WG, WU, WD = wg[sb % 2], wu[sb % 2], wd[sb % 2]
                for (W, tab) in ((WG, wgt), (WU, wut), (WD, wdt)):
                    Wf = W[:].rearrange(fl)
                    for k2 in range(2):
                        s.indirect(Wf[:, k2 * 2048:(k2 + 1) * 2048], None, tab[:, :],
                                   IOA(ap=IDXWi[:, sb, k2:k2 + 1], axis=0), reads=[IDXWi], writes=[W],
                                   bounds=DEPTH * NE * 256 - 1)

            def loadx(sb):
                s.dma("sp", xb[sb % 2][:], xbuf[sb * 512:(sb + 1) * 512, :].rearrange("(j p) d -> p j d", p=128),
                      writes=[xb[sb % 2]])

            def transposes(sb):
                XB, XT = xb[sb % 2], xbT[sb % 2]
                for k in range(8):
                    pT = banks[k % 2]
                    for j in range(4):
                        s.op("pe", lambda e, pT=pT, j=j, k=k: e.transpose(pT[:, j * 128:(j + 1) * 128], XB[:, j, k::8],
                                                                          ident[:]), reads=[XB, ident], writes=[pT])
                    evac(k, XT[:, k, :], pT[:, :], [pT], [XT])

            loadw(0)
            loadx(0)
            transposes(0)
            for sb in range(NSB):
                if sb + 1 < NSB:
                    loadw(sb + 1)
                    loadx(sb + 1)
                WG, WU, WD, XT, HID, YB = wg[sb % 2], wu[sb % 2], wd[sb % 2], xbT[sb % 2], hid[sb % 2], yb[sb % 2]
                for m in range(4):
                    pG, pU, SG = banks[2 + gi % 2], banks[4 + gi % 2], sg[gi % 2]
                    gi += 1
                    for k in range(8):
                        s.op("pe", lambda e, pG=pG, k=k, m=m: e.matmul(pG[:, :], lhsT=WG[:, k, m::4], rhs=XT[:, k, :],
                                                                       start=(k == 0), stop=(k == 7)),
                             reads=[WG, XT], writes=[pG])
                    for k in range(8):
                        s.op("pe", lambda e, pU=pU, k=k, m=m: e.matmul(pU[:, :], lhsT=WU[:, k, m::4], rhs=XT[:, k, :],
                                                                       start=(k == 0), stop=(k == 7)),
                             reads=[WU, XT], writes=[pU])
                    s.op("act", lambda e, SG=SG, pG=pG: e.activation(out=SG[:], in_=pG[:, :], func=AF.Silu),
                         reads=[pG], writes=[SG])
                    s.op("dve", lambda e, SG=SG, pU=pU, m=m: e.tensor_tensor(out=HID[:, m, :], in0=SG[:], in1=pU[:, :],
                                                                             op=ALU.mult), reads=[SG, pU], writes=[HID])
                if sb + 1 < NSB:
                    transposes(sb + 1)
                ev = 0
                for j in range(4):
                    for hf in range(2):
                        pY = banks[6 + (j * 2 + hf) % 2]
                        hc = slice(hf * 512, (hf + 1) * 512)
                        for m in range(4):
                            s.op("pe", lambda e, pY=pY, m=m, j=j, hc=hc: e.matmul(
                                pY[:, :], lhsT=HID[:, m, j * 128:(j + 1) * 128], rhs=WD[:, m, hc], start=(m == 0),
                                stop=(m == 3)), reads=[HID, WD], writes=[pY])
                        evac(ev, YB[:, j, hc], pY[:, :], [pY], [YB])
                        ev += 1
                s.dma("sp", ybuf[sb * 512:(sb + 1) * 512, :].rearrange("(j p) d -> p j d", p=128), YB[:], reads=[YB])
        s.barrier()

    def phase_G(xdst):
        with ExitStack() as es:
            xt = [tile(es, "xg%d" % i, [128, D]) for i in range(2)]
            y1 = [tile(es, "y1%d" % i, [128, D]) for i in range(2)]
            y2 = [tile(es, "y2%d" % i, [128, D]) for i in range(2)]
            for t in range(32):
                X, Y1, Y2 = xt[t % 2], y1[t % 2], y2[t % 2]
                s.dma("sp", X[:], xmid[t * 128:(t + 1) * 128, :], writes=[X])
                s.indirect(Y1[:], None, ybuf[:, :], IOA(ap=S1i[:, t:t + 1], axis=0), reads=[S1i], writes=[Y1])
                s.indirect(Y2[:], None, ybuf[:, :], IOA(ap=S2i[:, t:t + 1], axis=0), reads=[S2i], writes=[Y2])
                s.op("dve", lambda e, X=X, Y1=Y1, t=t: e.scalar_tensor_tensor(out=X[:], in0=Y1[:], scalar=W12[:, t, 0:1],
                                                                              in1=X[:], op0=ALU.mult, op1=ALU.add),
                     reads=[Y1, W12, X], writes=[X])
                s.op("dve", lambda e, X=X, Y2=Y2, t=t: e.scalar_tensor_tensor(out=X[:], in0=Y2[:], scalar=W12[:, t, 1:2],
                                                                              in1=X[:], op0=ALU.mult, op1=ALU.add),
                     reads=[Y2, W12, X], writes=[X])
                s.dma("sp", xdst[t * 128:(t + 1) * 128, :], X[:], reads=[X])
        s.barrier()

    def zero_xbuf():
        with ExitStack() as es:
            zt = tile(es, "zt", [128, 8, D])
            s.op("dve", lambda e: e.memset(zt[:], 0.0), writes=[zt])
            for i in range(NSB * 512 // 1024):
                s.dma("sp", xbuf[i * 1024:(i + 1) * 1024, :].rearrange("(j p) d -> p j d", p=128), zt[:], reads=[zt])
        s.barrier()

    xcur = x_in
    for l in range(n_layers):
        if MOE_SPARSE and l > 0:
            phase_A(l, xmid, fuse_g=True, xstore=xres[l % 2])
            xcur = xres[l % 2]
        else:
            phase_A(l, xcur)
        if stop_after == "A":
            break
        phase_B(l)
        if stop_after == "B":
            break
        phase_C(l)
        if stop_after == "C":
            break
        phase_D(l, xcur)
        if stop_after == "D":
            break
        xnext = out if l == n_layers - 1 else xres[l % 2]
        if MOE_SPARSE:
            phase_R(l)
            phase_S()
            phase_E2(l)
            if l == n_layers - 1:
                phase_G(xnext)
        else:
            phase_E(l, xnext)
        xcur = xnext
    s.barrier()
    gstack.close()
    return nc, s


_CONSTS = None


def _consts():
    global _CONSTS
    if _CONSTS is None:
        j = np.arange(128)[:, None]
        i = np.arange(128)[None, :]
        mask = np.concatenate([(i <= j), (i >= j)], axis=1).astype(np.float32)
        lt = (j < i).astype(np.float32)
        pio2 = (2 * np.arange(128)[:, None] + np.arange(2)[None, :]).astype(np.float32)
        _CONSTS = {"c_ident": np.eye(128, dtype=np.float32), "c_mask": np.ascontiguousarray(mask),
                   "c_lt": np.ascontiguousarray(lt), "c_pio2": np.ascontiguousarray(pio2),
                   "c_mbias": np.ascontiguousarray((np.concatenate([mask[:, 128:], mask[:, :128]], axis=1) - 1.0)
                                                   * 30000.0)}
    return _CONSTS


def kernel(**inputs):
    nc, _ = build()
    x = np.ascontiguousarray(inputs["x"], dtype=np.float32)
    shared = {k: np.ascontiguousarray(v, dtype=np.float32) for k, v in inputs.items() if k != "x"}
    shared.update(_consts())
    in_maps = []
    for b in range(8):
        m = dict(shared)
        m["x"] = x[b]
        in_maps.append(m)
    res = run_bass_kernel_spmd(nc, in_maps, core_ids=list(range(8)))
    return np.stack([np.asarray(r["out"], dtype=np.float32) for r in res.results], axis=0)
```

```python
from contextlib import ExitStack

import numpy as np
import concourse.bass as bass
import concourse.mybir as mybir
from concourse.bass_utils import run_bass_kernel_spmd

F32 = mybir.dt.float32
F32R = mybir.dt.float32r
BF16 = mybir.dt.bfloat16
AF = mybir.ActivationFunctionType
ALU = mybir.AluOpType

S = 4096
D = 1024
DEPTH = 4
DIN = 2560
NE = 32
EPS = 1e-6
SAME_ENGINE_SYNC = True
MOE_SPARSE = True
_DBG_SKIP_W = False
NSB = 47
I32 = mybir.dt.int32


class Buf:
    __slots__ = ("ap", "w", "r", "name")

    def __init__(self, ap, name=""):
        self.ap = ap
        self.w = {}
        self.r = {}
        self.name = name

    def __getitem__(self, k):
        return self.ap[k]


def _merge(d, ev):
    for k, v in ev.items():
        if d.get(k, 0) < v:
            d[k] = v


class Sched:
    def __init__(self, nc, n_dma_sems=24):
        self.nc = nc
        self.E = {"pe": nc.tensor, "act": nc.scalar, "dve": nc.vector, "pool": nc.gpsimd, "sp": nc.sync}
        self.csem = {}
        self.ccnt = {}
        for e in ("pe", "act", "dve", "pool"):
            self.csem[e] = nc.alloc_semaphore("c_" + e)
            self.ccnt[e] = 0
        self.nring = n_dma_sems
        self.dsem = [nc.alloc_semaphore("d_%d" % i) for i in range(2 * n_dma_sems)]
        self.dcnt = [0] * (2 * n_dma_sems)
        self.dnext = {False: 0, True: 0}
        self.seen = {e: {} for e in self.E}
        self.n_inst = 0
        self.n_wait = 0
        self.bregs = {}

    def _sem(self, key):
        return self.csem[key[1]] if key[0] == "c" else self.dsem[key[1]]

    def _wait(self, eng, ev):
        seen = self.seen[eng]
        for key, val in ev.items():
            if key[0] == "c" and key[1] == eng:
                if eng == "pe" or not SAME_ENGINE_SYNC:
                    continue
            if seen.get(key, 0) >= val:
                continue
            self.E[eng].wait_ge(self._sem(key), val)
            seen[key] = val
            self.n_wait += 1

    def _deps(self, eng, reads, writes):
        for b in reads:
            self._wait(eng, b.w)
        for b in writes:
            self._wait(eng, b.w)
            self._wait(eng, b.r)

    def _post(self, ev, reads, writes):
        for b in reads:
            _merge(b.r, ev)
        for b in writes:
            _merge(b.w, ev)

    def op(self, eng, fn, reads=(), writes=()):
        self._deps(eng, reads, writes)
        inst = fn(self.E[eng])
        self.ccnt[eng] += 1
        inst.then_inc(self.csem[eng], 1)
        ev = {("c", eng): self.ccnt[eng]}
        self._post(ev, reads, writes)
        self.n_inst += 1
        return ev

    def _ring(self, sw):
        i = self.dnext[sw]
        self.dnext[sw] = (i + 1) % self.nring
        return i + (self.nring if sw else 0)

    def dma(self, eng, out, in_, reads=(), writes=(), **kw):
        i = self._ring(eng == "pool")
        if self.dcnt[i] > 0:
            self._wait(eng, {("d", i): self.dcnt[i]})
        self._deps(eng, reads, writes)
        self.dcnt[i] += 16
        self.E[eng].dma_start(out=out, in_=in_, **kw).then_inc(self.dsem[i], 16)
        ev = {("d", i): self.dcnt[i]}
        self._post(ev, reads, writes)
        self.n_inst += 1
        return ev

    def indirect(self, out, out_off, in_, in_off, reads=(), writes=(), bounds=None):
        i = self._ring(True)
        if self.dcnt[i] > 0:
            self._wait("pool", {("d", i): self.dcnt[i]})
        self._deps("pool", reads, writes)
        self.dcnt[i] += 16
        kw = {}
        if bounds is not None:
            if bounds not in self.bregs:
                self.bregs[bounds] = self.nc.gpsimd.to_reg(bounds)
            kw = dict(bounds_check=self.bregs[bounds], oob_is_err=False)
        self.nc.gpsimd.indirect_dma_start(out=out, out_offset=out_off, in_=in_, in_offset=in_off, **kw).then_inc(
            self.dsem[i], 16)
        ev = {("d", i): self.dcnt[i]}
        self._post(ev, reads, writes)
        self.n_inst += 1
        return ev

    def barrier(self):
        allev = {}
        for e, c in self.ccnt.items():
            if c:
                allev[("c", e)] = c
        for i, c in enumerate(self.dcnt):
            if c:
                allev[("d", i)] = c
        for eng in self.E:
            self._wait(eng, allev)


def build(n_layers=DEPTH, debug=False, stop_after=None):
    nc = bass.Bass("TRN2", target_bir_lowering=False)
    s = Sched(nc)

    def din(name, shape):
        return nc.dram_tensor(name, shape, F32, kind="ExternalInput").ap()

    def dtmp(name, shape, dt=F32):
        return nc.dram_tensor(name, shape, dt, kind=("ExternalOutput" if debug else "Internal")).ap()

    x_in = din("x", [S, D])
    norm_mix = din("norm_mix", [DEPTH, D])
    w_in = din("w_in", [DEPTH, D, DIN])
    conv_w = din("conv_w", [DEPTH, 4, 512])
    conv_b = din("conv_b", [DEPTH, 512])
    lru_w_a = din("lru_w_a", [DEPTH, 8, 64, 64])
    lru_b_a = din("lru_b_a", [DEPTH, 512])
    lru_w_x = din("lru_w_x", [DEPTH, 8, 64, 64])
    lru_b_x = din("lru_b_x", [DEPTH, 512])
    lru_lambda = din("lru_lambda", [DEPTH, 512])
    q_norm = din("q_norm", [DEPTH, 64])
    k_norm = din("k_norm", [DEPTH, 64])
    norm_out_lru = din("norm_out_lru", [DEPTH, 512])
    norm_out_attn = din("norm_out_attn", [DEPTH, 512])
    w_out = din("w_out", [DEPTH, D, D])
    norm_ffn = din("norm_ffn", [DEPTH, D])
    router_group_w = din("router_group_w", [DEPTH, D, 4])
    router_group_b = din("router_group_b", [DEPTH, 4])
    router_expert_w = din("router_expert_w", [DEPTH, D, NE])
    router_expert_b = din("router_expert_b", [DEPTH, NE])
    w_gate = din("w_gate", [DEPTH, NE, D, 512])
    w_up = din("w_up", [DEPTH, NE, D, 512])
    w_down = din("w_down", [DEPTH, NE, 512, D])
    c_ident = din("c_ident", [128, 128])
    c_mask = din("c_mask", [128, 256])
    c_lt = din("c_lt", [128, 128])
    c_mbias = din("c_mbias", [128, 256])
    c_pio2 = din("c_pio2", [128, 2])
    out = nc.dram_tensor("out", [S, D], F32, kind="ExternalOutput").ap()

    projT = dtmp("projT", [2048, S])
    vtok = dtmp("vtok", [S, 512], BF16)
    ymixT = dtmp("ymixT", [1024, S])
    mergedT = dtmp("mergedT", [1024, S])
    xmid = dtmp("xmid", [S, D])
    h2T = dtmp("h2T", [1024, S])
    xres = [dtmp("xres%d" % i, [S, D]) for i in range(2)]
    h2tok = dtmp("h2tok", [S, D])
    xbuf = dtmp("xbuf", [NSB * 512, D])
    ybuf = dtmp("ybuf", [NSB * 512, D])

    tcount = [0]

    def tile(es, name, shape, dt=F32):
        tcount[0] += 1
        name = "%s_%d" % (name, tcount[0])
        return Buf(es.enter_context(nc.sbuf_tensor(name, shape, dt)), name)

    gstack = ExitStack()
    ident = tile(gstack, "ident", [128, 128])
    ones_r = tile(gstack, "ones_r", [128, 128], F32R)
    ones_b = tile(gstack, "ones_b", [128, 128], BF16)
    OH1 = tile(gstack, "OH1", [128, 32, NE])
    OH2 = tile(gstack, "OH2", [128, 32, NE])
    W12 = tile(gstack, "W12", [128, 32, 2])
    S1i = tile(gstack, "S1i", [128, 32], I32)
    S2i = tile(gstack, "S2i", [128, 32], I32)
    IDXWi = tile(gstack, "IDXWi", [128, NSB, 2], I32)
    pio2 = tile(gstack, "pio2", [128, 2])
    Ltb = tile(gstack, "Ltb", [128, 128], BF16)
    identb = tile(gstack, "identb", [128, 128], BF16)
    mask01 = tile(gstack, "mask01", [128, 256], BF16)
    Gall = None if MOE_SPARSE else tile(gstack, "Gall", [128, 32, NE])
    banks = [Buf(nc.alloc_psum_tensor("bank%d" % i, [128, 512], F32), "bank%d" % i) for i in range(8)]

    s.dma("sp", ident[:], c_ident[:, :], writes=[ident])
    s.dma("pool", Ltb[:], c_lt[:, :], writes=[Ltb])
    s.dma("pool", identb[:], c_ident[:, :], writes=[identb])
    s.dma("pool", mask01[:], c_mbias[:, :], writes=[mask01])
    s.dma("sp", pio2[:], c_pio2[:, :], writes=[pio2])
    ones_f = tile(gstack, "ones_f", [128, 128])
    eps_t = tile(gstack, "eps_t", [128, 1])
    s.op("dve", lambda e: e.memset(eps_t[:], EPS), writes=[eps_t])
    zeros_f = tile(gstack, "zeros_f", [128, 512])
    s.op("dve", lambda e: e.memset(ones_f[:], 1.0), writes=[ones_f])
    s.op("dve", lambda e: e.memset(zeros_f[:], 0.0), writes=[zeros_f])
    s.op("dve", lambda e: e.tensor_copy(out=ones_r[:], in_=ones_f[:]), reads=[ones_f], writes=[ones_r])
    s.op("dve", lambda e: e.memset(ones_b[:], 1.0), writes=[ones_b])

    def evac(i, dst_ap, src_ap, reads, writes):
        if i % 2 == 0:
            return s.op("act", lambda e: e.copy(out=dst_ap, in_=src_ap), reads=reads, writes=writes)
        return s.op("dve", lambda e: e.tensor_copy(out=dst_ap, in_=src_ap), reads=reads, writes=writes)

    def rstd_inplace(T, ap, scale):
        np_ = ap.partition_size()
        s.op("act", lambda e: e.activation(out=ap, in_=ap, func=AF.Ln, scale=scale, bias=eps_t[0:np_, 0:1]),
             reads=[T, eps_t], writes=[T])
        s.op("act", lambda e: e.activation(out=ap, in_=ap, func=AF.Exp, scale=-0.5), reads=[T], writes=[T])

    def rms_token_major(X, Y, SS, junk, gbc):
        for j in range(4):
            jt, jap = (junk, junk[:]) if junk is not None else (Y, Y[:, j, :])
            s.op("act", lambda e, j=j, jap=jap: e.activation(out=jap, in_=X[:, j, :], func=AF.Square,
                                                             accum_out=SS[:, j:j + 1]),
                 reads=[X], writes=[jt, SS])
        rstd_inplace(SS, SS[:], 1.0 / D)
        for j in range(4):
            s.op("dve", lambda e, j=j: e.scalar_tensor_tensor(out=Y[:, j, :], in0=X[:, j, :], scalar=SS[:, j:j + 1],
                                                              in1=gbc[:], op0=ALU.mult, op1=ALU.mult),
                 reads=[X, SS, gbc], writes=[Y])

    def transpose_chunk(X, HT, pbanks):
        for k in range(8):
            pT = pbanks[k % len(pbanks)]
            for j in range(4):
                s.op("pe", lambda e, j=j, k=k, pT=pT: e.transpose(pT[:, j * 128:(j + 1) * 128],
                                                                  X[:, j, k * 128:(k + 1) * 128], ident[:]),
                     reads=[X, ident], writes=[pT])
            evac(k, HT[:, k, :], pT[:, :], [pT], [HT])

    def phase_A(l, xsrc, fuse_g=False, xstore=None):
        with ExitStack() as es:
            win = tile(es, "win", [128, 8, DIN], F32R)
            gbc = tile(es, "gbc", [128, D])
            xin = [tile(es, "xin%d" % i, [128, 4, D]) for i in range(3)]
            junk = tile(es, "junk", [128, D])
            ss = [tile(es, "ss%d" % i, [128, 4]) for i in range(3)]
            hT = [tile(es, "hT%d" % i, [128, 8, 512], F32R) for i in range(2)]
            ost = [tile(es, "ost%d" % i, [128, 512]) for i in range(4)]
            vst = [tile(es, "vst%d" % i, [128, 512], BF16) for i in range(2)]
            if fuse_g:
                y1t = [tile(es, "y1t%d" % i, [128, D]) for i in range(2)]
                y2t = [tile(es, "y2t%d" % i, [128, D]) for i in range(2)]
            wsrc = w_in[l].rearrange("(k p) n -> p k n", p=128)
            for c5 in range(5):
                s.dma("pool", win[:, :, c5 * 512:(c5 + 1) * 512], wsrc[:, :, c5 * 512:(c5 + 1) * 512], writes=[win])
            s.dma("pool", gbc[:], norm_mix[l].partition_broadcast(128), writes=[gbc])

            def load(c):
                X = xin[c % 3]
                rows = slice(c * 512, (c + 1) * 512)
                s.dma("sp", X[:], xsrc[rows, :].rearrange("(j p) d -> p j d", p=128), writes=[X])
                if fuse_g:
                    for j in range(4):
                        t = c * 4 + j
                        Y1, Y2 = y1t[t % 2], y2t[t % 2]
                        s.indirect(Y1[:], None, ybuf[:, :], IOA(ap=S1i[:, t:t + 1], axis=0), reads=[S1i], writes=[Y1])
                        s.indirect(Y2[:], None, ybuf[:, :], IOA(ap=S2i[:, t:t + 1], axis=0), reads=[S2i], writes=[Y2])
                        s.op("dve", lambda e, Y1=Y1, t=t, j=j: e.scalar_tensor_tensor(
                            out=X[:, j, :], in0=Y1[:], scalar=W12[:, t, 0:1], in1=X[:, j, :], op0=ALU.mult, op1=ALU.add),
                            reads=[Y1, W12, X], writes=[X])
                        s.op("dve", lambda e, Y2=Y2, t=t, j=j: e.scalar_tensor_tensor(
                            out=X[:, j, :], in0=Y2[:], scalar=W12[:, t, 1:2], in1=X[:, j, :], op0=ALU.mult, op1=ALU.add),
                            reads=[Y2, W12, X], writes=[X])
                    s.dma("sp", xstore[rows, :].rearrange("(j p) d -> p j d", p=128), X[:], reads=[X])

            def prep_rms(c):
                rms_token_major(xin[c % 3], xin[c % 3], ss[c % 3], junk, gbc)

            def prep_T(c):
                transpose_chunk(xin[c % 3], hT[c % 2], banks[0:2])

            evc = [0]

            def mm(c):
                HT = hT[c % 2]
                for f in range(16):
                    pO = banks[2 + f % 4]
                    for k in range(8):
                        s.op("pe", lambda e, f=f, k=k, pO=pO: e.matmul(pO[:, :], lhsT=win[:, k, f * 128:(f + 1) * 128],
                                                                       rhs=HT[:, k, :], start=(k == 0), stop=(k == 7)),
                             reads=[win, HT], writes=[pO])
                    O = ost[f % 4]
                    evac(evc[0], O[:], pO[:, :], [pO], [O])
                    evc[0] += 1
                    s.dma("sp", projT[f * 128:(f + 1) * 128, c * 512:(c + 1) * 512], O[:], reads=[O])
                for j in range(4):
                    pV = banks[6 + j % 2]
                    for k in range(8):
                        s.op("pe", lambda e, j=j, k=k, pV=pV: e.matmul(pV[:, :], lhsT=HT[:, k, j * 128:(j + 1) * 128],
                                                                       rhs=win[:, k, 2048:2560], start=(k == 0),
                                                                       stop=(k == 7)),
                             reads=[win, HT], writes=[pV])
                    V = vst[j % 2]
                    evac(evc[0], V[:], pV[:, :], [pV], [V])
                    evc[0] += 1
                    t0 = c * 512 + j * 128
                    s.dma("sp", vtok[t0:t0 + 128, :], V[:], reads=[V])

            load(0)
            load(1)
            if MOE_SPARSE and l == 0:
                zt = tile(es, "zt", [128, 2, D])
                s.op("pool", lambda e: e.memset(zt[:], 0.0), writes=[zt])
                for i in range(NSB * 2):
                    s.dma("pool", xbuf[i * 256:(i + 1) * 256, :].rearrange("(j p) d -> p j d", p=128), zt[:], reads=[zt])
            prep_rms(0)
            prep_T(0)
            prep_rms(1)
            for c in range(8):
                if c + 2 < 8:
                    load(c + 2)
                    prep_rms(c + 2)
                if c + 1 < 8:
                    prep_T(c + 1)
                mm(c)
        s.barrier()

    def phase_B(l):
        HT_ = 1024
        NQ = S // HT_
        with ExitStack() as es:
            lp = tile(es, "lp", [128, 4, 16])
            tmpp = tile(es, "tmpp", [128, 4, 4])
            wab = tile(es, "wab", [128, 4, 128], F32R)
            wxb = tile(es, "wxb", [128, 4, 128], F32R)
            xl_2 = [tile(es, "xl%d" % i_, [128, 3 + HT_], F32R) for i_ in range(2)]
            xc_2 = [tile(es, "xc%d" % i_, [128, HT_], F32R) for i_ in range(2)]
            dg = tile(es, "dg", [128, 4, 128], F32R)
            rt_2 = [tile(es, "rt%d" % i_, [128, HT_]) for i_ in range(2)]
            it_2 = [tile(es, "it%d" % i_, [128, HT_]) for i_ in range(2)]
            wt_2 = [tile(es, "wt%d" % i_, [128, HT_]) for i_ in range(2)]
            em_2 = [tile(es, "em%d" % i_, [128, HT_]) for i_ in range(2)]
            t1_2 = [tile(es, "t1%d" % i_, [128, HT_]) for i_ in range(2)]
            at_2 = [tile(es, "at%d" % i_, [128, HT_]) for i_ in range(2)]
            hh_2 = [tile(es, "hh%d" % i_, [128, HT_]) for i_ in range(2)]
            gt_2 = [tile(es, "gt%d" % i_, [128, HT_]) for i_ in range(2)]
            t2_2 = [tile(es, "t2%d" % i_, [128, HT_]) for i_ in range(2)]
            hlast = tile(es, "hlast", [128, 1])
            def pload(col, src):
                s.dma("sp", lp[:, :, col], src.rearrange("(j p) -> p j", p=128), writes=[lp],
                      allow_slow_non_contiguous=True)
            for t in range(4):
                pload(t, conv_w[l, t])
            pload(4, conv_b[l])
            pload(5, lru_b_a[l])
            pload(6, lru_b_x[l])
            pload(7, lru_lambda[l])
            z = tmpp[:, :, 0]
            q = tmpp[:, :, 1]
            lnz = tmpp[:, :, 2]
            msk = tmpp[:, :, 3]
            s.op("act", lambda e: e.activation(out=z, in_=lp[:, :, 7], func=AF.Exp, scale=-1.0), reads=[lp], writes=[tmpp])
            s.op("act", lambda e: e.activation(out=lnz, in_=z, func=AF.Ln, bias=1.0), reads=[tmpp], writes=[tmpp])
            coef = [1.0, -1.0 / 2, 1.0 / 3, -1.0 / 4, 1.0 / 5, -1.0 / 6, 1.0 / 7]
            s.op("dve", lambda e: e.tensor_scalar(out=q, in0=z, scalar1=coef[6], scalar2=None, op0=ALU.mult),
                 reads=[tmpp], writes=[tmpp])
            for ci in (5, 4, 3, 2, 1, 0):
                s.op("dve", lambda e, ci=ci: e.scalar_tensor_tensor(out=q, in0=q, scalar=coef[ci], in1=z, op0=ALU.add,
                                                                    op1=ALU.mult), reads=[tmpp], writes=[tmpp])
            s.op("dve", lambda e: e.tensor_scalar(out=msk, in0=z, scalar1=0.1, scalar2=None, op0=ALU.is_lt),
                 reads=[tmpp], writes=[tmpp])
            s.op("dve", lambda e: e.tensor_tensor(out=q, in0=q, in1=lnz, op=ALU.subtract), reads=[tmpp], writes=[tmpp])
            s.op("dve", lambda e: e.tensor_tensor(out=q, in0=q, in1=msk, op=ALU.mult), reads=[tmpp], writes=[tmpp])
            s.op("dve", lambda e: e.tensor_tensor(out=q, in0=q, in1=lnz, op=ALU.add), reads=[tmpp], writes=[tmpp])
            s.op("dve", lambda e: e.tensor_scalar(out=lp[:, :, 8], in0=q, scalar1=-8.0, scalar2=None, op0=ALU.mult),
                 reads=[tmpp], writes=[lp])
            s.op("dve", lambda e: e.tensor_scalar(out=lp[:, :, 9], in0=q, scalar1=-4.0, scalar2=None, op0=ALU.mult),
                 reads=[tmpp], writes=[lp])
            s.op("dve", lambda e: e.tensor_copy(out=wab[:].rearrange("p a b -> p (a b)"), in_=zeros_f[:]),
                 reads=[zeros_f], writes=[wab])
            s.op("dve", lambda e: e.tensor_copy(out=wxb[:].rearrange("p a b -> p (a b)"), in_=zeros_f[:]),
                 reads=[zeros_f], writes=[wxb])
            for jj in range(4):
                for hb in range(2):
                    p0 = hb * 64
                    s.dma("pool", wab[p0:p0 + 64, jj, p0:p0 + 64], lru_w_a[l, 2 * jj + hb], writes=[wab])
                    s.dma("pool", wxb[p0:p0 + 64, jj, p0:p0 + 64], lru_w_x[l, 2 * jj + hb], writes=[wxb])
            bk = [0]
            def run_pass(jj, hf, pi_):
                t0 = hf * HT_
                xl, xc, rt, it, wt, em = xl_2[pi_ % 2], xc_2[pi_ % 2], rt_2[pi_ % 2], it_2[pi_ % 2], wt_2[pi_ % 2], em_2[pi_ % 2]
                t1, at, hh, gt, t2 = t1_2[pi_ % 2], at_2[pi_ % 2], hh_2[pi_ % 2], gt_2[pi_ % 2], t2_2[pi_ % 2]
                if hf == 0:
                    s.op("dve", lambda e: e.tensor_copy(out=xl[:, 0:3], in_=zeros_f[:, 0:3]), reads=[zeros_f],
                         writes=[xl])
                    yield
                    s.dma("pool", xl[:, 3:3 + HT_], projT[jj * 128:(jj + 1) * 128, 0:HT_], writes=[xl])
                    yield
                    for k in range(4):
                        s.op("dve", lambda e, jj=jj, k=k: e.tensor_scalar(out=dg[:, k, :], in0=ident[:],
                                                                          scalar1=lp[:, jj, k:k + 1], scalar2=None,
                                                                          op0=ALU.mult), reads=[ident, lp], writes=[dg])
                        yield
                else:
                    s.dma("pool", xl[:, :], projT[jj * 128:(jj + 1) * 128, t0 - 3:t0 + HT_], writes=[xl])
                    yield
                s.dma("sp", gt[:], projT[512 + jj * 128:512 + (jj + 1) * 128, t0:t0 + HT_], writes=[gt])
                yield
                for cb in range(HT_ // 512):
                    pC = banks[bk[0] % 8]
                    bk[0] += 1
                    for k in range(4):
                        s.op("pe", lambda e, pC=pC, cb=cb, k=k: e.matmul(
                            pC[:, :], lhsT=dg[:, k, :], rhs=xl[:, cb * 512 + k:cb * 512 + k + 512], start=(k == 0),
                            stop=(k == 3)), reads=[dg, xl], writes=[pC])
                        yield
                    s.op("act", lambda e, pC=pC, cb=cb, jj=jj: e.add(out=xc[:, cb * 512:(cb + 1) * 512], in_=pC[:, :],
                                                                     add=lp[:, jj, 4:5]), reads=[pC, lp], writes=[xc])
                    yield
                for cb in range(HT_ // 512):
                    cols = slice(cb * 512, (cb + 1) * 512)
                    for (wb, dst, bcol) in ((wab, rt, 5), (wxb, it, 6)):
                        pR = banks[bk[0] % 8]
                        bk[0] += 1
                        s.op("pe", lambda e, wb=wb, pR=pR, cols=cols, jj=jj: e.matmul(
                            pR[:, :], lhsT=wb[:, jj, :], rhs=xc[:, cols], start=True, stop=True),
                            reads=[wb, xc], writes=[pR])
                        yield
                        s.op("act", lambda e, dst=dst, pR=pR, cols=cols, jj=jj, bcol=bcol: e.activation(
                            out=dst[:, cols], in_=pR[:, :], func=AF.Sigmoid, bias=lp[:, jj, bcol:bcol + 1]),
                            reads=[pR, lp], writes=[dst])
                        yield
                s.op("act", lambda e, jj=jj: e.activation(out=at[:], in_=rt[:], func=AF.Exp, scale=lp[:, jj, 8:9]),
                     reads=[rt, lp], writes=[at])
                yield
                s.op("act", lambda e, jj=jj: e.activation(out=t1[:], in_=rt[:], func=AF.Tanh, scale=lp[:, jj, 9:10]),
                     reads=[rt, lp], writes=[t1])
                yield
                s.op("dve", lambda e: e.scalar_tensor_tensor(out=em[:], in0=at[:], scalar=1.0, in1=t1[:], op0=ALU.add,
                                                             op1=ALU.mult), reads=[at, t1], writes=[em])
                yield
                s.op("dve", lambda e: e.scalar_tensor_tensor(out=t1[:], in0=em[:], scalar=2.0, in1=em[:], op0=ALU.add,
                                                             op1=ALU.mult), reads=[em], writes=[t1])
                yield
                s.op("act", lambda e: e.activation(out=t1[:], in_=t1[:], func=AF.Sqrt, scale=-1.0),
                     reads=[t1], writes=[t1])
                yield
                s.op("pool", lambda e: e.tensor_tensor(out=it[:], in0=it[:], in1=xc[:].bitcast(F32), op=ALU.mult),
                     reads=[it, xc], writes=[it])
                yield
                s.op("dve", lambda e: e.tensor_tensor(out=it[:], in0=it[:], in1=t1[:], op=ALU.mult),
                     reads=[it, t1], writes=[it])
                yield
                if hf == 0:
                    s.op("dve", lambda e: e.tensor_tensor_scan(out=hh[:], data0=at[:], data1=it[:], initial=0.0,
                                                               op0=ALU.mult, op1=ALU.add),
                         reads=[at, it], writes=[hh])
                    yield
                else:
                    s.op("dve", lambda e: e.tensor_tensor_scan(out=hh[:], data0=at[:], data1=it[:],
                                                               initial=hlast[:, 0:1], op0=ALU.mult, op1=ALU.add),
                         reads=[at, it, hlast], writes=[hh])
                    yield
                if hf + 1 < NQ:
                    s.op("act", lambda e: e.copy(out=hlast[:, 0:1], in_=hh[:, HT_ - 1:HT_]), reads=[hh], writes=[hlast])
                    yield
                s.op("pool", lambda e: e.tensor_tensor(out=t2[:], in0=gt[:], in1=gt[:], op=ALU.mult),
                     reads=[gt], writes=[t2])
                yield
                s.op("pool", lambda e: e.tensor_scalar(out=t2[:], in0=t2[:], scalar1=0.044715, scalar2=1.0, op0=ALU.mult,
                                                       op1=ALU.add), reads=[t2], writes=[t2])
                yield
                s.op("pool", lambda e: e.tensor_tensor(out=t2[:], in0=t2[:], in1=gt[:], op=ALU.mult),
                     reads=[t2, gt], writes=[t2])
                yield
                s.op("act", lambda e: e.activation(out=t2[:], in_=t2[:], func=AF.Sigmoid, scale=1.5957691216057308),
                     reads=[t2], writes=[t2])
                yield
                s.op("pool", lambda e: e.tensor_tensor(out=t2[:], in0=t2[:], in1=gt[:], op=ALU.mult),
                     reads=[t2, gt], writes=[t2])
                yield
                s.op("dve", lambda e: e.tensor_tensor(out=t2[:], in0=t2[:], in1=hh[:], op=ALU.mult),
                     reads=[t2, hh], writes=[t2])
                yield
                s.dma("sp", ymixT[jj * 128:(jj + 1) * 128, t0:t0 + HT_], t2[:], reads=[t2])
                yield

            passes = [(jj, hf) for jj in range(4) for hf in range(NQ)]
            gens = [run_pass(jj, hf, i_) for i_, (jj, hf) in enumerate(passes)]
            SKEW = 18
            active = []
            nxt = 0
            while active or nxt < len(gens):
                if nxt < len(gens) and len(active) < 2 and (not active or active[0][1] >= SKEW):
                    active.append([gens[nxt], 0])
                    nxt += 1
                for ent in list(active):
                    try:
                        next(ent[0])
                        ent[1] += 1
                    except StopIteration:
                        active.remove(ent)
        s.barrier()

    def phase_C(l):
        NBUF, LA = 6, 3
        with ExitStack() as es:
            gqk = tile(es, "gqk", [64, 2])
            qraw = [tile(es, "qraw%d" % i, [64, S]) for i in range(2)]
            kraw = [tile(es, "kraw%d" % i, [64, S]) for i in range(2)]
            qn = [tile(es, "qn%d" % i, [64, S], BF16) for i in range(2)]
            kn = [tile(es, "kn%d" % i, [64, S], BF16) for i in range(2)]
            Vd = [[tile(es, "Vd%d_%d" % (i, j), [128, 32, 128], BF16) for j in range(3)] for i in range(2)]
            for i in range(2):
                for j in range(3):
                    s.op("dve", lambda e, i=i, j=j: e.memset(Vd[i][j][:], 1.0), writes=[Vd[i][j]])
            acc = tile(es, "acc", [128, S])
            dsh = tile(es, "dsh", [64, S])
            sq = [tile(es, "sq%d" % i, [64, 512], F32R) for i in range(2)]
            rs = [tile(es, "rs%d" % i, [64, 512]) for i in range(2)]
            pm_ = [tile(es, "pm%d" % i, [128, 256], BF16) for i in range(NBUF)]
            NPS = 3
            pS_ = [banks[i] for i in range(NPS)]
            pO_ = [banks[3 + i] for i in range(NPS)]
            pN_ = [banks[6], banks[7]]
            s.dma("sp", gqk[:, 0:1], q_norm[l].rearrange("(p o) -> p o", o=1), writes=[gqk])
            s.dma("sp", gqk[:, 1:2], k_norm[l].rearrange("(p o) -> p o", o=1), writes=[gqk])

            def load(h):
                s.dma("sp", qraw[h % 2][:], projT[1024 + h * 64:1024 + (h + 1) * 64, :], writes=[qraw[h % 2]])
                s.dma("sp", kraw[h % 2][:], projT[1536 + h * 64:1536 + (h + 1) * 64, :], writes=[kraw[h % 2]])
                for bi, d in enumerate((1, 4, 16)):
                    nb = 32 // d
                    vv = vtok.rearrange("(n p r) c -> r p n c", p=128, r=d)
                    V = Vd[h % 2][bi]
                    for r in range(d):
                        s.dma("sp", V[:, r * nb:(r + 1) * nb, 0:64], vv[r][:, :, h * 64:(h + 1) * 64], writes=[V])

            def qknorm_steps(h):
                steps = []
                for (raw, nrm, gc) in ((qraw[h % 2], qn[h % 2], 0), (kraw[h % 2], kn[h % 2], 1)):
                    for cb in range(8):
                        cols = slice(cb * 512, (cb + 1) * 512)
                        SQ, RS, pN = sq[cb % 2], rs[cb % 2], pN_[cb % 2]

                        def half_a(raw=raw, cols=cols, SQ=SQ, pN=pN):
                            s.op("act", lambda e: e.activation(out=SQ[:], in_=raw[:, cols], func=AF.Square),
                                 reads=[raw], writes=[SQ])
                            s.op("pe", lambda e: e.matmul(pN[0:64, :], lhsT=ones_r[0:64, 0:64], rhs=SQ[:], start=True,
                                                          stop=True), reads=[ones_r, SQ], writes=[pN])

                        def half_b(raw=raw, nrm=nrm, gc=gc, cols=cols, RS=RS, pN=pN):
                            s.op("act", lambda e: e.activation(out=RS[:], in_=pN[0:64, :], func=AF.Ln, scale=1.0 / 64,
                                                               bias=eps_t[0:64, 0:1]), reads=[pN, eps_t], writes=[RS])
                            s.op("act", lambda e: e.activation(out=RS[:], in_=RS[:], func=AF.Exp, scale=-0.5),
                                 reads=[RS], writes=[RS])
                            s.op("dve", lambda e: e.scalar_tensor_tensor(out=nrm[:, cols], in0=raw[:, cols],
                                                                         scalar=gqk[:, gc:gc + 1], in1=RS[:],
                                                                         op0=ALU.mult, op1=ALU.mult),
                                 reads=[raw, gqk, RS], writes=[nrm])
                        steps.append(half_a)
                        steps.append(half_b)
                return steps

            load(0)
            for st in qknorm_steps(0):
                st()
            for h in range(8):
                if h + 1 < 8:
                    load(h + 1)
                    pending = qknorm_steps(h + 1)
                else:
                    pending = []
                QN, KN, VD = qn[h % 2], kn[h % 2], Vd[h % 2]
                blocks = []
                for bi, d in enumerate((1, 4, 16)):
                    nb = 32 // d
                    for r in range(d):
                        for n in range(nb):
                            blocks.append((bi, d, nb, r, n))

                def stage1(i):
                    bi, d, nb, r, n = blocks[i]
                    qv = QN[:].rearrange("p (n i r) -> p r n i", i=128, r=d)
                    kv = KN[:].rearrange("p (n i r) -> p r n i", i=128, r=d)
                    pS, PM_ = pS_[i % NPS], pm_[i % NBUF]
                    w = 256 if n + 1 < nb else 128
                    nq = w // 128
                    s.op("pe", lambda e: e.matmul(pS[:, 0:w], lhsT=kv[:, r, n, :], rhs=qv[:, r, n:n + nq, :], start=True,
                                                  stop=True), reads=[KN, QN], writes=[pS])
                    s.op("act", lambda e: e.activation(out=PM_[:, 0:w], in_=pS[:, 0:w], func=AF.Exp, scale=0.125),
                         reads=[pS], writes=[PM_])
                    s.op("dve", lambda e: e.tensor_tensor(out=PM_[:, 0:w], in0=PM_[:, 0:w], in1=mask01[:, 0:w], op=ALU.mult),
                         reads=[PM_, mask01], writes=[PM_])

                def stage2(i):
                    bi, d, nb, r, n = blocks[i]
                    av = acc[:].rearrange("p (n i r) -> p r n i", i=128, r=d)
                    pO, PMc, PMp = pO_[i % NPS], pm_[i % NBUF], pm_[(i - 1) % NBUF]
                    V = VD[bi]
                    blk = r * nb + n
                    if n > 0:
                        s.op("pe", lambda e: e.matmul(pO[:, 0:128], lhsT=V[:, blk - 1, :], rhs=PMp[:, 128:256], start=True,
                                                      stop=False), reads=[V, PMp], writes=[pO])
                        s.op("pe", lambda e: e.matmul(pO[:, 0:128], lhsT=V[:, blk, :], rhs=PMc[:, 0:128], start=False,
                                                      stop=True), reads=[V, PMc], writes=[pO])
                    else:
                        s.op("pe", lambda e: e.matmul(pO[:, 0:128], lhsT=V[:, blk, :], rhs=PMc[:, 0:128], start=True,
                                                      stop=True), reads=[V, PMc], writes=[pO])
                    dst = av[:, r, n, :]
                    if bi == 0:
                        s.op("dve", lambda e: e.tensor_copy(out=dst, in_=pO[:, 0:128]), reads=[pO], writes=[acc])
                    else:
                        s.op("dve", lambda e: e.tensor_tensor(out=dst, in0=dst, in1=pO[:, 0:128], op=ALU.add),
                             reads=[pO, acc], writes=[acc])

                nblk = len(blocks)
                for i in range(nblk + LA):
                    if i < nblk:
                        stage1(i)
                    if i - LA >= 0:
                        stage2(i - LA)
                    if pending and i % 3 == 2:
                        pending.pop(0)()
                while pending:
                    pending.pop(0)()
                s.op("act", lambda e: e.activation(out=acc[64:128, :], in_=acc[64:128, :], func=AF.Ln), reads=[acc],
                     writes=[acc])
                s.op("act", lambda e: e.activation(out=acc[64:128, :], in_=acc[64:128, :], func=AF.Exp, scale=-1.0),
                     reads=[acc], writes=[acc])
                s.op("dve", lambda e: e.tensor_copy(out=dsh[:], in_=acc[64:128, :]), reads=[acc], writes=[dsh])
                s.op("dve", lambda e: e.tensor_tensor(out=dsh[:], in0=acc[0:64, :], in1=dsh[:], op=ALU.mult),
                     reads=[acc, dsh], writes=[dsh])
                s.dma("sp", ymixT[512 + h * 64:512 + (h + 1) * 64, :], dsh[:], reads=[dsh])
        s.barrier()

    def phase_N(l):
        with ExitStack() as es:
            gg = tile(es, "gg", [128, 2, 4])
            ya = [tile(es, "ya%d" % i, [128, 4, 512]) for i in range(2)]
            sq4 = [tile(es, "sq4%d" % i, [128, 4, 512], F32R) for i in range(2)]
            rsn = [tile(es, "rsn%d" % i, [128, 512]) for i in range(2)]
            mo = [tile(es, "mo%d" % i, [128, 4, 512]) for i in range(2)]
            s.dma("sp", gg[:, 0, :], norm_out_lru[l].rearrange("(j p) -> p j", p=128), writes=[gg],
                  allow_slow_non_contiguous=True)
            s.dma("sp", gg[:, 1, :], norm_out_attn[l].rearrange("(j p) -> p j", p=128), writes=[gg],
                  allow_slow_non_contiguous=True)
            it_ = 0
            for half in range(2):
                for cb in range(8):
                    cols = slice(cb * 512, (cb + 1) * 512)
                    YA, SQ, RS, MO, pS = ya[it_ % 2], sq4[it_ % 2], rsn[it_ % 2], mo[it_ % 2], banks[it_ % 2]
                    it_ += 1
                    s.dma("sp", YA[:], ymixT[half * 512:(half + 1) * 512, cols].rearrange("(j p) c -> p j c", p=128),
                          writes=[YA])
                    for jj in range(4):
                        s.op("act", lambda e, YA=YA, SQ=SQ, jj=jj: e.activation(out=SQ[:, jj, :], in_=YA[:, jj, :],
                                                                                func=AF.Square),
                             reads=[YA], writes=[SQ])
                    for jj in range(4):
                        s.op("pe", lambda e, SQ=SQ, pS=pS, jj=jj: e.matmul(pS[:, :], lhsT=ones_r[:, :], rhs=SQ[:, jj, :],
                                                                           start=(jj == 0), stop=(jj == 3)),
                             reads=[ones_r, SQ], writes=[pS])
                    s.op("dve", lambda e, RS=RS, pS=pS: e.tensor_scalar(out=RS[:], in0=pS[:, :], scalar1=1.0 / 512,
                                                                        scalar2=EPS, op0=ALU.mult, op1=ALU.add),
                         reads=[pS], writes=[RS])
                    s.op("act", lambda e, RS=RS: e.activation(out=RS[:], in_=RS[:], func=AF.Sqrt), reads=[RS], writes=[RS])
                    s.op("dve", lambda e, RS=RS: e.reciprocal(out=RS[:], in_=RS[:]), reads=[RS], writes=[RS])
                    for jj in range(4):
                        s.op("dve", lambda e, YA=YA, MO=MO, RS=RS, jj=jj, half=half: e.scalar_tensor_tensor(
                            out=MO[:, jj, :], in0=YA[:, jj, :], scalar=gg[:, half, jj:jj + 1], in1=RS[:], op0=ALU.mult,
                            op1=ALU.mult), reads=[YA, gg, RS], writes=[MO])
                    s.dma("sp", mergedT[half * 512:(half + 1) * 512, cols].rearrange("(j p) c -> p j c", p=128), MO[:],
                          reads=[MO])
        s.barrier()

    def phase_D(l, xsrc):
        with ExitStack() as es:
            wout = tile(es, "wout", [128, 8, D], F32R)
            gbc = tile(es, "gbc2", [128, D])
            wr = tile(es, "wr", [128, 8, 36])
            bbc = tile(es, "bbc", [128, 36])
            xin = [tile(es, "xd%d" % i, [128, 4, D]) for i in range(2)]
            mT = [tile(es, "mT%d" % i, [128, 8, 512], F32R) for i in range(2)]
            ym = [tile(es, "ym%d" % i, [128, 8, 512]) for i in range(2)]
            sq4 = [tile(es, "sq4%d" % i, [128, 4, 512], F32R) for i in range(1)] * 2
            rsn = [tile(es, "rsn%d" % i, [128, 512]) for i in range(1)] * 2
            gg = tile(es, "gg", [128, 8])
            h2b = [tile(es, "h2_%d" % i, [128, 4, D]) for i in range(2)]
            h2t = [tile(es, "h2t%d" % i, [128, 8, 512]) for i in range(1)] * 2
            s.dma("sp", gg[:, 0:4], norm_out_lru[l].rearrange("(j p) -> p j", p=128), writes=[gg],
                  allow_slow_non_contiguous=True)
            s.dma("sp", gg[:, 4:8], norm_out_attn[l].rearrange("(j p) -> p j", p=128), writes=[gg],
                  allow_slow_non_contiguous=True)
            junk = None
            ss = [tile(es, "ssd%d" % i, [128, 4]) for i in range(2)]
            rt_ = [tile(es, "rtd%d" % i, [128, 96]) for i in range(2)]
            LGS = tile(es, "LGS", [128, 32, 4])
            DD = tile(es, "DD", [128, 32])
            PT = tile(es, "PT", [128, 32])
            wsrc = w_out[l].rearrange("(k p) n -> p k n", p=128)
            for c2 in range(2):
                s.dma("pool", wout[:, :, c2 * 512:(c2 + 1) * 512], wsrc[:, :, c2 * 512:(c2 + 1) * 512], writes=[wout])
            s.dma("pool", gbc[:], norm_ffn[l].partition_broadcast(128), writes=[gbc])
            s.dma("sp", wr[:, :, 0:4], router_group_w[l].rearrange("(k p) n -> p k n", p=128), writes=[wr])
            s.dma("sp", wr[:, :, 4:36], router_expert_w[l].rearrange("(k p) n -> p k n", p=128), writes=[wr])
            s.dma("pool", bbc[:, 0:4], router_group_b[l].partition_broadcast(128), writes=[bbc])
            s.dma("pool", bbc[:, 4:36], router_expert_b[l].partition_broadcast(128), writes=[bbc])

            def load(c):
                cols = slice(c * 512, (c + 1) * 512)
                s.dma("sp", ym[c % 2][:], ymixT[:, cols].rearrange("(k p) c -> p k c", p=128), writes=[ym[c % 2]])
                s.dma("sp", xin[c % 2][:], xsrc[c * 512:(c + 1) * 512, :].rearrange("(j p) d -> p j d", p=128),
                      writes=[xin[c % 2]])

            nrm_i = [0]

            def mixnorm(c):
                YM, MT = ym[c % 2], mT[c % 2]
                for half in range(2):
                    i_ = nrm_i[0]
                    nrm_i[0] += 1
                    SQ, RS, pS = sq4[i_ % 2], rsn[i_ % 2], banks[i_ % 2]
                    for jj in range(4):
                        s.op("act", lambda e, jj=jj: e.activation(out=SQ[:, jj, :], in_=YM[:, half * 4 + jj, :],
                                                                  func=AF.Square), reads=[YM], writes=[SQ])
                    for jj in range(4):
                        s.op("pe", lambda e, jj=jj: e.matmul(pS[:, :], lhsT=ones_r[:, :], rhs=SQ[:, jj, :], start=(jj == 0),
                                                             stop=(jj == 3)), reads=[ones_r, SQ], writes=[pS])
                    s.op("act", lambda e: e.activation(out=RS[:], in_=pS[:, :], func=AF.Ln, scale=1.0 / 512,
                                                       bias=eps_t[:, 0:1]), reads=[pS, eps_t], writes=[RS])
                    s.op("act", lambda e: e.activation(out=RS[:], in_=RS[:], func=AF.Exp, scale=-0.5), reads=[RS],
                         writes=[RS])
                    for jj in range(4):
                        k_ = half * 4 + jj
                        s.op("dve", lambda e, k_=k_: e.scalar_tensor_tensor(out=MT[:, k_, :], in0=YM[:, k_, :],
                                                                            scalar=gg[:, k_:k_ + 1], in1=RS[:],
                                                                            op0=ALU.mult, op1=ALU.mult),
                             reads=[YM, gg, RS], writes=[MT])

            pi = [0]

            def MM(c):
                X, MT = xin[c % 2], mT[c % 2]
                for j in range(4):
                    for hf in range(2):
                        pY = banks[2 + pi[0] % 4]
                        pi[0] += 1
                        hc = slice(hf * 512, (hf + 1) * 512)
                        for k in range(8):
                            s.op("pe", lambda e, pY=pY, k=k, j=j, hc=hc: e.matmul(
                                pY[:, :], lhsT=MT[:, k, j * 128:(j + 1) * 128], rhs=wout[:, k, hc], start=(k == 0),
                                stop=(k == 7)), reads=[MT, wout], writes=[pY])
                        s.op("dve", lambda e, pY=pY, j=j, hc=hc: e.tensor_tensor(out=X[:, j, hc], in0=pY[:, :],
                                                                                in1=X[:, j, hc], op=ALU.add),
                             reads=[pY, X], writes=[X])
                s.dma("pool", xmid[c * 512:(c + 1) * 512, :].rearrange("(j p) d -> p j d", p=128), X[:], reads=[X])

            def RMS(c):
                rms_token_major(xin[c % 2], h2b[c % 2], ss[c % 2], junk, gbc)

            def TR(c):
                H2T = h2t[c % 2]
                h2 = h2b[c % 2]
                transpose_chunk(h2, H2T, banks[0:2])
                if MOE_SPARSE:
                    s.dma("pool", h2tok[c * 512:(c + 1) * 512, :].rearrange("(j p) d -> p j d", p=128), h2[:], reads=[h2])
                else:
                    s.dma("pool", h2T[:, c * 512:(c + 1) * 512].rearrange("(k p) c -> p k c", p=128), H2T[:], reads=[H2T])
                router(c, H2T)

            def router(c, H2T):
                for j in range(4):
                    R, pR = rt_[j % 2], banks[6 + j % 2]
                    ti = c * 4 + j
                    for k in range(8):
                        s.op("pe", lambda e, pR=pR, k=k, j=j: e.matmul(
                            pR[:, 0:36], lhsT=H2T[:, k, j * 128:(j + 1) * 128], rhs=wr[:, k, :], start=(k == 0),
                            stop=(k == 7)), reads=[H2T, wr], writes=[pR])
                    lg, gmax, oh, pen, m8 = R[:, 0:36], R[:, 36:37], R[:, 40:44], R[:, 44:48], R[:, 52:60]

                    def dv(fn, R=R, er=(), ew=()):
                        s.op("dve", fn, reads=[R] + list(er), writes=[R] + list(ew))

                    s.op("dve", lambda e, lg=lg, pR=pR: e.tensor_tensor(out=lg, in0=pR[:, 0:36], in1=bbc[:], op=ALU.add),
                         reads=[pR, bbc], writes=[R])
                    dv(lambda e, lg=lg, gmax=gmax: e.tensor_reduce(out=gmax, in_=lg[:, 0:4], axis=mybir.AxisListType.X,
                                                                   op=ALU.max))
                    dv(lambda e, lg=lg, gmax=gmax, ti=ti: e.tensor_scalar(out=LGS[:, ti, :], in0=lg[:, 0:4], scalar1=gmax,
                                                                          scalar2=None, op0=ALU.subtract), ew=[LGS])
                    dv(lambda e, lg=lg, gmax=gmax, oh=oh: e.tensor_scalar(out=oh, in0=lg[:, 0:4], scalar1=gmax, scalar2=None,
                                                                          op0=ALU.is_equal))
                    dv(lambda e, oh=oh, pen=pen: e.tensor_scalar(out=pen, in0=oh, scalar1=-1.0, scalar2=1.0e30,
                                                                 op0=ALU.add, op1=ALU.mult))
                    for g in range(4):
                        dv(lambda e, lg=lg, pen=pen, g=g: e.tensor_scalar(
                            out=lg[:, 4 + g * 8:12 + g * 8], in0=lg[:, 4 + g * 8:12 + g * 8], scalar1=pen[:, g:g + 1],
                            scalar2=None, op0=ALU.add))
                    dv(lambda e, m8=m8, lg=lg: e.max(out=m8, in_=lg[:, 4:36]))
                    dv(lambda e, m8=m8, ti=ti: e.tensor_tensor(out=DD[:, ti:ti + 1], in0=m8[:, 1:2], in1=m8[:, 0:1],
                                                               op=ALU.subtract), ew=[DD])
                    dv(lambda e, lg=lg, m8=m8, ti=ti: e.tensor_scalar(out=OH1[:, ti, :], in0=lg[:, 4:36], scalar1=m8[:, 0:1],
                                                                      scalar2=None, op0=ALU.is_equal), ew=[OH1])
                    dv(lambda e, lg=lg, m8=m8, ti=ti: e.tensor_scalar(out=OH2[:, ti, :], in0=lg[:, 4:36], scalar1=m8[:, 1:2],
                                                                      scalar2=None, op0=ALU.is_equal), ew=[OH2])

            def gate_weights():
                fl = "p a b -> p (a b)"
                s.op("act", lambda e: e.activation(out=LGS[:].rearrange(fl), in_=LGS[:].rearrange(fl), func=AF.Exp),
                     reads=[LGS], writes=[LGS])
                s.op("dve", lambda e: e.tensor_reduce(out=PT[:], in_=LGS[:], axis=mybir.AxisListType.X, op=ALU.add),
                     reads=[LGS], writes=[PT])
                s.op("dve", lambda e: e.reciprocal(out=PT[:], in_=PT[:]), reads=[PT], writes=[PT])
                s.op("act", lambda e: e.activation(out=DD[:], in_=DD[:], func=AF.Exp), reads=[DD], writes=[DD])
                s.op("dve", lambda e: e.tensor_scalar(out=DD[:], in0=DD[:], scalar1=1.0, scalar2=None, op0=ALU.add),
                     reads=[DD], writes=[DD])
                s.op("dve", lambda e: e.reciprocal(out=DD[:], in_=DD[:]), reads=[DD], writes=[DD])
                s.op("dve", lambda e: e.tensor_tensor(out=W12[:, :, 0], in0=DD[:], in1=PT[:], op=ALU.mult),
                     reads=[DD, PT], writes=[W12])
                s.op("dve", lambda e: e.tensor_tensor(out=W12[:, :, 1], in0=PT[:], in1=W12[:, :, 0], op=ALU.subtract),
                     reads=[PT, W12], writes=[W12])

            load(0)
            mixnorm(0)
            if 1 < 8:
                load(1)
                mixnorm(1)
            MM(0)
            RMS(0)
            for c in range(8):
                if c + 2 < 8:
                    load(c + 2)
                    mixnorm(c + 2)
                if c + 1 < 8:
                    MM(c + 1)
                    RMS(c + 1)
                TR(c)
            gate_weights()
        s.barrier()

    def phase_E(l, xdst):
        T = 1024
        with ExitStack() as es:
            hsc = tile(es, "hsc", [128, 8, T], F32R)
            yacc = tile(es, "yacc", [128, T // 128, D])
            wg = [tile(es, "wg%d" % i, [128, 8, 512], F32R) for i in range(2)]
            wu = [tile(es, "wu%d" % i, [128, 8, 512], F32R) for i in range(2)]
            wd = [tile(es, "wd%d" % i, [128, 4, D], F32R) for i in range(2)]
            hid = [tile(es, "hid%d" % i, [128, 4, 512], F32R) for i in range(2)]
            sg = [tile(es, "sg%d" % i, [128, 512]) for i in range(2)]
            wi = 0
            gi = 0
            hi = 0
            for sc in range(S // T):
                t0 = sc * T
                s.dma("pool", hsc[:], h2T[:, t0:t0 + T].rearrange("(k p) c -> p k c", p=128), writes=[hsc])
                s.dma("sp", yacc[:], xmid[t0:t0 + T, :].rearrange("(j p) d -> p j d", p=128), writes=[yacc])
                for e_ in range(NE):
                    WG, WU, WD = wg[wi % 2], wu[wi % 2], wd[wi % 2]
                    wi += 1
                    s.dma("pool", WG[:], w_gate[l, e_].rearrange("(k p) n -> p k n", p=128), writes=[WG])
                    s.dma("pool", WU[:], w_up[l, e_].rearrange("(k p) n -> p k n", p=128), writes=[WU])
                    s.dma("pool", WD[:], w_down[l, e_].rearrange("(m p) n -> p m n", p=128), writes=[WD])
                    for sb in range(T // 512):
                        cols = slice(sb * 512, (sb + 1) * 512)
                        HID = hid[hi % 2]
                        hi += 1
                        for m in range(4):
                            pG, pU, SG = banks[gi % 2], banks[2 + gi % 2], sg[gi % 2]
                            gi += 1
                            for k in range(8):
                                s.op("pe", lambda e, pG=pG, WG=WG, k=k, m=m, cols=cols: e.matmul(
                                    pG[:, :], lhsT=WG[:, k, m * 128:(m + 1) * 128], rhs=hsc[:, k, cols], start=(k == 0),
                                    stop=(k == 7)), reads=[WG, hsc], writes=[pG])
                            for k in range(8):
                                s.op("pe", lambda e, pU=pU, WU=WU, k=k, m=m, cols=cols: e.matmul(
                                    pU[:, :], lhsT=WU[:, k, m * 128:(m + 1) * 128], rhs=hsc[:, k, cols], start=(k == 0),
                                    stop=(k == 7)), reads=[WU, hsc], writes=[pU])
                            s.op("act", lambda e, SG=SG, pG=pG: e.activation(out=SG[:], in_=pG[:, :], func=AF.Silu),
                                 reads=[pG], writes=[SG])
                            s.op("dve", lambda e, HID=HID, SG=SG, pU=pU, m=m: e.tensor_tensor(
                                out=HID[:, m, :], in0=SG[:], in1=pU[:, :], op=ALU.mult), reads=[SG, pU], writes=[HID])
                        for j in range(4):
                            tl = sb * 4 + j
                            tg = sc * (T // 128) + tl
                            for hf in range(2):
                                pY = banks[4 + (j * 2 + hf) % 4]
                                hc = slice(hf * 512, (hf + 1) * 512)
                                for m in range(4):
                                    s.op("pe", lambda e, pY=pY, HID=HID, WD=WD, m=m, j=j, hc=hc: e.matmul(
                                        pY[:, :], lhsT=HID[:, m, j * 128:(j + 1) * 128], rhs=WD[:, m, hc], start=(m == 0),
                                        stop=(m == 3)), reads=[HID, WD], writes=[pY])
                                s.op("dve", lambda e, pY=pY, tl=tl, tg=tg, hc=hc, e_=e_: e.scalar_tensor_tensor(
                                    out=yacc[:, tl, hc], in0=pY[:, :], scalar=Gall[:, tg, e_:e_ + 1], in1=yacc[:, tl, hc],
                                    op0=ALU.mult, op1=ALU.add), reads=[pY, Gall, yacc], writes=[yacc])
                s.dma("sp", xdst[t0:t0 + T, :].rearrange("(j p) d -> p j d", p=128), yacc[:], reads=[yacc])
        s.barrier()

    IOA = bass.IndirectOffsetOnAxis

    def phase_R(l):
        with ExitStack() as es:
            Ab = tile(es, "Ab", [128, 1024], BF16)
            RK = tile(es, "RK", [128, 32, NE])
            CNT = tile(es, "CNT", [128, 32, NE])
            INC = tile(es, "INC", [128, 32, NE])
            TMP = tile(es, "TMP", [128, 32, NE])
            ones32 = tile(es, "ones32", [128, 32])
            sm = tile(es, "sm", [128, 8, 32])
            smi = tile(es, "smi", [128, 32], I32)
            SF = tile(es, "SF", [128, 2, 32])
            EB = tile(es, "EB", [128, NSB])
            EBX = tile(es, "EBX", [128, NSB])
            IDXf = tile(es, "IDXf", [128, NSB, 2])
            TOT, PC, PEND, BASE, YY, NBf, junk = (sm[:, i, :] for i in range(7))
            fl = "p a b -> p (a b)"
            s.op("dve", lambda e: e.memset(ones32[:], 1.0), writes=[ones32])
            s.op("dve", lambda e: e.tensor_tensor(out=Ab[:], in0=OH1[:].rearrange(fl), in1=OH2[:].rearrange(fl), op=ALU.add),
                 reads=[OH1, OH2], writes=[Ab])
            for c in range(2):
                cs = slice(c * 512, (c + 1) * 512)
                s.op("pe", lambda e, c=c, cs=cs: e.matmul(banks[c][:, :], lhsT=Ltb[:], rhs=Ab[:, cs], start=True, stop=True),
                     reads=[Ltb, Ab], writes=[banks[c]])
                evac(c, RK[:].rearrange(fl)[:, cs], banks[c][:, :], [banks[c]], [RK])
                s.op("pe", lambda e, c=c, cs=cs: e.matmul(banks[2 + c][:, :], lhsT=ones_b[:], rhs=Ab[:, cs], start=True,
                                                          stop=True), reads=[ones_b, Ab], writes=[banks[2 + c]])
                evac(c + 1, CNT[:].rearrange(fl)[:, cs], banks[2 + c][:, :], [banks[2 + c]], [CNT])
            for e_ in range(NE):
                s.op("dve", lambda e, e_=e_: e.tensor_tensor_scan(out=INC[:, :, e_], data0=ones32[:], data1=CNT[:, :, e_],
                                                                  initial=0.0, op0=ALU.mult, op1=ALU.add),
                     reads=[ones32, CNT], writes=[INC])
            s.op("dve", lambda e: e.tensor_copy(out=TOT, in_=INC[:, 31, :]), reads=[INC], writes=[sm])
            s.op("dve", lambda e: e.tensor_scalar(out=YY, in0=TOT, scalar1=1.0 / 512, scalar2=511.0 / 512 - 0.4990234375,
                                                  op0=ALU.mult, op1=ALU.add), reads=[sm], writes=[sm])
            s.op("dve", lambda e: e.tensor_copy(out=smi[:], in_=YY), reads=[sm], writes=[smi])
            s.op("dve", lambda e: e.tensor_copy(out=NBf, in_=smi[:]), reads=[smi], writes=[sm])
            s.op("dve", lambda e: e.tensor_scalar(out=PC, in0=NBf, scalar1=512.0, scalar2=None, op0=ALU.mult),
                 reads=[sm], writes=[sm])
            s.op("dve", lambda e: e.tensor_tensor_scan(out=PEND, data0=ones32[:], data1=PC, initial=0.0, op0=ALU.mult,
                                                       op1=ALU.add), reads=[sm, ones32], writes=[sm])
            s.op("dve", lambda e: e.tensor_tensor(out=BASE, in0=PEND, in1=PC, op=ALU.subtract), reads=[sm], writes=[sm])
            s.op("dve", lambda e: e.tensor_tensor(out=RK[:], in0=RK[:], in1=INC[:], op=ALU.add), reads=[RK, INC], writes=[RK])
            s.op("dve", lambda e: e.tensor_tensor(out=RK[:], in0=RK[:], in1=CNT[:], op=ALU.subtract), reads=[RK, CNT],
                 writes=[RK])
            for t in range(32):
                s.op("dve", lambda e, t=t: e.tensor_tensor(out=RK[:, t, :], in0=RK[:, t, :], in1=BASE, op=ALU.add),
                     reads=[RK, sm], writes=[RK])
            for (OH, k, Si) in ((OH1, 0, S1i), (OH2, 1, S2i)):
                s.op("dve", lambda e, OH=OH: e.tensor_tensor(out=TMP[:], in0=OH[:], in1=RK[:], op=ALU.mult),
                     reads=[OH, RK], writes=[TMP])
                s.op("dve", lambda e, k=k: e.tensor_reduce(out=SF[:, k, :], in_=TMP[:], axis=mybir.AxisListType.X,
                                                           op=ALU.add), reads=[TMP], writes=[SF])
                s.op("dve", lambda e, k=k, Si=Si: e.tensor_copy(out=Si[:], in_=SF[:, k, :]), reads=[SF], writes=[Si])
            for sb in range(NSB):
                s.op("dve", lambda e, sb=sb: e.tensor_scalar(out=junk, in0=PEND, scalar1=512.0 * sb, scalar2=None,
                                                             op0=ALU.is_le, op1=ALU.add, accum_out=EB[:, sb:sb + 1]),
                     reads=[sm], writes=[sm, EB])
            s.op("dve", lambda e: e.tensor_scalar(out=EBX[:], in0=EB[:], scalar1=float(NE) - 0.5, scalar2=1.0e4,
                                                  op0=ALU.is_gt, op1=ALU.mult), reads=[EB], writes=[EBX])
            s.op("dve", lambda e: e.scalar_tensor_tensor(out=EB[:], in0=EB[:], scalar=float(NE * l), in1=EBX[:],
                                                         op0=ALU.add, op1=ALU.add), reads=[EB, EBX], writes=[EB])
            for k2 in range(2):
                s.op("dve", lambda e, k2=k2: e.tensor_scalar(out=IDXf[:, :, k2], in0=EB[:], scalar1=256.0,
                                                             scalar2=pio2[:, k2:k2 + 1], op0=ALU.mult, op1=ALU.add),
                     reads=[EB, pio2], writes=[IDXf])
            s.op("dve", lambda e: e.tensor_copy(out=IDXWi[:], in_=IDXf[:]), reads=[IDXf], writes=[IDXWi])
        s.barrier()

    def phase_S():
        with ExitStack() as es:
            ht = [tile(es, "ht%d" % i, [128, D]) for i in range(2)]
            for t in range(32):
                H = ht[t % 2]
                s.dma("sp", H[:], h2tok[t * 128:(t + 1) * 128, :], writes=[H])
                s.indirect(xbuf[:, :], IOA(ap=S1i[:, t:t + 1], axis=0), H[:], None, reads=[H, S1i])
                s.indirect(xbuf[:, :], IOA(ap=S2i[:, t:t + 1], axis=0), H[:], None, reads=[H, S2i])
        s.barrier()

    def phase_E2(l):
        wgt = w_gate.rearrange("l e (p k2 k4) n -> (l e p k2) (k4 n)", p=128, k2=2)
        wut = w_up.rearrange("l e (p k2 k4) n -> (l e p k2) (k4 n)", p=128, k2=2)
        wdt = w_down.rearrange("l e (p m2 m4) n -> (l e p m2) (m4 n)", p=128, m2=2)
        with ExitStack() as es:
            wg = [tile(es, "wg%d" % i, [128, 8, 512], F32R) for i in range(2)]
            wu = [tile(es, "wu%d" % i, [128, 8, 512], F32R) for i in range(2)]
            wd = [tile(es, "wd%d" % i, [128, 4, D], F32R) for i in range(2)]
            xb = [tile(es, "xb%d" % i, [128, 4, D]) for i in range(2)]
            xbT = [tile(es, "xbT%d" % i, [128, 8, 512], F32R) for i in range(2)]
            hid = [tile(es, "hid%d" % i, [128, 4, 512], F32R) for i in range(1)] * 2
            sg = [tile(es, "sg%d" % i, [128, 512]) for i in range(2)]
            yb = [tile(es, "yb%d" % i, [128, 4, D]) for i in range(1)] * 2
            fl = "p a b -> p (a b)"
            gi = 0

            def loadw(sb):
                if _DBG_SKIP_W and sb >= 2:
                    return
                WG, WU, WD = wg[sb % 2], wu[sb % 2], wd[sb % 2]
                for (W, tab) in ((WG, wgt), (WU, wut), (WD, wdt)):
                    Wf = W[:].rearrange(fl)
                    for k2 in range(2):
                        s.indirect(Wf[:, k2 * 2048:(k2 + 1) * 2048], None, tab[:, :],
                                   IOA(ap=IDXWi[:, sb, k2:k2 + 1], axis=0), reads=[IDXWi], writes=[W],
                                   bounds=DEPTH * NE * 256 - 1)

            def loadx(sb):
                s.dma("sp", xb[sb % 2][:], xbuf[sb * 512:(sb + 1) * 512, :].rearrange("(j p) d -> p j d", p=128),
                      writes=[xb[sb % 2]])

            def transposes(sb):
                XB, XT = xb[sb % 2], xbT[sb % 2]
                for k in range(8):
                    pT = banks[k % 2]
                    for j in range(4):
                        s.op("pe", lambda e, pT=pT, j=j, k=k: e.transpose(pT[:, j * 128:(j + 1) * 128], XB[:, j, k::8],
                                                                          ident[:]), reads=[XB, ident], writes=[pT])
                    evac(k, XT[:, k, :], pT[:, :], [pT], [XT])

            loadw(0)
            loadx(0)
            transposes(0)
            for sb in range(NSB):
                if sb + 1 < NSB:
                    loadw(sb + 1)
                    loadx(sb + 1)
                WG, WU, WD, XT, HID, YB = wg[sb % 2], wu[sb % 2], wd[sb % 2], xbT[sb % 2], hid[sb % 2], yb[sb % 2]
                for m in range(4):
                    pG, pU, SG = banks[2 + gi % 2], banks[4 + gi % 2], sg[gi % 2]
                    gi += 1
                    for k in range(8):
                        s.op("pe", lambda e, pG=pG, k=k, m=m: e.matmul(pG[:, :], lhsT=WG[:, k, m::4], rhs=XT[:, k, :],
                                                                       start=(k == 0), stop=(k == 7)),
                             reads=[WG, XT], writes=[pG])
                    for k in range(8):
                        s.op("pe", lambda e, pU=pU, k=k, m=m: e.matmul(pU[:, :], lhsT=WU[:, k, m::4], rhs=XT[:, k, :],
                                                                       start=(k == 0), stop=(k == 7)),
                             reads=[WU, XT], writes=[pU])
                    s.op("act", lambda e, SG=SG, pG=pG: e.activation(out=SG[:], in_=pG[:, :], func=AF.Silu),
                         reads=[pG], writes=[SG])
                    s.op("dve", lambda e, SG=SG, pU=pU, m=m: e.tensor_tensor(out=HID[:, m, :], in0=SG[:], in1=pU[:, :],
                                                                             op=ALU.mult), reads=[SG, pU], writes=[HID])
                if sb + 1 < NSB:
                    transposes(sb + 1)
                ev = 0
                for j in range(4):
                    for hf in range(2):
                        pY = banks[6 + (j * 2 + hf) % 2]
                        hc = slice(hf * 512, (hf + 1) * 512)
                        for m in range(4):
                            s.op("pe", lambda e, pY=pY, m=m, j=j, hc=hc: e.matmul(
                                pY[:, :], lhsT=HID[:, m, j * 128:(j + 1) * 128], rhs=WD[:, m, hc], start=(m == 0),
                                stop=(m == 3)), reads=[HID, WD], writes=[pY])
                        evac(ev, YB[:, j, hc], pY[:, :], [pY], [YB])
                        ev += 1
                s.dma("sp", ybuf[sb * 512:(sb + 1) * 512, :].rearrange("(j p) d -> p j d", p=128), YB[:], reads=[YB])
        s.barrier()

    def phase_G(xdst):
        with ExitStack() as es:
            xt = [tile(es, "xg%d" % i, [128, D]) for i in range(2)]
            y1 = [tile(es, "y1%d" % i, [128, D]) for i in range(2)]
            y2 = [tile(es, "y2%d" % i, [128, D]) for i in range(2)]
            for t in range(32):
                X, Y1, Y2 = xt[t % 2], y1[t % 2], y2[t % 2]
                s.dma("sp", X[:], xmid[t * 128:(t + 1) * 128, :], writes=[X])
                s.indirect(Y1[:], None, ybuf[:, :], IOA(ap=S1i[:, t:t + 1], axis=0), reads=[S1i], writes=[Y1])
                s.indirect(Y2[:], None, ybuf[:, :], IOA(ap=S2i[:, t:t + 1], axis=0), reads=[S2i], writes=[Y2])
                s.op("dve", lambda e, X=X, Y1=Y1, t=t: e.scalar_tensor_tensor(out=X[:], in0=Y1[:], scalar=W12[:, t, 0:1],
                                                                              in1=X[:], op0=ALU.mult, op1=ALU.add),
                     reads=[Y1, W12, X], writes=[X])
                s.op("dve", lambda e, X=X, Y2=Y2, t=t: e.scalar_tensor_tensor(out=X[:], in0=Y2[:], scalar=W12[:, t, 1:2],
                                                                              in1=X[:], op0=ALU.mult, op1=ALU.add),
                     reads=[Y2, W12, X], writes=[X])
                s.dma("sp", xdst[t * 128:(t + 1) * 128, :], X[:], reads=[X])
        s.barrier()

    def zero_xbuf():
        with ExitStack() as es:
            zt = tile(es, "zt", [128, 8, D])
            s.op("dve", lambda e: e.memset(zt[:], 0.0), writes=[zt])
            for i in range(NSB * 512 // 1024):
                s.dma("sp", xbuf[i * 1024:(i + 1) * 1024, :].rearrange("(j p) d -> p j d", p=128), zt[:], reads=[zt])
        s.barrier()

    xcur = x_in
    for l in range(n_layers):
        if MOE_SPARSE and l > 0:
            phase_A(l, xmid, fuse_g=True, xstore=xres[l % 2])
            xcur = xres[l % 2]
        else:
            phase_A(l, xcur)
        if stop_after == "A":
            break
        phase_B(l)
        if stop_after == "B":
            break
        phase_C(l)
        if stop_after == "C":
            break
        phase_D(l, xcur)
        if stop_after == "D":
            break
        xnext = out if l == n_layers - 1 else xres[l % 2]
        if MOE_SPARSE:
            phase_R(l)
            phase_S()
            phase_E2(l)
            if l == n_layers - 1:
                phase_G(xnext)
        else:
            phase_E(l, xnext)
        xcur = xnext
    s.barrier()
    gstack.close()
    return nc, s


_CONSTS = None


def _consts():
    global _CONSTS
    if _CONSTS is None:
        j = np.arange(128)[:, None]
        i = np.arange(128)[None, :]
        mask = np.concatenate([(i <= j), (i >= j)], axis=1).astype(np.float32)
        lt = (j < i).astype(np.float32)
        pio2 = (2 * np.arange(128)[:, None] + np.arange(2)[None, :]).astype(np.float32)
        _CONSTS = {"c_ident": np.eye(128, dtype=np.float32), "c_mask": np.ascontiguousarray(mask),
                   "c_lt": np.ascontiguousarray(lt), "c_pio2": np.ascontiguousarray(pio2),
                   "c_mbias": np.ascontiguousarray(np.concatenate([mask[:, 128:], mask[:, :128]], axis=1))}
    return _CONSTS


def kernel(**inputs):
    nc, _ = build()
    x = np.ascontiguousarray(inputs["x"], dtype=np.float32)
    shared = {k: np.ascontiguousarray(v, dtype=np.float32) for k, v in inputs.items() if k != "x"}
    shared.update(_consts())
    in_maps = []
    for b in range(8):
        m = dict(shared)
        m["x"] = x[b]
        in_maps.append(m)
    res = run_bass_kernel_spmd(nc, in_maps, core_ids=list(range(8)))
    return np.stack([np.asarray(r["out"], dtype=np.float32) for r in res.results], axis=0)
```

```python
from contextlib import ExitStack

import numpy as np
import concourse.bass as bass
import concourse.mybir as mybir
from concourse.bass_utils import run_bass_kernel_spmd

F32 = mybir.dt.float32
F32R = mybir.dt.float32r
BF16 = mybir.dt.bfloat16
AF = mybir.ActivationFunctionType
ALU = mybir.AluOpType

S = 4096
D = 1024
DEPTH = 4
DIN = 2560
NE = 32
EPS = 1e-6
SAME_ENGINE_SYNC = True
MOE_SPARSE = True
_DBG_SKIP_W = False
NSB = 47
I32 = mybir.dt.int32


class Buf:
    __slots__ = ("ap", "w", "r", "name")

    def __init__(self, ap, name=""):
        self.ap = ap
        self.w = {}
        self.r = {}
        self.name = name

    def __getitem__(self, k):
        return self.ap[k]


def _merge(d, ev):
    for k, v in ev.items():
        if d.get(k, 0) < v:
            d[k] = v


class Sched:
    def __init__(self, nc, n_dma_sems=24):
        self.nc = nc
        self.E = {"pe": nc.tensor, "act": nc.scalar, "dve": nc.vector, "pool": nc.gpsimd, "sp": nc.sync}
        self.csem = {}
        self.ccnt = {}
        for e in ("pe", "act", "dve", "pool"):
            self.csem[e] = nc.alloc_semaphore("c_" + e)
            self.ccnt[e] = 0
        self.nring = n_dma_sems
        self.dsem = [nc.alloc_semaphore("d_%d" % i) for i in range(2 * n_dma_sems)]
        self.dcnt = [0] * (2 * n_dma_sems)
        self.dnext = {False: 0, True: 0}
        self.seen = {e: {} for e in self.E}
        self.n_inst = 0
        self.n_wait = 0
        self.bregs = {}

    def _sem(self, key):
        return self.csem[key[1]] if key[0] == "c" else self.dsem[key[1]]

    def _wait(self, eng, ev):
        seen = self.seen[eng]
        for key, val in ev.items():
            if key[0] == "c" and key[1] == eng:
                if eng == "pe" or not SAME_ENGINE_SYNC:
                    continue
            if seen.get(key, 0) >= val:
                continue
            self.E[eng].wait_ge(self._sem(key), val)
            seen[key] = val
            self.n_wait += 1

    def _deps(self, eng, reads, writes):
        for b in reads:
            self._wait(eng, b.w)
        for b in writes:
            self._wait(eng, b.w)
            self._wait(eng, b.r)

    def _post(self, ev, reads, writes):
        for b in reads:
            _merge(b.r, ev)
        for b in writes:
            _merge(b.w, ev)

    def op(self, eng, fn, reads=(), writes=()):
        self._deps(eng, reads, writes)
        inst = fn(self.E[eng])
        self.ccnt[eng] += 1
        inst.then_inc(self.csem[eng], 1)
        ev = {("c", eng): self.ccnt[eng]}
        self._post(ev, reads, writes)
        self.n_inst += 1
        return ev

    def _ring(self, sw):
        i = self.dnext[sw]
        self.dnext[sw] = (i + 1) % self.nring
        return i + (self.nring if sw else 0)

    def dma(self, eng, out, in_, reads=(), writes=(), **kw):
        i = self._ring(eng == "pool")
        if self.dcnt[i] > 0:
            self._wait(eng, {("d", i): self.dcnt[i]})
        self._deps(eng, reads, writes)
        self.dcnt[i] += 16
        self.E[eng].dma_start(out=out, in_=in_, **kw).then_inc(self.dsem[i], 16)
        ev = {("d", i): self.dcnt[i]}
        self._post(ev, reads, writes)
        self.n_inst += 1
        return ev

    def indirect(self, out, out_off, in_, in_off, reads=(), writes=(), bounds=None):
        i = self._ring(True)
        if self.dcnt[i] > 0:
            self._wait("pool", {("d", i): self.dcnt[i]})
        self._deps("pool", reads, writes)
        self.dcnt[i] += 16
        kw = {}
        if bounds is not None:
            if bounds not in self.bregs:
                self.bregs[bounds] = self.nc.gpsimd.to_reg(bounds)
            kw = dict(bounds_check=self.bregs[bounds], oob_is_err=False)
        self.nc.gpsimd.indirect_dma_start(out=out, out_offset=out_off, in_=in_, in_offset=in_off, **kw).then_inc(
            self.dsem[i], 16)
        ev = {("d", i): self.dcnt[i]}
        self._post(ev, reads, writes)
        self.n_inst += 1
        return ev

    def barrier(self):
        allev = {}
        for e, c in self.ccnt.items():
            if c:
                allev[("c", e)] = c
        for i, c in enumerate(self.dcnt):
            if c:
                allev[("d", i)] = c
        for eng in self.E:
            self._wait(eng, allev)


def build(n_layers=DEPTH, debug=False, stop_after=None):
    nc = bass.Bass("TRN2", target_bir_lowering=False)
    s = Sched(nc)

    def din(name, shape):
        return nc.dram_tensor(name, shape, F32, kind="ExternalInput").ap()

    def dtmp(name, shape, dt=F32):
        return nc.dram_tensor(name, shape, dt, kind=("ExternalOutput" if debug else "Internal")).ap()

    x_in = din("x", [S, D])
    norm_mix = din("norm_mix", [DEPTH, D])
    w_in = din("w_in", [DEPTH, D, DIN])
    conv_w = din("conv_w", [DEPTH, 4, 512])
    conv_b = din("conv_b", [DEPTH, 512])
    lru_w_a = din("lru_w_a", [DEPTH, 8, 64, 64])
    lru_b_a = din("lru_b_a", [DEPTH, 512])
    lru_w_x = din("lru_w_x", [DEPTH, 8, 64, 64])
    lru_b_x = din("lru_b_x", [DEPTH, 512])
    lru_lambda = din("lru_lambda", [DEPTH, 512])
    q_norm = din("q_norm", [DEPTH, 64])
    k_norm = din("k_norm", [DEPTH, 64])
    norm_out_lru = din("norm_out_lru", [DEPTH, 512])
    norm_out_attn = din("norm_out_attn", [DEPTH, 512])
    w_out = din("w_out", [DEPTH, D, D])
    norm_ffn = din("norm_ffn", [DEPTH, D])
    router_group_w = din("router_group_w", [DEPTH, D, 4])
    router_group_b = din("router_group_b", [DEPTH, 4])
    router_expert_w = din("router_expert_w", [DEPTH, D, NE])
    router_expert_b = din("router_expert_b", [DEPTH, NE])
    w_gate = din("w_gate", [DEPTH, NE, D, 512])
    w_up = din("w_up", [DEPTH, NE, D, 512])
    w_down = din("w_down", [DEPTH, NE, 512, D])
    c_ident = din("c_ident", [128, 128])
    c_mask = din("c_mask", [128, 256])
    c_lt = din("c_lt", [128, 128])
    c_mbias = din("c_mbias", [128, 256])
    c_pio2 = din("c_pio2", [128, 2])
    out = nc.dram_tensor("out", [S, D], F32, kind="ExternalOutput").ap()

    projT = dtmp("projT", [2048, S])
    vtok = dtmp("vtok", [S, 512], BF16)
    ymixT = dtmp("ymixT", [1024, S])
    mergedT = dtmp("mergedT", [1024, S])
    xmid = dtmp("xmid", [S, D])
    h2T = dtmp("h2T", [1024, S])
    xres = [dtmp("xres%d" % i, [S, D]) for i in range(2)]
    h2tok = dtmp("h2tok", [S, D])
    xbuf = dtmp("xbuf", [NSB * 512, D])
    ybuf = dtmp("ybuf", [NSB * 512, D])

    tcount = [0]

    def tile(es, name, shape, dt=F32):
        tcount[0] += 1
        name = "%s_%d" % (name, tcount[0])
        return Buf(es.enter_context(nc.sbuf_tensor(name, shape, dt)), name)

    gstack = ExitStack()
    ident = tile(gstack, "ident", [128, 128])
    ones_r = tile(gstack, "ones_r", [128, 128], F32R)
    ones_b = tile(gstack, "ones_b", [128, 128], BF16)
    OH1 = tile(gstack, "OH1", [128, 32, NE])
    OH2 = tile(gstack, "OH2", [128, 32, NE])
    W12 = tile(gstack, "W12", [128, 32, 2])
    S1i = tile(gstack, "S1i", [128, 32], I32)
    S2i = tile(gstack, "S2i", [128, 32], I32)
    IDXWi = tile(gstack, "IDXWi", [128, NSB, 2], I32)
    pio2 = tile(gstack, "pio2", [128, 2])
    Ltb = tile(gstack, "Ltb", [128, 128], BF16)
    identb = tile(gstack, "identb", [128, 128], BF16)
    mask01 = tile(gstack, "mask01", [128, 256], BF16)
    Gall = None if MOE_SPARSE else tile(gstack, "Gall", [128, 32, NE])
    banks = [Buf(nc.alloc_psum_tensor("bank%d" % i, [128, 512], F32), "bank%d" % i) for i in range(8)]

    s.dma("sp", ident[:], c_ident[:, :], writes=[ident])
    s.dma("pool", Ltb[:], c_lt[:, :], writes=[Ltb])
    s.dma("pool", identb[:], c_ident[:, :], writes=[identb])
    s.dma("pool", mask01[:], c_mbias[:, :], writes=[mask01])
    s.dma("sp", pio2[:], c_pio2[:, :], writes=[pio2])
    ones_f = tile(gstack, "ones_f", [128, 128])
    eps_t = tile(gstack, "eps_t", [128, 1])
    s.op("dve", lambda e: e.memset(eps_t[:], EPS), writes=[eps_t])
    zeros_f = tile(gstack, "zeros_f", [128, 512])
    s.op("dve", lambda e: e.memset(ones_f[:], 1.0), writes=[ones_f])
    s.op("dve", lambda e: e.memset(zeros_f[:], 0.0), writes=[zeros_f])
    s.op("dve", lambda e: e.tensor_copy(out=ones_r[:], in_=ones_f[:]), reads=[ones_f], writes=[ones_r])
    s.op("dve", lambda e: e.memset(ones_b[:], 1.0), writes=[ones_b])

    def evac(i, dst_ap, src_ap, reads, writes):
        if i % 2 == 0:
            return s.op("act", lambda e: e.copy(out=dst_ap, in_=src_ap), reads=reads, writes=writes)
        return s.op("dve", lambda e: e.tensor_copy(out=dst_ap, in_=src_ap), reads=reads, writes=writes)

    def rstd_inplace(T, ap, scale):
        np_ = ap.partition_size()
        s.op("act", lambda e: e.activation(out=ap, in_=ap, func=AF.Ln, scale=scale, bias=eps_t[0:np_, 0:1]),
             reads=[T, eps_t], writes=[T])
        s.op("act", lambda e: e.activation(out=ap, in_=ap, func=AF.Exp, scale=-0.5), reads=[T], writes=[T])

    def rms_token_major(X, Y, SS, junk, gbc):
        for j in range(4):
            jt, jap = (junk, junk[:]) if junk is not None else (Y, Y[:, j, :])
            s.op("act", lambda e, j=j, jap=jap: e.activation(out=jap, in_=X[:, j, :], func=AF.Square,
                                                             accum_out=SS[:, j:j + 1]),
                 reads=[X], writes=[jt, SS])
        rstd_inplace(SS, SS[:], 1.0 / D)
        for j in range(4):
            s.op("dve", lambda e, j=j: e.scalar_tensor_tensor(out=Y[:, j, :], in0=X[:, j, :], scalar=SS[:, j:j + 1],
                                                              in1=gbc[:], op0=ALU.mult, op1=ALU.mult),
                 reads=[X, SS, gbc], writes=[Y])

    def transpose_chunk(X, HT, pbanks):
        for k in range(8):
            pT = pbanks[k % len(pbanks)]
            for j in range(4):
                s.op("pe", lambda e, j=j, k=k, pT=pT: e.transpose(pT[:, j * 128:(j + 1) * 128],
                                                                  X[:, j, k * 128:(k + 1) * 128], ident[:]),
                     reads=[X, ident], writes=[pT])
            evac(k, HT[:, k, :], pT[:, :], [pT], [HT])

    def phase_A(l, xsrc, fuse_g=False, xstore=None):
        with ExitStack() as es:
            win = tile(es, "win", [128, 8, DIN], F32R)
            gbc = tile(es, "gbc", [128, D])
            xin = [tile(es, "xin%d" % i, [128, 4, D]) for i in range(3)]
            junk = tile(es, "junk", [128, D])
            ss = [tile(es, "ss%d" % i, [128, 4]) for i in range(3)]
            hT = [tile(es, "hT%d" % i, [128, 8, 512], F32R) for i in range(2)]
            ost = [tile(es, "ost%d" % i, [128, 512]) for i in range(4)]
            vst = [tile(es, "vst%d" % i, [128, 512], BF16) for i in range(2)]
            if fuse_g:
                y1t = [tile(es, "y1t%d" % i, [128, D]) for i in range(2)]
                y2t = [tile(es, "y2t%d" % i, [128, D]) for i in range(2)]
            wsrc = w_in[l].rearrange("(k p) n -> p k n", p=128)
            for c5 in range(5):
                s.dma("pool", win[:, :, c5 * 512:(c5 + 1) * 512], wsrc[:, :, c5 * 512:(c5 + 1) * 512], writes=[win])
            s.dma("pool", gbc[:], norm_mix[l].partition_broadcast(128), writes=[gbc])

            def load(c):
                X = xin[c % 3]
                rows = slice(c * 512, (c + 1) * 512)
                s.dma("sp", X[:], xsrc[rows, :].rearrange("(j p) d -> p j d", p=128), writes=[X])
                if fuse_g:
                    for j in range(4):
                        t = c * 4 + j
                        Y1, Y2 = y1t[t % 2], y2t[t % 2]
                        s.indirect(Y1[:], None, ybuf[:, :], IOA(ap=S1i[:, t:t + 1], axis=0), reads=[S1i], writes=[Y1])
                        s.indirect(Y2[:], None, ybuf[:, :], IOA(ap=S2i[:, t:t + 1], axis=0), reads=[S2i], writes=[Y2])
                        s.op("dve", lambda e, Y1=Y1, t=t, j=j: e.scalar_tensor_tensor(
                            out=X[:, j, :], in0=Y1[:], scalar=W12[:, t, 0:1], in1=X[:, j, :], op0=ALU.mult, op1=ALU.add),
                            reads=[Y1, W12, X], writes=[X])
                        s.op("dve", lambda e, Y2=Y2, t=t, j=j: e.scalar_tensor_tensor(
                            out=X[:, j, :], in0=Y2[:], scalar=W12[:, t, 1:2], in1=X[:, j, :], op0=ALU.mult, op1=ALU.add),
                            reads=[Y2, W12, X], writes=[X])
                    s.dma("sp", xstore[rows, :].rearrange("(j p) d -> p j d", p=128), X[:], reads=[X])

            def prep_rms(c):
                rms_token_major(xin[c % 3], xin[c % 3], ss[c % 3], junk, gbc)

            def prep_T(c):
                transpose_chunk(xin[c % 3], hT[c % 2], banks[0:2])

            evc = [0]

            def mm(c):
                HT = hT[c % 2]
                for f in range(16):
                    pO = banks[2 + f % 4]
                    for k in range(8):
                        s.op("pe", lambda e, f=f, k=k, pO=pO: e.matmul(pO[:, :], lhsT=win[:, k, f * 128:(f + 1) * 128],
                                                                       rhs=HT[:, k, :], start=(k == 0), stop=(k == 7)),
                             reads=[win, HT], writes=[pO])
                    O = ost[f % 4]
                    evac(evc[0], O[:], pO[:, :], [pO], [O])
                    evc[0] += 1
                    s.dma("sp", projT[f * 128:(f + 1) * 128, c * 512:(c + 1) * 512], O[:], reads=[O])
                for j in range(4):
                    pV = banks[6 + j % 2]
                    for k in range(8):
                        s.op("pe", lambda e, j=j, k=k, pV=pV: e.matmul(pV[:, :], lhsT=HT[:, k, j * 128:(j + 1) * 128],
                                                                       rhs=win[:, k, 2048:2560], start=(k == 0),
                                                                       stop=(k == 7)),
                             reads=[win, HT], writes=[pV])
                    V = vst[j % 2]
                    evac(evc[0], V[:], pV[:, :], [pV], [V])
                    evc[0] += 1
                    t0 = c * 512 + j * 128
                    s.dma("sp", vtok[t0:t0 + 128, :], V[:], reads=[V])

            load(0)
            load(1)
            if MOE_SPARSE and l == 0:
                zt = tile(es, "zt", [128, 2, D])
                s.op("pool", lambda e: e.memset(zt[:], 0.0), writes=[zt])
                for i in range(NSB * 2):
                    s.dma("pool", xbuf[i * 256:(i + 1) * 256, :].rearrange("(j p) d -> p j d", p=128), zt[:], reads=[zt])
            prep_rms(0)
            prep_T(0)
            prep_rms(1)
            for c in range(8):
                if c + 2 < 8:
                    load(c + 2)
                    prep_rms(c + 2)
                if c + 1 < 8:
                    prep_T(c + 1)
                mm(c)
        s.barrier()

    def phase_B(l):
        HT_ = 1024
        NQ = S // HT_
        with ExitStack() as es:
            lp = tile(es, "lp", [128, 4, 16])
            tmpp = tile(es, "tmpp", [128, 4, 4])
            wab = tile(es, "wab", [128, 4, 128], F32R)
            wxb = tile(es, "wxb", [128, 4, 128], F32R)
            xl_2 = [tile(es, "xl%d" % i_, [128, 3 + HT_], F32R) for i_ in range(2)]
            xc_2 = [tile(es, "xc%d" % i_, [128, HT_], F32R) for i_ in range(2)]
            dg = tile(es, "dg", [128, 4, 128], F32R)
            rt_2 = [tile(es, "rt%d" % i_, [128, HT_]) for i_ in range(2)]
            it_2 = [tile(es, "it%d" % i_, [128, HT_]) for i_ in range(2)]
            wt_2 = [tile(es, "wt%d" % i_, [128, HT_]) for i_ in range(2)]
            em_2 = [tile(es, "em%d" % i_, [128, HT_]) for i_ in range(2)]
            t1_2 = [tile(es, "t1%d" % i_, [128, HT_]) for i_ in range(2)]
            at_2 = [tile(es, "at%d" % i_, [128, HT_]) for i_ in range(2)]
            hh_2 = [tile(es, "hh%d" % i_, [128, HT_]) for i_ in range(2)]
            gt_2 = [tile(es, "gt%d" % i_, [128, HT_]) for i_ in range(2)]
            t2_2 = [tile(es, "t2%d" % i_, [128, HT_]) for i_ in range(2)]
            hlast = tile(es, "hlast", [128, 1])
            def pload(col, src):
                s.dma("sp", lp[:, :, col], src.rearrange("(j p) -> p j", p=128), writes=[lp],
                      allow_slow_non_contiguous=True)
            for t in range(4):
                pload(t, conv_w[l, t])
            pload(4, conv_b[l])
            pload(5, lru_b_a[l])
            pload(6, lru_b_x[l])
            pload(7, lru_lambda[l])
            z = tmpp[:, :, 0]
            q = tmpp[:, :, 1]
            lnz = tmpp[:, :, 2]
            msk = tmpp[:, :, 3]
            s.op("act", lambda e: e.activation(out=z, in_=lp[:, :, 7], func=AF.Exp, scale=-1.0), reads=[lp], writes=[tmpp])
            s.op("act", lambda e: e.activation(out=lnz, in_=z, func=AF.Ln, bias=1.0), reads=[tmpp], writes=[tmpp])
            coef = [1.0, -1.0 / 2, 1.0 / 3, -1.0 / 4, 1.0 / 5, -1.0 / 6, 1.0 / 7]
            s.op("dve", lambda e: e.tensor_scalar(out=q, in0=z, scalar1=coef[6], scalar2=None, op0=ALU.mult),
                 reads=[tmpp], writes=[tmpp])
            for ci in (5, 4, 3, 2, 1, 0):
                s.op("dve", lambda e, ci=ci: e.scalar_tensor_tensor(out=q, in0=q, scalar=coef[ci], in1=z, op0=ALU.add,
                                                                    op1=ALU.mult), reads=[tmpp], writes=[tmpp])
            s.op("dve", lambda e: e.tensor_scalar(out=msk, in0=z, scalar1=0.1, scalar2=None, op0=ALU.is_lt),
                 reads=[tmpp], writes=[tmpp])
            s.op("dve", lambda e: e.tensor_tensor(out=q, in0=q, in1=lnz, op=ALU.subtract), reads=[tmpp], writes=[tmpp])
            s.op("dve", lambda e: e.tensor_tensor(out=q, in0=q, in1=msk, op=ALU.mult), reads=[tmpp], writes=[tmpp])
            s.op("dve", lambda e: e.tensor_tensor(out=q, in0=q, in1=lnz, op=ALU.add), reads=[tmpp], writes=[tmpp])
            s.op("dve", lambda e: e.tensor_scalar(out=lp[:, :, 8], in0=q, scalar1=-8.0, scalar2=None, op0=ALU.mult),
                 reads=[tmpp], writes=[lp])
            s.op("dve", lambda e: e.tensor_scalar(out=lp[:, :, 9], in0=q, scalar1=-4.0, scalar2=None, op0=ALU.mult),
                 reads=[tmpp], writes=[lp])
            s.op("dve", lambda e: e.tensor_copy(out=wab[:].rearrange("p a b -> p (a b)"), in_=zeros_f[:]),
                 reads=[zeros_f], writes=[wab])
            s.op("dve", lambda e: e.tensor_copy(out=wxb[:].rearrange("p a b -> p (a b)"), in_=zeros_f[:]),
                 reads=[zeros_f], writes=[wxb])
            for jj in range(4):
                for hb in range(2):
                    p0 = hb * 64
                    s.dma("pool", wab[p0:p0 + 64, jj, p0:p0 + 64], lru_w_a[l, 2 * jj + hb], writes=[wab])
                    s.dma("pool", wxb[p0:p0 + 64, jj, p0:p0 + 64], lru_w_x[l, 2 * jj + hb], writes=[wxb])
            bk = [0]
            def run_pass(jj, hf, pi_):
                t0 = hf * HT_
                xl, xc, rt, it, wt, em = xl_2[pi_ % 2], xc_2[pi_ % 2], rt_2[pi_ % 2], it_2[pi_ % 2], wt_2[pi_ % 2], em_2[pi_ % 2]
                t1, at, hh, gt, t2 = t1_2[pi_ % 2], at_2[pi_ % 2], hh_2[pi_ % 2], gt_2[pi_ % 2], t2_2[pi_ % 2]
                if hf == 0:
                    s.op("dve", lambda e: e.tensor_copy(out=xl[:, 0:3], in_=zeros_f[:, 0:3]), reads=[zeros_f],
                         writes=[xl])
                    yield
                    s.dma("pool", xl[:, 3:3 + HT_], projT[jj * 128:(jj + 1) * 128, 0:HT_], writes=[xl])
                    yield
                    for k in range(4):
                        s.op("dve", lambda e, jj=jj, k=k: e.tensor_scalar(out=dg[:, k, :], in0=ident[:],
                                                                          scalar1=lp[:, jj, k:k + 1], scalar2=None,
                                                                          op0=ALU.mult), reads=[ident, lp], writes=[dg])
                        yield
                else:
                    s.dma("pool", xl[:, :], projT[jj * 128:(jj + 1) * 128, t0 - 3:t0 + HT_], writes=[xl])
                    yield
                s.dma("sp", gt[:], projT[512 + jj * 128:512 + (jj + 1) * 128, t0:t0 + HT_], writes=[gt])
                yield
                for cb in range(HT_ // 512):
                    pC = banks[bk[0] % 8]
                    bk[0] += 1
                    for k in range(4):
                        s.op("pe", lambda e, pC=pC, cb=cb, k=k: e.matmul(
                            pC[:, :], lhsT=dg[:, k, :], rhs=xl[:, cb * 512 + k:cb * 512 + k + 512], start=(k == 0),
                            stop=(k == 3)), reads=[dg, xl], writes=[pC])
                        yield
                    s.op("act", lambda e, pC=pC, cb=cb, jj=jj: e.add(out=xc[:, cb * 512:(cb + 1) * 512], in_=pC[:, :],
                                                                     add=lp[:, jj, 4:5]), reads=[pC, lp], writes=[xc])
                    yield
                for cb in range(HT_ // 512):
                    cols = slice(cb * 512, (cb + 1) * 512)
                    for (wb, dst, bcol) in ((wab, rt, 5), (wxb, it, 6)):
                        pR = banks[bk[0] % 8]
                        bk[0] += 1
                        s.op("pe", lambda e, wb=wb, pR=pR, cols=cols, jj=jj: e.matmul(
                            pR[:, :], lhsT=wb[:, jj, :], rhs=xc[:, cols], start=True, stop=True),
                            reads=[wb, xc], writes=[pR])
                        yield
                        s.op("act", lambda e, dst=dst, pR=pR, cols=cols, jj=jj, bcol=bcol: e.activation(
                            out=dst[:, cols], in_=pR[:, :], func=AF.Sigmoid, bias=lp[:, jj, bcol:bcol + 1]),
                            reads=[pR, lp], writes=[dst])
                        yield
                s.op("act", lambda e, jj=jj: e.activation(out=at[:], in_=rt[:], func=AF.Exp, scale=lp[:, jj, 8:9]),
                     reads=[rt, lp], writes=[at])
                yield
                s.op("act", lambda e, jj=jj: e.activation(out=t1[:], in_=rt[:], func=AF.Tanh, scale=lp[:, jj, 9:10]),
                     reads=[rt, lp], writes=[t1])
                yield
                s.op("dve", lambda e: e.scalar_tensor_tensor(out=em[:], in0=at[:], scalar=1.0, in1=t1[:], op0=ALU.add,
                                                             op1=ALU.mult), reads=[at, t1], writes=[em])
                yield
                s.op("dve", lambda e: e.scalar_tensor_tensor(out=t1[:], in0=em[:], scalar=2.0, in1=em[:], op0=ALU.add,
                                                             op1=ALU.mult), reads=[em], writes=[t1])
                yield
                s.op("act", lambda e: e.activation(out=t1[:], in_=t1[:], func=AF.Sqrt, scale=-1.0),
                     reads=[t1], writes=[t1])
                yield
                s.op("pool", lambda e: e.tensor_tensor(out=it[:], in0=it[:], in1=xc[:].bitcast(F32), op=ALU.mult),
                     reads=[it, xc], writes=[it])
                yield
                s.op("dve", lambda e: e.tensor_tensor(out=it[:], in0=it[:], in1=t1[:], op=ALU.mult),
                     reads=[it, t1], writes=[it])
                yield
                if hf == 0:
                    s.op("dve", lambda e: e.tensor_tensor_scan(out=hh[:], data0=at[:], data1=it[:], initial=0.0,
                                                               op0=ALU.mult, op1=ALU.add),
                         reads=[at, it], writes=[hh])
                    yield
                else:
                    s.op("dve", lambda e: e.tensor_tensor_scan(out=hh[:], data0=at[:], data1=it[:],
                                                               initial=hlast[:, 0:1], op0=ALU.mult, op1=ALU.add),
                         reads=[at, it, hlast], writes=[hh])
                    yield
                if hf + 1 < NQ:
                    s.op("act", lambda e: e.copy(out=hlast[:, 0:1], in_=hh[:, HT_ - 1:HT_]), reads=[hh], writes=[hlast])
                    yield
                s.op("pool", lambda e: e.tensor_tensor(out=t2[:], in0=gt[:], in1=gt[:], op=ALU.mult),
                     reads=[gt], writes=[t2])
                yield
                s.op("pool", lambda e: e.tensor_scalar(out=t2[:], in0=t2[:], scalar1=0.044715, scalar2=1.0, op0=ALU.mult,
                                                       op1=ALU.add), reads=[t2], writes=[t2])
                yield
                s.op("pool", lambda e: e.tensor_tensor(out=t2[:], in0=t2[:], in1=gt[:], op=ALU.mult),
                     reads=[t2, gt], writes=[t2])
                yield
                s.op("act", lambda e: e.activation(out=t2[:], in_=t2[:], func=AF.Sigmoid, scale=1.5957691216057308),
                     reads=[t2], writes=[t2])
                yield
                s.op("pool", lambda e: e.tensor_tensor(out=t2[:], in0=t2[:], in1=gt[:], op=ALU.mult),
                     reads=[t2, gt], writes=[t2])
                yield
                s.op("dve", lambda e: e.tensor_tensor(out=t2[:], in0=t2[:], in1=hh[:], op=ALU.mult),
                     reads=[t2, hh], writes=[t2])
                yield
                s.dma("sp", ymixT[jj * 128:(jj + 1) * 128, t0:t0 + HT_], t2[:], reads=[t2])
                yield

            passes = [(jj, hf) for jj in range(4) for hf in range(NQ)]
            gens = [run_pass(jj, hf, i_) for i_, (jj, hf) in enumerate(passes)]
            SKEW = 18
            active = []
            nxt = 0
            while active or nxt < len(gens):
                if nxt < len(gens) and len(active) < 2 and (not active or active[0][1] >= SKEW):
                    active.append([gens[nxt], 0])
                    nxt += 1
                for ent in list(active):
                    try:
                        next(ent[0])
                        ent[1] += 1
                    except StopIteration:
                        active.remove(ent)
        s.barrier()

    def phase_C(l):
        NBUF, LA = 6, 3
        with ExitStack() as es:
            gqk = tile(es, "gqk", [64, 2])
            qraw = [tile(es, "qraw%d" % i, [64, S]) for i in range(1)] * 2
            kraw = [tile(es, "kraw%d" % i, [64, S]) for i in range(1)] * 2
            qn = [tile(es, "qn%d" % i, [64, S], BF16) for i in range(2)]
            kn = [tile(es, "kn%d" % i, [64, S], BF16) for i in range(2)]
            Vd = [[tile(es, "Vd%d_%d" % (i, j), [128, 32, 128], BF16) for j in range(3)] for i in range(2)]
            for i in range(2):
                for j in range(3):
                    s.op("dve", lambda e, i=i, j=j: e.memset(Vd[i][j][:], 1.0), writes=[Vd[i][j]])
            acc2 = [tile(es, "acc%d" % i, [128, S]) for i in range(2)]
            dsh = tile(es, "dsh", [64, S])
            sq = [tile(es, "sq%d" % i, [64, 512], F32R) for i in range(2)]
            rs = [tile(es, "rs%d" % i, [64, 512]) for i in range(2)]
            pm_ = [tile(es, "pm%d" % i, [128, 256], BF16) for i in range(NBUF)]
            NPS = 3
            pS_ = [banks[i] for i in range(NPS)]
            pO_ = [banks[3 + i] for i in range(NPS)]
            pN_ = [banks[6], banks[7]]
            s.dma("sp", gqk[:, 0:1], q_norm[l].rearrange("(p o) -> p o", o=1), writes=[gqk])
            s.dma("sp", gqk[:, 1:2], k_norm[l].rearrange("(p o) -> p o", o=1), writes=[gqk])

            def load(h):
                s.dma("sp", qraw[h % 2][:], projT[1024 + h * 64:1024 + (h + 1) * 64, :], writes=[qraw[h % 2]])
                s.dma("sp", kraw[h % 2][:], projT[1536 + h * 64:1536 + (h + 1) * 64, :], writes=[kraw[h % 2]])
                for bi, d in enumerate((1, 4, 16)):
                    nb = 32 // d
                    vv = vtok.rearrange("(n p r) c -> r p n c", p=128, r=d)
                    V = Vd[h % 2][bi]
                    for r in range(d):
                        s.dma("sp", V[:, r * nb:(r + 1) * nb, 0:64], vv[r][:, :, h * 64:(h + 1) * 64], writes=[V])

            def qknorm_steps(h):
                steps = []
                for (raw, nrm, gc) in ((qraw[h % 2], qn[h % 2], 0), (kraw[h % 2], kn[h % 2], 1)):
                    for cb in range(8):
                        cols = slice(cb * 512, (cb + 1) * 512)
                        SQ, RS, pN = sq[cb % 2], rs[cb % 2], pN_[cb % 2]

                        def half_a(raw=raw, cols=cols, SQ=SQ, pN=pN):
                            s.op("act", lambda e: e.activation(out=SQ[:], in_=raw[:, cols], func=AF.Square),
                                 reads=[raw], writes=[SQ])
                            s.op("pe", lambda e: e.matmul(pN[0:64, :], lhsT=ones_r[0:64, 0:64], rhs=SQ[:], start=True,
                                                          stop=True), reads=[ones_r, SQ], writes=[pN])

                        def half_b(raw=raw, nrm=nrm, gc=gc, cols=cols, RS=RS, pN=pN):
                            s.op("act", lambda e: e.activation(out=RS[:], in_=pN[0:64, :], func=AF.Ln, scale=1.0 / 64,
                                                               bias=eps_t[0:64, 0:1]), reads=[pN, eps_t], writes=[RS])
                            s.op("act", lambda e: e.activation(out=RS[:], in_=RS[:], func=AF.Exp, scale=-0.5),
                                 reads=[RS], writes=[RS])
                            s.op("dve", lambda e: e.scalar_tensor_tensor(out=nrm[:, cols], in0=raw[:, cols],
                                                                         scalar=gqk[:, gc:gc + 1], in1=RS[:],
                                                                         op0=ALU.mult, op1=ALU.mult),
                                 reads=[raw, gqk, RS], writes=[nrm])
                        steps.append(half_a)
                        steps.append(half_b)
                return steps

            load(0)
            for st in qknorm_steps(0):
                st()
            fin_pending = []
            for h in range(8):
                pending = list(fin_pending)
                fin_pending = []
                if h + 1 < 8:
                    load(h + 1)
                    pending += qknorm_steps(h + 1)
                QN, KN, VD = qn[h % 2], kn[h % 2], Vd[h % 2]
                acc = acc2[h % 2]
                blocks = []
                for bi, d in enumerate((1, 4, 16)):
                    nb = 32 // d
                    for r in range(d):
                        for n in range(nb):
                            blocks.append((bi, d, nb, r, n))

                def stage1(i):
                    bi, d, nb, r, n = blocks[i]
                    qv = QN[:].rearrange("p (n i r) -> p r n i", i=128, r=d)
                    kv = KN[:].rearrange("p (n i r) -> p r n i", i=128, r=d)
                    pS, PM_ = pS_[i % NPS], pm_[i % NBUF]
                    w = 256 if n + 1 < nb else 128
                    nq = w // 128
                    s.op("pe", lambda e: e.matmul(pS[:, 0:w], lhsT=kv[:, r, n, :], rhs=qv[:, r, n:n + nq, :], start=True,
                                                  stop=True), reads=[KN, QN], writes=[pS])
                    s.op("act", lambda e: e.activation(out=PM_[:, 0:w], in_=pS[:, 0:w], func=AF.Exp, scale=0.125),
                         reads=[pS], writes=[PM_])
                    s.op("dve", lambda e: e.tensor_tensor(out=PM_[:, 0:w], in0=PM_[:, 0:w], in1=mask01[:, 0:w], op=ALU.mult),
                         reads=[PM_, mask01], writes=[PM_])

                def stage2(i):
                    bi, d, nb, r, n = blocks[i]
                    av = acc[:].rearrange("p (n i r) -> p r n i", i=128, r=d)
                    pO, PMc, PMp = pO_[i % NPS], pm_[i % NBUF], pm_[(i - 1) % NBUF]
                    V = VD[bi]
                    blk = r * nb + n
                    if n > 0:
                        s.op("pe", lambda e: e.matmul(pO[:, 0:128], lhsT=V[:, blk - 1, :], rhs=PMp[:, 128:256], start=True,
                                                      stop=False), reads=[V, PMp], writes=[pO])
                        s.op("pe", lambda e: e.matmul(pO[:, 0:128], lhsT=V[:, blk, :], rhs=PMc[:, 0:128], start=False,
                                                      stop=True), reads=[V, PMc], writes=[pO])
                    else:
                        s.op("pe", lambda e: e.matmul(pO[:, 0:128], lhsT=V[:, blk, :], rhs=PMc[:, 0:128], start=True,
                                                      stop=True), reads=[V, PMc], writes=[pO])
                    dst = av[:, r, n, :]
                    if bi == 0:
                        s.op("dve", lambda e: e.tensor_copy(out=dst, in_=pO[:, 0:128]), reads=[pO], writes=[acc])
                    else:
                        s.op("dve", lambda e: e.tensor_tensor(out=dst, in0=dst, in1=pO[:, 0:128], op=ALU.add),
                             reads=[pO, acc], writes=[acc])

                nblk = len(blocks)
                for i in range(nblk + LA):
                    if i < nblk:
                        stage1(i)
                    if i - LA >= 0:
                        stage2(i - LA)
                    if pending and i % 2 == 1:
                        pending.pop(0)()
                while pending:
                    pending.pop(0)()
                def fin_steps(h=h, A=acc):
                    st = []
                    for cb in range(8):
                        def piece(cb=cb):
                            cols = slice(cb * 512, (cb + 1) * 512)
                            s.op("act", lambda e: e.activation(out=A[64:128, cols], in_=A[64:128, cols], func=AF.Ln),
                                 reads=[A], writes=[A])
                            s.op("act", lambda e: e.activation(out=A[64:128, cols], in_=A[64:128, cols], func=AF.Exp,
                                                               scale=-1.0), reads=[A], writes=[A])
                            s.op("dve", lambda e: e.tensor_copy(out=dsh[:, cols], in_=A[64:128, cols]), reads=[A],
                                 writes=[dsh])
                            s.op("dve", lambda e: e.tensor_tensor(out=dsh[:, cols], in0=A[0:64, cols], in1=dsh[:, cols],
                                                                  op=ALU.mult), reads=[A, dsh], writes=[dsh])
                        st.append(piece)
                    st.append(lambda: s.dma("sp", ymixT[512 + h * 64:512 + (h + 1) * 64, :], dsh[:], reads=[dsh]))
                    return st
                fin_pending = fin_steps()
            for st_ in fin_pending:
                st_()
        s.barrier()

    def phase_N(l):
        with ExitStack() as es:
            gg = tile(es, "gg", [128, 2, 4])
            ya = [tile(es, "ya%d" % i, [128, 4, 512]) for i in range(2)]
            sq4 = [tile(es, "sq4%d" % i, [128, 4, 512], F32R) for i in range(2)]
            rsn = [tile(es, "rsn%d" % i, [128, 512]) for i in range(2)]
            mo = [tile(es, "mo%d" % i, [128, 4, 512]) for i in range(2)]
            s.dma("sp", gg[:, 0, :], norm_out_lru[l].rearrange("(j p) -> p j", p=128), writes=[gg],
                  allow_slow_non_contiguous=True)
            s.dma("sp", gg[:, 1, :], norm_out_attn[l].rearrange("(j p) -> p j", p=128), writes=[gg],
                  allow_slow_non_contiguous=True)
            it_ = 0
            for half in range(2):
                for cb in range(8):
                    cols = slice(cb * 512, (cb + 1) * 512)
                    YA, SQ, RS, MO, pS = ya[it_ % 2], sq4[it_ % 2], rsn[it_ % 2], mo[it_ % 2], banks[it_ % 2]
                    it_ += 1
                    s.dma("sp", YA[:], ymixT[half * 512:(half + 1) * 512, cols].rearrange("(j p) c -> p j c", p=128),
                          writes=[YA])
                    for jj in range(4):
                        s.op("act", lambda e, YA=YA, SQ=SQ, jj=jj: e.activation(out=SQ[:, jj, :], in_=YA[:, jj, :],
                                                                                func=AF.Square),
                             reads=[YA], writes=[SQ])
                    for jj in range(4):
                        s.op("pe", lambda e, SQ=SQ, pS=pS, jj=jj: e.matmul(pS[:, :], lhsT=ones_r[:, :], rhs=SQ[:, jj, :],
                                                                           start=(jj == 0), stop=(jj == 3)),
                             reads=[ones_r, SQ], writes=[pS])
                    s.op("dve", lambda e, RS=RS, pS=pS: e.tensor_scalar(out=RS[:], in0=pS[:, :], scalar1=1.0 / 512,
                                                                        scalar2=EPS, op0=ALU.mult, op1=ALU.add),
                         reads=[pS], writes=[RS])
                    s.op("act", lambda e, RS=RS: e.activation(out=RS[:], in_=RS[:], func=AF.Sqrt), reads=[RS], writes=[RS])
                    s.op("dve", lambda e, RS=RS: e.reciprocal(out=RS[:], in_=RS[:]), reads=[RS], writes=[RS])
                    for jj in range(4):
                        s.op("dve", lambda e, YA=YA, MO=MO, RS=RS, jj=jj, half=half: e.scalar_tensor_tensor(
                            out=MO[:, jj, :], in0=YA[:, jj, :], scalar=gg[:, half, jj:jj + 1], in1=RS[:], op0=ALU.mult,
                            op1=ALU.mult), reads=[YA, gg, RS], writes=[MO])
                    s.dma("sp", mergedT[half * 512:(half + 1) * 512, cols].rearrange("(j p) c -> p j c", p=128), MO[:],
                          reads=[MO])
        s.barrier()

    def phase_D(l, xsrc):
        with ExitStack() as es:
            wout = tile(es, "wout", [128, 8, D], F32R)
            gbc = tile(es, "gbc2", [128, D])
            wr = tile(es, "wr", [128, 8, 36])
            bbc = tile(es, "bbc", [128, 36])
            xin = [tile(es, "xd%d" % i, [128, 4, D]) for i in range(2)]
            mT = [tile(es, "mT%d" % i, [128, 8, 512], F32R) for i in range(2)]
            ym = [tile(es, "ym%d" % i, [128, 8, 512]) for i in range(2)]
            sq4 = [tile(es, "sq4%d" % i, [128, 4, 512], F32R) for i in range(1)] * 2
            rsn = [tile(es, "rsn%d" % i, [128, 512]) for i in range(1)] * 2
            gg = tile(es, "gg", [128, 8])
            h2b = [tile(es, "h2_%d" % i, [128, 4, D]) for i in range(2)]
            h2t = [tile(es, "h2t%d" % i, [128, 8, 512]) for i in range(1)] * 2
            s.dma("sp", gg[:, 0:4], norm_out_lru[l].rearrange("(j p) -> p j", p=128), writes=[gg],
                  allow_slow_non_contiguous=True)
            s.dma("sp", gg[:, 4:8], norm_out_attn[l].rearrange("(j p) -> p j", p=128), writes=[gg],
                  allow_slow_non_contiguous=True)
            junk = None
            ss = [tile(es, "ssd%d" % i, [128, 4]) for i in range(2)]
            rt_ = [tile(es, "rtd%d" % i, [128, 96]) for i in range(2)]
            LGS = tile(es, "LGS", [128, 32, 4])
            DD = tile(es, "DD", [128, 32])
            PT = tile(es, "PT", [128, 32])
            wsrc = w_out[l].rearrange("(k p) n -> p k n", p=128)
            for c2 in range(2):
                s.dma("pool", wout[:, :, c2 * 512:(c2 + 1) * 512], wsrc[:, :, c2 * 512:(c2 + 1) * 512], writes=[wout])
            s.dma("pool", gbc[:], norm_ffn[l].partition_broadcast(128), writes=[gbc])
            s.dma("sp", wr[:, :, 0:4], router_group_w[l].rearrange("(k p) n -> p k n", p=128), writes=[wr])
            s.dma("sp", wr[:, :, 4:36], router_expert_w[l].rearrange("(k p) n -> p k n", p=128), writes=[wr])
            s.dma("pool", bbc[:, 0:4], router_group_b[l].partition_broadcast(128), writes=[bbc])
            s.dma("pool", bbc[:, 4:36], router_expert_b[l].partition_broadcast(128), writes=[bbc])

            def load(c):
                cols = slice(c * 512, (c + 1) * 512)
                s.dma("sp", ym[c % 2][:], ymixT[:, cols].rearrange("(k p) c -> p k c", p=128), writes=[ym[c % 2]])
                s.dma("sp", xin[c % 2][:], xsrc[c * 512:(c + 1) * 512, :].rearrange("(j p) d -> p j d", p=128),
                      writes=[xin[c % 2]])

            nrm_i = [0]

            def mixnorm(c):
                YM, MT = ym[c % 2], mT[c % 2]
                for half in range(2):
                    i_ = nrm_i[0]
                    nrm_i[0] += 1
                    SQ, RS, pS = sq4[i_ % 2], rsn[i_ % 2], banks[i_ % 2]
                    for jj in range(4):
                        s.op("act", lambda e, jj=jj: e.activation(out=SQ[:, jj, :], in_=YM[:, half * 4 + jj, :],
                                                                  func=AF.Square), reads=[YM], writes=[SQ])
                    for jj in range(4):
                        s.op("pe", lambda e, jj=jj: e.matmul(pS[:, :], lhsT=ones_r[:, :], rhs=SQ[:, jj, :], start=(jj == 0),
                                                             stop=(jj == 3)), reads=[ones_r, SQ], writes=[pS])
                    s.op("act", lambda e: e.activation(out=RS[:], in_=pS[:, :], func=AF.Ln, scale=1.0 / 512,
                                                       bias=eps_t[:, 0:1]), reads=[pS, eps_t], writes=[RS])
                    s.op("act", lambda e: e.activation(out=RS[:], in_=RS[:], func=AF.Exp, scale=-0.5), reads=[RS],
                         writes=[RS])
                    for jj in range(4):
                        k_ = half * 4 + jj
                        s.op("dve", lambda e, k_=k_: e.scalar_tensor_tensor(out=MT[:, k_, :], in0=YM[:, k_, :],
                                                                            scalar=gg[:, k_:k_ + 1], in1=RS[:],
                                                                            op0=ALU.mult, op1=ALU.mult),
                             reads=[YM, gg, RS], writes=[MT])

            pi = [0]

            def MM(c):
                X, MT = xin[c % 2], mT[c % 2]
                for j in range(4):
                    for hf in range(2):
                        pY = banks[2 + pi[0] % 4]
                        pi[0] += 1
                        hc = slice(hf * 512, (hf + 1) * 512)
                        for k in range(8):
                            s.op("pe", lambda e, pY=pY, k=k, j=j, hc=hc: e.matmul(
                                pY[:, :], lhsT=MT[:, k, j * 128:(j + 1) * 128], rhs=wout[:, k, hc], start=(k == 0),
                                stop=(k == 7)), reads=[MT, wout], writes=[pY])
                        s.op("dve", lambda e, pY=pY, j=j, hc=hc: e.tensor_tensor(out=X[:, j, hc], in0=pY[:, :],
                                                                                in1=X[:, j, hc], op=ALU.add),
                             reads=[pY, X], writes=[X])
                s.dma("pool", xmid[c * 512:(c + 1) * 512, :].rearrange("(j p) d -> p j d", p=128), X[:], reads=[X])

            def RMS(c):
                rms_token_major(xin[c % 2], h2b[c % 2], ss[c % 2], junk, gbc)

            def TR(c):
                H2T = h2t[c % 2]
                h2 = h2b[c % 2]
                transpose_chunk(h2, H2T, banks[0:2])
                if MOE_SPARSE:
                    s.dma("pool", h2tok[c * 512:(c + 1) * 512, :].rearrange("(j p) d -> p j d", p=128), h2[:], reads=[h2])
                else:
                    s.dma("pool", h2T[:, c * 512:(c + 1) * 512].rearrange("(k p) c -> p k c", p=128), H2T[:], reads=[H2T])
                router(c, H2T)

            def router(c, H2T):
                for j in range(4):
                    R, pR = rt_[j % 2], banks[6 + j % 2]
                    ti = c * 4 + j
                    for k in range(8):
                        s.op("pe", lambda e, pR=pR, k=k, j=j: e.matmul(
                            pR[:, 0:36], lhsT=H2T[:, k, j * 128:(j + 1) * 128], rhs=wr[:, k, :], start=(k == 0),
                            stop=(k == 7)), reads=[H2T, wr], writes=[pR])
                    lg, gmax, oh, pen, m8 = R[:, 0:36], R[:, 36:37], R[:, 40:44], R[:, 44:48], R[:, 52:60]

                    def dv(fn, R=R, er=(), ew=()):
                        s.op("dve", fn, reads=[R] + list(er), writes=[R] + list(ew))

                    s.op("dve", lambda e, lg=lg, pR=pR: e.tensor_tensor(out=lg, in0=pR[:, 0:36], in1=bbc[:], op=ALU.add),
                         reads=[pR, bbc], writes=[R])
                    dv(lambda e, lg=lg, gmax=gmax: e.tensor_reduce(out=gmax, in_=lg[:, 0:4], axis=mybir.AxisListType.X,
                                                                   op=ALU.max))
                    dv(lambda e, lg=lg, gmax=gmax, ti=ti: e.tensor_scalar(out=LGS[:, ti, :], in0=lg[:, 0:4], scalar1=gmax,
                                                                          scalar2=None, op0=ALU.subtract), ew=[LGS])
                    dv(lambda e, lg=lg, gmax=gmax, oh=oh: e.tensor_scalar(out=oh, in0=lg[:, 0:4], scalar1=gmax, scalar2=None,
                                                                          op0=ALU.is_equal))
                    dv(lambda e, oh=oh, pen=pen: e.tensor_scalar(out=pen, in0=oh, scalar1=-1.0, scalar2=1.0e30,
                                                                 op0=ALU.add, op1=ALU.mult))
                    for g in range(4):
                        dv(lambda e, lg=lg, pen=pen, g=g: e.tensor_scalar(
                            out=lg[:, 4 + g * 8:12 + g * 8], in0=lg[:, 4 + g * 8:12 + g * 8], scalar1=pen[:, g:g + 1],
                            scalar2=None, op0=ALU.add))
                    dv(lambda e, m8=m8, lg=lg: e.max(out=m8, in_=lg[:, 4:36]))
                    dv(lambda e, m8=m8, ti=ti: e.tensor_tensor(out=DD[:, ti:ti + 1], in0=m8[:, 1:2], in1=m8[:, 0:1],
                                                               op=ALU.subtract), ew=[DD])
                    dv(lambda e, lg=lg, m8=m8, ti=ti: e.tensor_scalar(out=OH1[:, ti, :], in0=lg[:, 4:36], scalar1=m8[:, 0:1],
                                                                      scalar2=None, op0=ALU.is_equal), ew=[OH1])
                    dv(lambda e, lg=lg, m8=m8, ti=ti: e.tensor_scalar(out=OH2[:, ti, :], in0=lg[:, 4:36], scalar1=m8[:, 1:2],
                                                                      scalar2=None, op0=ALU.is_equal), ew=[OH2])

            def gate_weights():
                fl = "p a b -> p (a b)"
                s.op("act", lambda e: e.activation(out=LGS[:].rearrange(fl), in_=LGS[:].rearrange(fl), func=AF.Exp),
                     reads=[LGS], writes=[LGS])
                s.op("dve", lambda e: e.tensor_reduce(out=PT[:], in_=LGS[:], axis=mybir.AxisListType.X, op=ALU.add),
                     reads=[LGS], writes=[PT])
                s.op("dve", lambda e: e.reciprocal(out=PT[:], in_=PT[:]), reads=[PT], writes=[PT])
                s.op("act", lambda e: e.activation(out=DD[:], in_=DD[:], func=AF.Exp), reads=[DD], writes=[DD])
                s.op("dve", lambda e: e.tensor_scalar(out=DD[:], in0=DD[:], scalar1=1.0, scalar2=None, op0=ALU.add),
                     reads=[DD], writes=[DD])
                s.op("dve", lambda e: e.reciprocal(out=DD[:], in_=DD[:]), reads=[DD], writes=[DD])
                s.op("dve", lambda e: e.tensor_tensor(out=W12[:, :, 0], in0=DD[:], in1=PT[:], op=ALU.mult),
                     reads=[DD, PT], writes=[W12])
                s.op("dve", lambda e: e.tensor_tensor(out=W12[:, :, 1], in0=PT[:], in1=W12[:, :, 0], op=ALU.subtract),
                     reads=[PT, W12], writes=[W12])

            load(0)
            mixnorm(0)
            if 1 < 8:
                load(1)
                mixnorm(1)
            MM(0)
            RMS(0)
            for c in range(8):
                if c + 2 < 8:
                    load(c + 2)
                    mixnorm(c + 2)
                if c + 1 < 8:
                    MM(c + 1)
                    RMS(c + 1)
                TR(c)
            gate_weights()
        s.barrier()

    def phase_E(l, xdst):
        T = 1024
        with ExitStack() as es:
            hsc = tile(es, "hsc", [128, 8, T], F32R)
            yacc = tile(es, "yacc", [128, T // 128, D])
            wg = [tile(es, "wg%d" % i, [128, 8, 512], F32R) for i in range(2)]
            wu = [tile(es, "wu%d" % i, [128, 8, 512], F32R) for i in range(2)]
            wd = [tile(es, "wd%d" % i, [128, 4, D], F32R) for i in range(2)]
            hid = [tile(es, "hid%d" % i, [128, 4, 512], F32R) for i in range(2)]
            sg = [tile(es, "sg%d" % i, [128, 512]) for i in range(2)]
            wi = 0
            gi = 0
            hi = 0
            for sc in range(S // T):
                t0 = sc * T
                s.dma("pool", hsc[:], h2T[:, t0:t0 + T].rearrange("(k p) c -> p k c", p=128), writes=[hsc])
                s.dma("sp", yacc[:], xmid[t0:t0 + T, :].rearrange("(j p) d -> p j d", p=128), writes=[yacc])
                for e_ in range(NE):
                    WG, WU, WD = wg[wi % 2], wu[wi % 2], wd[wi % 2]
                    wi += 1
                    s.dma("pool", WG[:], w_gate[l, e_].rearrange("(k p) n -> p k n", p=128), writes=[WG])
                    s.dma("pool", WU[:], w_up[l, e_].rearrange("(k p) n -> p k n", p=128), writes=[WU])
                    s.dma("pool", WD[:], w_down[l, e_].rearrange("(m p) n -> p m n", p=128), writes=[WD])
                    for sb in range(T // 512):
                        cols = slice(sb * 512, (sb + 1) * 512)
                        HID = hid[hi % 2]
                        hi += 1
                        for m in range(4):
                            pG, pU, SG = banks[gi % 2], banks[2 + gi % 2], sg[gi % 2]
                            gi += 1
                            for k in range(8):
                                s.op("pe", lambda e, pG=pG, WG=WG, k=k, m=m, cols=cols: e.matmul(
                                    pG[:, :], lhsT=WG[:, k, m * 128:(m + 1) * 128], rhs=hsc[:, k, cols], start=(k == 0),
                                    stop=(k == 7)), reads=[WG, hsc], writes=[pG])
                            for k in range(8):
                                s.op("pe", lambda e, pU=pU, WU=WU, k=k, m=m, cols=cols: e.matmul(
                                    pU[:, :], lhsT=WU[:, k, m * 128:(m + 1) * 128], rhs=hsc[:, k, cols], start=(k == 0),
                                    stop=(k == 7)), reads=[WU, hsc], writes=[pU])
                            s.op("act", lambda e, SG=SG, pG=pG: e.activation(out=SG[:], in_=pG[:, :], func=AF.Silu),
                                 reads=[pG], writes=[SG])
                            s.op("dve", lambda e, HID=HID, SG=SG, pU=pU, m=m: e.tensor_tensor(
                                out=HID[:, m, :], in0=SG[:], in1=pU[:, :], op=ALU.mult), reads=[SG, pU], writes=[HID])
                        for j in range(4):
                            tl = sb * 4 + j
                            tg = sc * (T // 128) + tl
                            for hf in range(2):
                                pY = banks[4 + (j * 2 + hf) % 4]
                                hc = slice(hf * 512, (hf + 1) * 512)
                                for m in range(4):
                                    s.op("pe", lambda e, pY=pY, HID=HID, WD=WD, m=m, j=j, hc=hc: e.matmul(
                                        pY[:, :], lhsT=HID[:, m, j * 128:(j + 1) * 128], rhs=WD[:, m, hc], start=(m == 0),
                                        stop=(m == 3)), reads=[HID, WD], writes=[pY])
                                s.op("dve", lambda e, pY=pY, tl=tl, tg=tg, hc=hc, e_=e_: e.scalar_tensor_tensor(
                                    out=yacc[:, tl, hc], in0=pY[:, :], scalar=Gall[:, tg, e_:e_ + 1], in1=yacc[:, tl, hc],
                                    op0=ALU.mult, op1=ALU.add), reads=[pY, Gall, yacc], writes=[yacc])
                s.dma("sp", xdst[t0:t0 + T, :].rearrange("(j p) d -> p j d", p=128), yacc[:], reads=[yacc])
        s.barrier()

    IOA = bass.IndirectOffsetOnAxis

    def phase_R(l):
        with ExitStack() as es:
            Ab = tile(es, "Ab", [128, 1024], BF16)
            RK = tile(es, "RK", [128, 32, NE])
            CNT = tile(es, "CNT", [128, 32, NE])
            INC = tile(es, "INC", [128, 32, NE])
            TMP = tile(es, "TMP", [128, 32, NE])
            ones32 = tile(es, "ones32", [128, 32])
            sm = tile(es, "sm", [128, 8, 32])
            smi = tile(es, "smi", [128, 32], I32)
            SF = tile(es, "SF", [128, 2, 32])
            EB = tile(es, "EB", [128, NSB])
            EBX = tile(es, "EBX", [128, NSB])
            IDXf = tile(es, "IDXf", [128, NSB, 2])
            TOT, PC, PEND, BASE, YY, NBf, junk = (sm[:, i, :] for i in range(7))
            fl = "p a b -> p (a b)"
            s.op("dve", lambda e: e.memset(ones32[:], 1.0), writes=[ones32])
            s.op("dve", lambda e: e.tensor_tensor(out=Ab[:], in0=OH1[:].rearrange(fl), in1=OH2[:].rearrange(fl), op=ALU.add),
                 reads=[OH1, OH2], writes=[Ab])
            for c in range(2):
                cs = slice(c * 512, (c + 1) * 512)
                s.op("pe", lambda e, c=c, cs=cs: e.matmul(banks[c][:, :], lhsT=Ltb[:], rhs=Ab[:, cs], start=True, stop=True),
                     reads=[Ltb, Ab], writes=[banks[c]])
                evac(c, RK[:].rearrange(fl)[:, cs], banks[c][:, :], [banks[c]], [RK])
                s.op("pe", lambda e, c=c, cs=cs: e.matmul(banks[2 + c][:, :], lhsT=ones_b[:], rhs=Ab[:, cs], start=True,
                                                          stop=True), reads=[ones_b, Ab], writes=[banks[2 + c]])
                evac(c + 1, CNT[:].rearrange(fl)[:, cs], banks[2 + c][:, :], [banks[2 + c]], [CNT])
            for e_ in range(NE):
                s.op("dve", lambda e, e_=e_: e.tensor_tensor_scan(out=INC[:, :, e_], data0=ones32[:], data1=CNT[:, :, e_],
                                                                  initial=0.0, op0=ALU.mult, op1=ALU.add),
                     reads=[ones32, CNT], writes=[INC])
            s.op("dve", lambda e: e.tensor_copy(out=TOT, in_=INC[:, 31, :]), reads=[INC], writes=[sm])
            s.op("dve", lambda e: e.tensor_scalar(out=YY, in0=TOT, scalar1=1.0 / 512, scalar2=511.0 / 512 - 0.4990234375,
                                                  op0=ALU.mult, op1=ALU.add), reads=[sm], writes=[sm])
            s.op("dve", lambda e: e.tensor_copy(out=smi[:], in_=YY), reads=[sm], writes=[smi])
            s.op("dve", lambda e: e.tensor_copy(out=NBf, in_=smi[:]), reads=[smi], writes=[sm])
            s.op("dve", lambda e: e.tensor_scalar(out=PC, in0=NBf, scalar1=512.0, scalar2=None, op0=ALU.mult),
                 reads=[sm], writes=[sm])
            s.op("dve", lambda e: e.tensor_tensor_scan(out=PEND, data0=ones32[:], data1=PC, initial=0.0, op0=ALU.mult,
                                                       op1=ALU.add), reads=[sm, ones32], writes=[sm])
            s.op("dve", lambda e: e.tensor_tensor(out=BASE, in0=PEND, in1=PC, op=ALU.subtract), reads=[sm], writes=[sm])
            s.op("dve", lambda e: e.tensor_tensor(out=RK[:], in0=RK[:], in1=INC[:], op=ALU.add), reads=[RK, INC], writes=[RK])
            s.op("dve", lambda e: e.tensor_tensor(out=RK[:], in0=RK[:], in1=CNT[:], op=ALU.subtract), reads=[RK, CNT],
                 writes=[RK])
            for t in range(32):
                s.op("dve", lambda e, t=t: e.tensor_tensor(out=RK[:, t, :], in0=RK[:, t, :], in1=BASE, op=ALU.add),
                     reads=[RK, sm], writes=[RK])
            for (OH, k, Si) in ((OH1, 0, S1i), (OH2, 1, S2i)):
                s.op("dve", lambda e, OH=OH: e.tensor_tensor(out=TMP[:], in0=OH[:], in1=RK[:], op=ALU.mult),
                     reads=[OH, RK], writes=[TMP])
                s.op("dve", lambda e, k=k: e.tensor_reduce(out=SF[:, k, :], in_=TMP[:], axis=mybir.AxisListType.X,
                                                           op=ALU.add), reads=[TMP], writes=[SF])
                s.op("dve", lambda e, k=k, Si=Si: e.tensor_copy(out=Si[:], in_=SF[:, k, :]), reads=[SF], writes=[Si])
            for sb in range(NSB):
                s.op("dve", lambda e, sb=sb: e.tensor_scalar(out=junk, in0=PEND, scalar1=512.0 * sb, scalar2=None,
                                                             op0=ALU.is_le, op1=ALU.add, accum_out=EB[:, sb:sb + 1]),
                     reads=[sm], writes=[sm, EB])
            s.op("dve", lambda e: e.tensor_scalar(out=EBX[:], in0=EB[:], scalar1=float(NE) - 0.5, scalar2=1.0e4,
                                                  op0=ALU.is_gt, op1=ALU.mult), reads=[EB], writes=[EBX])
            s.op("dve", lambda e: e.scalar_tensor_tensor(out=EB[:], in0=EB[:], scalar=float(NE * l), in1=EBX[:],
                                                         op0=ALU.add, op1=ALU.add), reads=[EB, EBX], writes=[EB])
            for k2 in range(2):
                s.op("dve", lambda e, k2=k2: e.tensor_scalar(out=IDXf[:, :, k2], in0=EB[:], scalar1=256.0,
                                                             scalar2=pio2[:, k2:k2 + 1], op0=ALU.mult, op1=ALU.add),
                     reads=[EB, pio2], writes=[IDXf])
            s.op("dve", lambda e: e.tensor_copy(out=IDXWi[:], in_=IDXf[:]), reads=[IDXf], writes=[IDXWi])
        s.barrier()

    def phase_S():
        with ExitStack() as es:
            ht = [tile(es, "ht%d" % i, [128, D]) for i in range(2)]
            for t in range(32):
                H = ht[t % 2]
                s.dma("sp", H[:], h2tok[t * 128:(t + 1) * 128, :], writes=[H])
                s.indirect(xbuf[:, :], IOA(ap=S1i[:, t:t + 1], axis=0), H[:], None, reads=[H, S1i])
                s.indirect(xbuf[:, :], IOA(ap=S2i[:, t:t + 1], axis=0), H[:], None, reads=[H, S2i])
        s.barrier()

    def phase_E2(l):
        wgt = w_gate.rearrange("l e (p k2 k4) n -> (l e p k2) (k4 n)", p=128, k2=2)
        wut = w_up.rearrange("l e (p k2 k4) n -> (l e p k2) (k4 n)", p=128, k2=2)
        wdt = w_down.rearrange("l e (p m2 m4) n -> (l e p m2) (m4 n)", p=128, m2=2)
        with ExitStack() as es:
            wg = [tile(es, "wg%d" % i, [128, 8, 512], F32R) for i in range(2)]
            wu = [tile(es, "wu%d" % i, [128, 8, 512], F32R) for i in range(2)]
            wd = [tile(es, "wd%d" % i, [128, 4, D], F32R) for i in range(2)]
            xb = [tile(es, "xb%d" % i, [128, 4, D]) for i in range(2)]
            xbT = [tile(es, "xbT%d" % i, [128, 8, 512], F32R) for i in range(2)]
            hid = [tile(es, "hid%d" % i, [128, 4, 512], F32R) for i in range(1)] * 2
            sg = [tile(es, "sg%d" % i, [128, 512]) for i in range(2)]
            yb = [tile(es, "yb%d" % i, [128, 4, D]) for i in range(1)] * 2
            fl = "p a b -> p (a b)"
            gi = 0

            def loadw(sb):
                if _DBG_SKIP_W and sb >= 2:
                    return
                WG, WU, WD = wg[sb % 2], wu[sb % 2], wd[sb % 2]
                for (W, tab) in ((WG, wgt), (WU, wut), (WD, wdt)):
                    Wf = W[:].rearrange(fl)
                    for k2 in range(2):
                        s.indirect(Wf[:, k2 * 2048:(k2 + 1) * 2048], None, tab[:, :],
                                   IOA(ap=IDXWi[:, sb, k2:k2 + 1], axis=0), reads=[IDXWi], writes=[W],
                                   bounds=DEPTH * NE * 256 - 1)

            def loadx(sb):
                s.dma("sp", xb[sb % 2][:], xbuf[sb * 512:(sb + 1) * 512, :].rearrange("(j p) d -> p j d", p=128),
                      writes=[xb[sb % 2]])

            def transposes(sb):
                XB, XT = xb[sb % 2], xbT[sb % 2]
                for k in range(8):
                    pT = banks[k % 2]
                    for j in range(4):
                        s.op("pe", lambda e, pT=pT, j=j, k=k: e.transpose(pT[:, j * 128:(j + 1) * 128], XB[:, j, k::8],
                                                                          ident[:]), reads=[XB, ident], writes=[pT])
                    evac(k, XT[:, k, :], pT[:, :], [pT], [XT])

            loadw(0)
            loadx(0)
            transposes(0)
            for sb in range(NSB):
                if sb + 1 < NSB:
                    loadw(sb + 1)
                    loadx(sb + 1)
                WG, WU, WD, XT, HID, YB = wg[sb % 2], wu[sb % 2], wd[sb % 2], xbT[sb % 2], hid[sb % 2], yb[sb % 2]
                for m in range(4):
                    pG, pU, SG = banks[2 + gi % 2], banks[4 + gi % 2], sg[gi % 2]
                    gi += 1
                    for k in range(8):
                        s.op("pe", lambda e, pG=pG, k=k, m=m: e.matmul(pG[:, :], lhsT=WG[:, k, m::4], rhs=XT[:, k, :],
                                                                       start=(k == 0), stop=(k == 7)),
                             reads=[WG, XT], writes=[pG])
                    for k in range(8):
                        s.op("pe", lambda e, pU=pU, k=k, m=m: e.matmul(pU[:, :], lhsT=WU[:, k, m::4], rhs=XT[:, k, :],
                                                                       start=(k == 0), stop=(k == 7)),
                             reads=[WU, XT], writes=[pU])
                    s.op("act", lambda e, SG=SG, pG=pG: e.activation(out=SG[:], in_=pG[:, :], func=AF.Silu),
                         reads=[pG], writes=[SG])
                    s.op("dve", lambda e, SG=SG, pU=pU, m=m: e.tensor_tensor(out=HID[:, m, :], in0=SG[:], in1=pU[:, :],
                                                                             op=ALU.mult), reads=[SG, pU], writes=[HID])
                if sb + 1 < NSB:
                    transposes(sb + 1)
                ev = 0
                for j in range(4):
                    for hf in range(2):
                        pY = banks[6 + (j * 2 + hf) % 2]
                        hc = slice(hf * 512, (hf + 1) * 512)
                        for m in range(4):
                            s.op("pe", lambda e, pY=pY, m=m, j=j, hc=hc: e.matmul(
                                pY[:, :], lhsT=HID[:, m, j * 128:(j + 1) * 128], rhs=WD[:, m, hc], start=(m == 0),
                                stop=(m == 3)), reads=[HID, WD], writes=[pY])
                        evac(ev, YB[:, j, hc], pY[:, :], [pY], [YB])
                        ev += 1
                s.dma("sp", ybuf[sb * 512:(sb + 1) * 512, :].rearrange("(j p) d -> p j d", p=128), YB[:], reads=[YB])
        s.barrier()

    def phase_G(xdst):
        with ExitStack() as es:
            xt = [tile(es, "xg%d" % i, [128, D]) for i in range(2)]
            y1 = [tile(es, "y1%d" % i, [128, D]) for i in range(2)]
            y2 = [tile(es, "y2%d" % i, [128, D]) for i in range(2)]
            for t in range(32):
                X, Y1, Y2 = xt[t % 2], y1[t % 2], y2[t % 2]
                s.dma("sp", X[:], xmid[t * 128:(t + 1) * 128, :], writes=[X])
                s.indirect(Y1[:], None, ybuf[:, :], IOA(ap=S1i[:, t:t + 1], axis=0), reads=[S1i], writes=[Y1])
                s.indirect(Y2[:], None, ybuf[:, :], IOA(ap=S2i[:, t:t + 1], axis=0), reads=[S2i], writes=[Y2])
                s.op("dve", lambda e, X=X, Y1=Y1, t=t: e.scalar_tensor_tensor(out=X[:], in0=Y1[:], scalar=W12[:, t, 0:1],
                                                                              in1=X[:], op0=ALU.mult, op1=ALU.add),
                     reads=[Y1, W12, X], writes=[X])
                s.op("dve", lambda e, X=X, Y2=Y2, t=t: e.scalar_tensor_tensor(out=X[:], in0=Y2[:], scalar=W12[:, t, 1:2],
                                                                              in1=X[:], op0=ALU.mult, op1=ALU.add),
                     reads=[Y2, W12, X], writes=[X])
                s.dma("sp", xdst[t * 128:(t + 1) * 128, :], X[:], reads=[X])
        s.barrier()

    def zero_xbuf():
        with ExitStack() as es:
            zt = tile(es, "zt", [128, 8, D])
            s.op("dve", lambda e: e.memset(zt[:], 0.0), writes=[zt])
            for i in range(NSB * 512 // 1024):
                s.dma("sp", xbuf[i * 1024:(i + 1) * 1024, :].rearrange("(j p) d -> p j d", p=128), zt[:], reads=[zt])
        s.barrier()

    xcur = x_in
    for l in range(n_layers):
        if MOE_SPARSE and l > 0:
            phase_A(l, xmid, fuse_g=True, xstore=xres[l % 2])
            xcur = xres[l % 2]
        else:
            phase_A(l, xcur)
        if stop_after == "A":
            break
        phase_B(l)
        if stop_after == "B":
            break
        phase_C(l)
        if stop_after == "C":
            break
        phase_D(l, xcur)
        if stop_after == "D":
            break
        xnext = out if l == n_layers - 1 else xres[l % 2]
        if MOE_SPARSE:
            phase_R(l)
            phase_S()
            phase_E2(l)
            if l == n_layers - 1:
                phase_G(xnext)
        else:
            phase_E(l, xnext)
        xcur = xnext
    s.barrier()
    gstack.close()
    return nc, s


_CONSTS = None


def _consts():
    global _CONSTS
    if _CONSTS is None:
        j = np.arange(128)[:, None]
        i = np.arange(128)[None, :]
        mask = np.concatenate([(i <= j), (i >= j)], axis=1).astype(np.float32)
        lt = (j < i).astype(np.float32)
        pio2 = (2 * np.arange(128)[:, None] + np.arange(2)[None, :]).astype(np.float32)
        _CONSTS = {"c_ident": np.eye(128, dtype=np.float32), "c_mask": np.ascontiguousarray(mask),
                   "c_lt": np.ascontiguousarray(lt), "c_pio2": np.ascontiguousarray(pio2),
                   "c_mbias": np.ascontiguousarray(np.concatenate([mask[:, 128:], mask[:, :128]], axis=1))}
    return _CONSTS


def kernel(**inputs):
    nc, _ = build()
    x = np.ascontiguousarray(inputs["x"], dtype=np.float32)
    shared = {k: np.ascontiguousarray(v, dtype=np.float32) for k, v in inputs.items() if k != "x"}
    shared.update(_consts())
    in_maps = []
    for b in range(8):
        m = dict(shared)
        m["x"] = x[b]
        in_maps.append(m)
    res = run_bass_kernel_spmd(nc, in_maps, core_ids=list(range(8)))
    return np.stack([np.asarray(r["out"], dtype=np.float32) for r in res.results], axis=0)
```
